# Optimizing a Trainium2 kernel written in Bass

```python
import functools
import jax, jax.numpy as jnp
from jax import lax
import numpy as np

D_MODEL = 1024
BATCH = 8
SEQ = 4096
DEPTH = 1

GRID_W = 64
CTX_LEN = 256
EPS = 1e-6
N_MOD = 6
D_RNN = 1024
RG_BLOCKS = 8
RG_BLOCK = D_RNN // RG_BLOCKS
CONV_W = 4
CONV_PAD_L = 2
RG_C = 8.0
MLA_HEADS = 8
QK_NOPE = 64
QK_ROPE = 32
V_HEAD = 64
Q_LORA = 256
KV_LORA = 128
ROPE_BASE = 10000.0
ATTN_SCALE = (QK_NOPE + QK_ROPE) ** -0.5
Q_BLOCK = 128
N_BRANCH = 2
N_EXPERTS = 16
CAPACITY_FACTOR = 2
D_EXPERT = 1024
SPLIT_POINTS = (D_RNN, 2 * D_RNN, 2 * D_RNN + Q_LORA, 2 * D_RNN + Q_LORA + KV_LORA, 2 * D_RNN + Q_LORA + KV_LORA + QK_ROPE)
D_IN = 2 * D_RNN + Q_LORA + KV_LORA + QK_ROPE + N_BRANCH * D_MODEL

kernel_name = "hybrid_rglru_mla_ecmoe_diffusion_layer"


def rmsnorm(x, g):
    xf = x.astype(jnp.float32)
    y = xf * lax.rsqrt(jnp.mean(xf * xf, axis=-1, keepdims=True) + EPS)
    return (y * g.astype(jnp.float32)).astype(x.dtype)


def modulate(x, shift, scale):
    return x * (1 + scale) + shift


def axial_rope_tables(n_tokens, dtype):
    rows = n_tokens // GRID_W
    row = jnp.repeat(jnp.arange(rows, dtype=jnp.float32), GRID_W)
    col = jnp.tile(jnp.arange(GRID_W, dtype=jnp.float32), rows)
    n_pairs = QK_ROPE // 4
    inv_freq = ROPE_BASE ** (-jnp.arange(n_pairs, dtype=jnp.float32) / n_pairs)
    ang = jnp.concatenate([row[:, None] * inv_freq, col[:, None] * inv_freq], axis=-1)
    return jnp.cos(ang).astype(dtype), jnp.sin(ang).astype(dtype)


def apply_rope(x, cos, sin):
    x1, x2 = jnp.split(x, 2, axis=-1)
    return jnp.concatenate([x1 * cos - x2 * sin, x1 * sin + x2 * cos], axis=-1)


def centred_depthwise_conv(x, w, b):
    L = x.shape[1]
    xp = jnp.pad(x, ((0, 0), (CONV_PAD_L, CONV_W - 1 - CONV_PAD_L), (0, 0)))
    return sum(xp[:, k:k + L] * w[k] for k in range(CONV_W)) + b


def block_diag_linear(x, w, b):
    xb = x.reshape(*x.shape[:-1], RG_BLOCKS, RG_BLOCK)
    return jnp.einsum('blni,nij->blnj', xb, w).reshape(x.shape) + b


def rglru_coeffs(x, w_a, b_a, w_x, b_x, lam):
    r = jax.nn.sigmoid(block_diag_linear(x, w_a, b_a))
    i = jax.nn.sigmoid(block_diag_linear(x, w_x, b_x))
    log_a = -RG_C * r * jax.nn.softplus(-lam)
    a = jnp.exp(log_a)
    return a, jnp.sqrt(-jnp.expm1(2.0 * log_a)) * (i * x)


def linear_scan(a, b, h0, reverse):
    def comb(l, r):
        return (l[0] * r[0], r[0] * l[1] + r[1])
    a_cum, b_cum = lax.associative_scan(comb, (a, b), axis=1, reverse=reverse)
    return a_cum * h0[:, None, :] + b_cum


def rglru_bidir(xr_c, xr_l, p):
    B = xr_l.shape[0]
    hs_c, hs_l = [], []
    for d, rev in enumerate((False, True)):
        coeff = functools.partial(rglru_coeffs, w_a=p['rg_wa'][d], b_a=p['rg_ba'][d],
                                  w_x=p['rg_wx'][d], b_x=p['rg_bx'][d], lam=p['rg_lambda'][d])
        a_c, b_c = coeff(xr_c)
        h_c = linear_scan(a_c, b_c, jnp.zeros((B, D_RNN), jnp.float32), rev)
        h_last = h_c[:, 0] if rev else h_c[:, -1]
        a_l, b_l = coeff(xr_l)
        hs_c.append(h_c)
        hs_l.append(linear_scan(a_l, b_l, h_last, rev))
    return hs_c[0] + hs_c[1], hs_l[0] + hs_l[1]


def mla_queries(zq, p, cos, sin):
    B, L, _ = zq.shape
    q = (rmsnorm(zq, p['g_q_lora']) @ p['w_uq']).reshape(B, L, MLA_HEADS, QK_NOPE + QK_ROPE)
    q_nope = rmsnorm(q[..., :QK_NOPE], p['g_q_nope'])
    q_rope = rmsnorm(q[..., QK_NOPE:], p['g_q_rope'])
    if cos is not None:
        q_rope = apply_rope(q_rope, cos[:, None, :], sin[:, None, :])
    return q_nope, q_rope


def mla_keys(zkv, zkr, p, cos, sin):
    B, L, _ = zkv.shape
    ckv = rmsnorm(zkv, p['g_kv_lora'])
    k_nope = rmsnorm((ckv @ p['w_uk']).reshape(B, L, MLA_HEADS, QK_NOPE), p['g_k_nope'])
    v = (ckv @ p['w_uv']).reshape(B, L, MLA_HEADS, V_HEAD)
    k_rope = rmsnorm(zkr, p['g_k_rope'])
    if cos is not None:
        k_rope = apply_rope(k_rope, cos, sin)
    return k_nope, k_rope, v


def attend(q_nope, q_rope, k_nope, k_rope, v):
    s = (jnp.einsum('bqhd,bkhd->bhqk', q_nope, k_nope)
         + jnp.einsum('bqhr,bkr->bhqk', q_rope, k_rope)).astype(jnp.float32) * ATTN_SCALE
    prob = jax.nn.softmax(s, axis=-1).astype(v.dtype)
    return jnp.einsum('bhqk,bkhd->bqhd', prob, v)


def blocked_attention(q_nope, q_rope, k_nope, k_rope, v):
    B, L = q_nope.shape[:2]
    nb = L // Q_BLOCK

    def to_blocks(t):
        return jnp.moveaxis(t.reshape(B, nb, Q_BLOCK, *t.shape[2:]), 1, 0)

    out = lax.map(lambda qs: attend(qs[0], qs[1], k_nope, k_rope, v), (to_blocks(q_nope), to_blocks(q_rope)))
    return jnp.moveaxis(out, 0, 1).reshape(B, L, MLA_HEADS * V_HEAD)


def merge_branches(rnn, attn, zg, p):
    B, L, _ = zg.shape
    g = jax.nn.sigmoid(zg).reshape(B, L, N_BRANCH, D_MODEL)
    merged = g[:, :, 0] * (rnn @ p['w_rnn_out']) + g[:, :, 1] * (attn @ p['w_mla_out'])
    return merged @ p['w_o']


def ec_moe(h, p):
    B, L, _ = h.shape
    cap = CAPACITY_FACTOR * L // N_EXPERTS
    aff = jax.nn.softmax((h @ p['w_router']).astype(jnp.float32), axis=-1)
    gate, idx = lax.top_k(jnp.swapaxes(aff, 1, 2), cap)
    bidx = jnp.arange(B)[:, None, None]
    xg = h[bidx, idx]
    hid = jax.nn.silu(jnp.einsum('becd,edf->becf', xg, p['w_e_gate'])) * jnp.einsum('becd,edf->becf', xg, p['w_e_up'])
    y = jnp.einsum('becf,efd->becd', hid, p['w_e_down']) * gate[..., None].astype(h.dtype)
    return jnp.zeros_like(h).at[bidx, idx].add(y)


def layer(x, ctx, c, c_ctx, p, update_ctx):
    B, S, _ = x.shape
    Lc = ctx.shape[1]
    mod_l = jnp.split((jax.nn.silu(c) @ p['w_ada'] + p['b_ada'])[:, None, :], N_MOD, axis=-1)
    mod_c = jnp.split(jax.nn.silu(c_ctx) @ p['w_ada'] + p['b_ada'], N_MOD, axis=-1)
    cos, sin = axial_rope_tables(S, x.dtype)

    z_l = jnp.split(modulate(rmsnorm(x, p['g_norm1']), mod_l[0], mod_l[1]) @ p['w_in'], SPLIT_POINTS, axis=-1)
    z_c = jnp.split(modulate(rmsnorm(ctx, p['g_norm1']), mod_c[0], mod_c[1]) @ p['w_in'], SPLIT_POINTS, axis=-1)

    xr_l = centred_depthwise_conv(z_l[0], p['conv_w'], p['conv_b']).astype(jnp.float32)
    xr_c = centred_depthwise_conv(z_c[0], p['conv_w'], p['conv_b']).astype(jnp.float32)
    h_c, h_l = rglru_bidir(xr_c, xr_l, p)
    rnn_l = (h_l * jax.nn.gelu(z_l[1].astype(jnp.float32))).astype(x.dtype)

    qn_l, qr_l = mla_queries(z_l[2], p, cos, sin)
    kn_l, kr_l, v_l = mla_keys(z_l[3], z_l[4], p, cos, sin)
    kn_c, kr_c, v_c = mla_keys(z_c[3], z_c[4], p, None, None)
    attn_l = blocked_attention(qn_l, qr_l, jnp.concatenate([kn_c, kn_l], axis=1),
                               jnp.concatenate([kr_c, kr_l], axis=1), jnp.concatenate([v_c, v_l], axis=1))

    x_new = x + mod_l[2] * merge_branches(rnn_l, attn_l, z_l[5], p)
    x_new = x_new + mod_l[5] * ec_moe(modulate(rmsnorm(x_new, p['g_norm2']), mod_l[3], mod_l[4]), p)

    if update_ctx:
        rnn_c = (h_c * jax.nn.gelu(z_c[1].astype(jnp.float32))).astype(ctx.dtype)
        qn_c, qr_c = mla_queries(z_c[2], p, None, None)
        attn_c = attend(qn_c, qr_c, kn_c, kr_c, v_c).reshape(B, Lc, MLA_HEADS * V_HEAD)
        ctx = ctx + mod_c[2] * merge_branches(rnn_c, attn_c, z_c[5], p)
        ctx = ctx + mod_c[5] * ec_moe(modulate(rmsnorm(ctx, p['g_norm2']), mod_c[3], mod_c[4]), p)
    return x_new, ctx


def setup_inputs(seed: int = 0) -> dict:
    key = jax.random.key(seed)
    ks = iter(jax.random.split(key, 40))

    def nrm(shape, fan_in, scale=1.0):
        return scale * fan_in ** -0.5 * jax.random.normal(next(ks), shape, jnp.float32)

    def gain(shape):
        return 1.0 + 0.05 * jax.random.normal(next(ks), shape, jnp.float32)

    def small(shape):
        return 0.02 * jax.random.normal(next(ks), shape, jnp.float32)

    a0 = jax.random.uniform(next(ks), (DEPTH, 2, D_RNN), jnp.float32, minval=0.9, maxval=0.999)
    return {
        "x": jax.random.normal(next(ks), (BATCH, SEQ, D_MODEL), jnp.float32),
        "c": jax.random.normal(next(ks), (BATCH, D_MODEL), jnp.float32),
        "ctx": jax.random.normal(next(ks), (BATCH, CTX_LEN, D_MODEL), jnp.float32),
        "c_ctx": jax.random.normal(next(ks), (D_MODEL,), jnp.float32),
        "w_ada": nrm((DEPTH, D_MODEL, N_MOD * D_MODEL), D_MODEL, 0.5),
        "b_ada": small((DEPTH, N_MOD * D_MODEL)),
        "g_norm1": gain((DEPTH, D_MODEL)),
        "g_norm2": gain((DEPTH, D_MODEL)),
        "w_in": nrm((DEPTH, D_MODEL, D_IN), D_MODEL),
        "conv_w": nrm((DEPTH, CONV_W, D_RNN), CONV_W),
        "conv_b": small((DEPTH, D_RNN)),
        "rg_wa": nrm((DEPTH, 2, RG_BLOCKS, RG_BLOCK, RG_BLOCK), RG_BLOCK),
        "rg_ba": small((DEPTH, 2, D_RNN)),
        "rg_wx": nrm((DEPTH, 2, RG_BLOCKS, RG_BLOCK, RG_BLOCK), RG_BLOCK),
        "rg_bx": small((DEPTH, 2, D_RNN)),
        "rg_lambda": jnp.log(a0) - jnp.log1p(-a0),
        "w_rnn_out": nrm((DEPTH, D_RNN, D_MODEL), D_RNN),
        "g_q_lora": gain((DEPTH, Q_LORA)),
        "w_uq": nrm((DEPTH, Q_LORA, MLA_HEADS * (QK_NOPE + QK_ROPE)), Q_LORA),
        "g_kv_lora": gain((DEPTH, KV_LORA)),
        "w_uk": nrm((DEPTH, KV_LORA, MLA_HEADS * QK_NOPE), KV_LORA),
        "w_uv": nrm((DEPTH, KV_LORA, MLA_HEADS * V_HEAD), KV_LORA),
        "g_q_nope": gain((DEPTH, QK_NOPE)),
        "g_q_rope": gain((DEPTH, QK_ROPE)),
        "g_k_nope": gain((DEPTH, QK_NOPE)),
        "g_k_rope": gain((DEPTH, QK_ROPE)),
        "w_mla_out": nrm((DEPTH, MLA_HEADS * V_HEAD, D_MODEL), MLA_HEADS * V_HEAD),
        "w_o": nrm((DEPTH, D_MODEL, D_MODEL), D_MODEL),
        "w_router": nrm((DEPTH, D_MODEL, N_EXPERTS), D_MODEL),
        "w_e_gate": nrm((DEPTH, N_EXPERTS, D_MODEL, D_EXPERT), D_MODEL),
        "w_e_up": nrm((DEPTH, N_EXPERTS, D_MODEL, D_EXPERT), D_MODEL),
        "w_e_down": nrm((DEPTH, N_EXPERTS, D_EXPERT, D_MODEL), D_EXPERT),
    }


def reference(x, c, ctx, c_ctx, w_ada, b_ada, g_norm1, g_norm2, w_in, conv_w, conv_b, rg_wa, rg_ba, rg_wx, rg_bx,
              rg_lambda, w_rnn_out, g_q_lora, w_uq, g_kv_lora, w_uk, w_uv, g_q_nope, g_q_rope, g_k_nope, g_k_rope,
              w_mla_out, w_o, w_router, w_e_gate, w_e_up, w_e_down):
    for l in range(DEPTH):
        p = dict(w_ada=w_ada[l], b_ada=b_ada[l], g_norm1=g_norm1[l], g_norm2=g_norm2[l], w_in=w_in[l],
                 conv_w=conv_w[l], conv_b=conv_b[l], rg_wa=rg_wa[l], rg_ba=rg_ba[l], rg_wx=rg_wx[l],
                 rg_bx=rg_bx[l], rg_lambda=rg_lambda[l], w_rnn_out=w_rnn_out[l], g_q_lora=g_q_lora[l],
                 w_uq=w_uq[l], g_kv_lora=g_kv_lora[l], w_uk=w_uk[l], w_uv=w_uv[l], g_q_nope=g_q_nope[l],
                 g_q_rope=g_q_rope[l], g_k_nope=g_k_nope[l], g_k_rope=g_k_rope[l], w_mla_out=w_mla_out[l],
                 w_o=w_o[l], w_router=w_router[l], w_e_gate=w_e_gate[l], w_e_up=w_e_up[l], w_e_down=w_e_down[l])
        x, ctx = layer(x, ctx, c, c_ctx, p, l < DEPTH - 1)
    return x
```

```python
import bisect
from contextlib import ExitStack

import numpy as np
import concourse.bass as bass
import concourse.mybir as mybir
from concourse.bass_utils import run_bass_kernel_spmd

F32 = mybir.dt.float32
BF16 = mybir.dt.bfloat16
I32 = mybir.dt.int32
AF = mybir.ActivationFunctionType
ALU = mybir.AluOpType
AX = mybir.AxisListType

ENGS = ("pe", "act", "dve", "pool", "sp")
EPOCH = 30000

L = 4096
LC = 256
T = L + LC
D = 1024
NE = 16
CAP = 512
ROWW = 1058
BIG = float(1 << 20)
EPS = 1e-6


class Sched:
    def __init__(self, nc, es):
        self.nc = nc
        self.es = es
        self.ops = []
        self.lastw = {}
        self.readers = {}
        self.last_eng = {}
        self.dma_ops = {}
        self.setups = {}
        self.emitted = 0
        self.cnt = {e: 0 for e in ENGS}
        self.esems = {e: [] for e in ENGS}
        self.dsems = {}
        self.known = {}
        self.setup_done = {}

    def setup(self, eng, fn):
        self.setups.setdefault(eng, []).append(fn)

    def op(self, eng, fn, reads=(), writes=(), dma=None):
        i = len(self.ops)
        deps = {}
        for r in reads:
            w = self.lastw.get(r)
            if w is not None:
                deps[w] = True
        for r in writes:
            w = self.lastw.get(r)
            if w is not None:
                deps.setdefault(w, False)
            for j in self.readers.get(r, {}).values():
                deps.setdefault(j, False)
        self.ops.append(dict(eng=eng, fn=fn, deps=deps, dma=dma, signal=False))
        slot = ("d", dma) if dma is not None else ("e", eng)
        for r in reads:
            self.readers.setdefault(r, {})[slot] = i
        for r in writes:
            self.lastw[r] = i
            self.readers[r] = {}
        if dma is not None:
            self.dma_ops.setdefault(dma, []).append(i)
        else:
            self.last_eng[eng] = i
        return i

    def barrier(self):
        deps = {}
        for e, i in self.last_eng.items():
            deps[i] = True
        for k, lst in self.dma_ops.items():
            deps[lst[-1]] = True
        for e in ENGS:
            self.ops.append(dict(eng=e, fn=None, deps=dict(deps), dma=None, signal=False, bar=True))
        self.lastw = {}
        self.readers = {}

    def flush(self):
        self.barrier()
        nc = self.nc
        ops = self.ops
        lo = self.emitted
        for o in ops[lo:]:
            for d in o["deps"]:
                ops[d]["signal"] = True
        for o in ops[lo:]:
            if o["fn"] is not None and o["dma"] is None and o["signal"]:
                self.cnt[o["eng"]] += 1
                o["val"] = self.cnt[o["eng"]]
        for k in self.dma_ops:
            if k not in self.dsems:
                self.dsems[k] = self.es.enter_context(nc.semaphore(f"d_{k}"))
        esems, dsems = self.esems, self.dsems

        def esem(e, v):
            k = (v - 1) // EPOCH
            while len(esems[e]) <= k:
                esems[e].append(self.es.enter_context(nc.semaphore(f"s_{e}{len(esems[e])}")))
            return esems[e][k], (v - 1) % EPOCH + 1

        def events(j, o):
            evs = []
            for d, raw in o["deps"].items():
                p = ops[d]
                if p["fn"] is None:
                    continue
                if p["dma"] is not None:
                    lst = self.dma_ops[p["dma"]]
                    n = bisect.bisect_left(lst, j)
                    evs.append((dsems[p["dma"]], 16 * n))
                else:
                    if p["eng"] == o["eng"]:
                        if o["eng"] == "pe":
                            continue
                    evs.append(esem(p["eng"], p["val"]))
            return evs

        for o in ops[lo:]:
            if "val" in o:
                esem(o["eng"], o["val"])

        def make(ename):
            def body(eng):
                known = self.known.setdefault(ename, {})
                if not self.setup_done.get(ename):
                    self.setup_done[ename] = True
                    for f in self.setups.get(ename, []):
                        f(eng)
                for j in range(lo, len(ops)):
                    o = ops[j]
                    if o["eng"] != ename:
                        continue
                    need = {}
                    for sem, v in events(j, o):
                        key = id(sem)
                        if known.get(key, 0) >= v:
                            continue
                        if key not in need or need[key][1] < v:
                            need[key] = (sem, v)
                    for key, (sem, v) in need.items():
                        eng.wait_ge(sem, v)
                        known[key] = v
                    if o["fn"] is None:
                        continue
                    ins = o["fn"](eng)
                    if o["dma"] is not None:
                        ins.then_inc(dsems[o["dma"]], 16)
                    elif o["signal"]:
                        sem, _ = esem(ename, o["val"])
                        ins.then_inc(sem, 1)
            return body

        with nc.Block() as block:
            block.sync(make("sp"))
            block.scalar(make("act"))
            block.vector(make("dve"))
            block.gpsimd(make("pool"))
            block.tensor(make("pe"))
        self.emitted = len(ops)


def blk(b):
    if b == 0:
        return 0, LC
    return LC + (b - 1) * 512, LC + b * 512


def build_program(limit=99, debug=False):
    nc = bass.Bass("TRN2", target_bir_lowering=False)

    def din(name, shape, dt=F32):
        return nc.dram_tensor(name, list(shape), dt, kind="ExternalInput").ap()

    def dscr(name, shape, dt):
        return nc.dram_tensor(name, list(shape), dt, kind="ExternalOutput" if debug else "Internal").ap()

    x_d = din("x", [L, D])
    ctx_d = din("ctx", [LC, D])
    c2T_d = din("c2T", [128, 8, 2])
    wada_d = din("w_ada", [D, 6 * D])
    bada_d = din("b_ada2", [2, 6 * D])
    g1T_d = din("g1T", [128, 8])
    g2row_d = din("g2row", [2, D])
    win_d = din("w_in", [D, 4512])
    cwT_d = din("cwT", [128, 8, 4])
    cbT_d = din("cbT", [128, 8])
    rgwa_d = din("rg_wa", [2, 8, 128, 128])
    rgwx_d = din("rg_wx", [2, 8, 128, 128])
    rgbT_d = din("rgbT", [128, 3, 2, 8])
    wrnn_d = din("w_rnn_out", [D, D])
    wmla_d = din("w_mla_out", [512, D])
    wo_d = din("w_o", [D, D])
    wuq_d = din("w_uq", [256, 768])
    gql256_d = din("gql256", [128, 256])
    wuk_d = din("w_uk", [128, 512])
    wuv_d = din("w_uv", [128, 512])
    gkv128_d = din("gkv128", [128, 128])
    gq96_d = din("gq96", [128, 96])
    gk96_d = din("gk96", [128, 96])
    cos_d = din("cosT", [128, 32, 16])
    sin_d = din("sinT", [128, 32, 16])
    wr_d = din("w_router", [D, NE])
    weg_d = din("w_e_gate", [NE, D, D])
    weu_d = din("w_e_up", [NE, D, D])
    wed_d = din("w_e_down", [NE, D, D])
    out_d = nc.dram_tensor("out", [L, D], F32, kind="ExternalOutput").ap()

    bc_d = dscr("bc_d", [128, 4096], F32)
    rnnT_d = dscr("rnnT_d", [8, 128, L], BF16)
    z0_d = nc.dram_tensor("z0_d", [8, 128, T], F32, kind="Internal").ap()
    w2_d = nc.dram_tensor("w2_d", [8, 128, L], F32, kind="Internal").ap()
    G_d = dscr("G_d", [16, 128, L], BF16)
    QT_d = dscr("QT_d", [8, 96, L], BF16)
    KT_d = dscr("KT_d", [8, 96, T], BF16)
    V_d = dscr("V_d", [T, 520], BF16)
    rows_d = dscr("rows_d", [L, ROWW], BF16)
    xg_d = dscr("xg_d", [NE * CAP, ROWW], BF16)
    if debug:
        hT_dbg = dscr("hT_dbg", [128, 8, T], BF16)
        modT_dbg = dscr("modT_dbg", [128, 48, 2], F32)
        aff_dbg = dscr("aff_dbg", [128, 32, 16], F32)
        idx_dbg = dscr("idx_dbg", [128, 32, 16], I32)
        attn_dbg = dscr("attn_dbg", [128, 32, 512], BF16)

    win_v = win_d.rearrange("(k p) n -> p k n", p=128)

    with ExitStack() as es:
        S = Sched(nc, es)
        R = {}
        S.setup("pool", lambda e: R.__setitem__("bc_xg", e.to_reg(NE * CAP - 1)))
        S.setup("pool", lambda e: R.__setitem__("bc_out", e.to_reg(L - 1)))

        def sb(stack, name, shape, dt):
            return stack.enter_context(nc.sbuf_tensor(name, list(shape), dt))

        def ps(stack, name, shape, dt):
            return stack.enter_context(nc.psum_tensor(name, list(shape), dt))

        def dma(eng, out, in_, reads, writes, key):
            S.op(eng, lambda e: e.dma_start(out=out, in_=in_), reads, writes, dma=key)

        def act(out, in_, func, reads, writes, **kw):
            S.op("act", lambda e: e.activation(out=out, in_=in_, func=func, **kw), reads, writes)

        def ts(eng, out, in0, s1, s2, op0, op1, reads, writes):
            S.op(eng, lambda e: e.tensor_scalar(out, in0, s1, s2, op0, op1), reads, writes)

        def tt(eng, out, in0, in1, op, reads, writes):
            S.op(eng, lambda e: e.tensor_tensor(out, in0, in1, op), reads, writes)

        def stt(out, in0, scalar, in1, op0, op1, reads, writes):
            S.op("dve", lambda e: e.scalar_tensor_tensor(out, in0, scalar, in1, op0, op1), reads, writes)

        def cp(eng, out, in_, reads, writes):
            if eng == "act":
                S.op("act", lambda e: e.copy(out, in_), reads, writes)
            else:
                S.op(eng, lambda e: e.tensor_copy(out, in_), reads, writes)

        def recip(out, in_, reads, writes):
            S.op("dve", lambda e: e.reciprocal(out, in_), reads, writes)

        def red(out, in_, reads, writes, op=ALU.add):
            S.op("dve", lambda e: e.tensor_reduce(out, in_, AX.X, op), reads, writes)

        def mset(eng, ap, val, writes):
            S.op(eng, lambda e: e.memset(ap, val), (), writes)

        def mmg(out, pairs, reads, writes):
            def fn(e):
                n = len(pairs)
                ins = None
                for i, (l, r) in enumerate(pairs):
                    ins = e.matmul(out, lhsT=l, rhs=r, start=(i == 0), stop=(i == n - 1))
                return ins
            S.op("pe", fn, reads, writes)

        def trs(items, reads, writes):
            def fn(e):
                ins = None
                for o, i, idn in items:
                    ins = e.transpose(o, i, idn)
                return ins
            S.op("pe", fn, reads, writes)

        ident_f = sb(es, "ident_f", [128, 128], F32)
        ident_b = sb(es, "ident_b", [128, 128], BF16)
        modT = sb(es, "modT", [128, 48, 2], F32)
        A1T = sb(es, "A1T", [128, 8, 2], F32)
        HBA = sb(es, "HBA", [128, 2, 8], F32)
        HBX = sb(es, "HBX", [128, 2, 8], F32)
        CN = sb(es, "CN", [128, 2, 8], F32)
        HC = sb(es, "HC", [128, 2, 8], F32)
        CWH = sb(es, "CWH", [128, 8, 4], F32)
        CBH = sb(es, "CBH", [128, 8], F32)
        AFF = sb(es, "AFF", [128, 32, 16], F32)
        TOKID = sb(es, "TOKID", [128, 32], I32)
        IDX = sb(es, "IDX", [128, 32, NE], I32)

        mset("pool", ident_f[:], 0.0, ["ident_f"])
        S.op("pool", lambda e: e.affine_select(out=ident_f[:], in_=ident_f[:], pattern=[[-1, 128]],
                                                compare_op=ALU.not_equal, fill=1.0, base=0, channel_multiplier=1),
             ["ident_f"], ["ident_f"])
        cp("pool", ident_b[:], ident_f[:], ["ident_f"], ["ident_b"])

        with ExitStack() as ph:
            c2 = sb(ph, "c2", [128, 8, 2], F32)
            sc = sb(ph, "sc", [128, 8, 2], F32)
            wa = [sb(ph, f"wa{i}", [128, 8, 512], F32) for i in range(2)]
            modrow = sb(ph, "modrow", [2, 6 * D], F32)
            brow = sb(ph, "brow", [2, 6 * D], F32)
            g2r = sb(ph, "g2r", [2, D], F32)
            sel = sb(ph, "sel", [2, 128], F32)
            g1 = sb(ph, "g1", [128, 8], F32)
            rgb = sb(ph, "rgb", [128, 3, 2, 8], F32)
            spl = sb(ph, "spl", [128, 2, 8], F32)
            cw = sb(ph, "cw", [128, 8, 4], F32)
            cb = sb(ph, "cb", [128, 8], F32)
            bcs = [sb(ph, f"bcs{i}", [128, 512], F32) for i in range(2)]
            pm = [ps(ph, f"pm{i}", [2, 512], F32) for i in range(2)]
            pT0 = ps(ph, "pT0", [128, 48, 2], F32)
            pbc = [ps(ph, f"pbc{i}", [128, 512], F32) for i in range(2)]

            dma("sp", c2[:], c2T_d, [], ["c2"], "ld0")
            dma("sp", brow[:], bada_d, [], ["brow"], "ld0")
            dma("sp", g2r[:], g2row_d, [], ["g2r"], "ld0")
            dma("sp", g1[:], g1T_d, [], ["g1"], "ld0")
            dma("sp", rgb[:], rgbT_d, [], ["rgb"], "ld0")
            dma("sp", cw[:], cwT_d, [], ["cw"], "ld0")
            dma("sp", cb[:], cbT_d, [], ["cb"], "ld0")
            act(sc[:], c2[:], AF.Silu, ["c2"], ["sc"])
            wada_v = wada_d.rearrange("(k p) n -> p k n", p=128)
            for j in range(12):
                s = j % 2
                dma("sp", wa[s][:], wada_v[:, :, j * 512:(j + 1) * 512], [], [("wa", s)], f"wa{s}")
                mmg(pm[s][:], [(sc[:, k, :], wa[s][:, k, :]) for k in range(8)], ["sc", ("wa", s)], [("pm", s)])
                tt("dve", modrow[:, j * 512:(j + 1) * 512], pm[s][:], brow[:, j * 512:(j + 1) * 512], ALU.add,
                   [("pm", s), "brow"], ["modrow"])
            trs([(pT0[:, c, :], modrow[:, c * 128:(c + 1) * 128], ident_f[0:2, 0:2]) for c in range(48)],
                ["modrow", "ident_f"], ["pT0"])
            cp("dve", modT[:], pT0[:], ["pT0"], ["modT"])
            for r in range(2):
                stt(A1T[:, :, r], modT[:, 8:16, r], 1.0, g1[:], ALU.add, ALU.mult, ["modT", "g1"], ["A1T"])
            stt(modrow[:, 4096:5120], modrow[:, 4096:5120], 1.0, g2r[:], ALU.add, ALU.mult, ["modrow", "g2r"], ["modrow"])
            mset("pool", sel[:], 0.0, ["sel"])
            mset("pool", sel[0:1, :], 1.0, ["sel"])
            for q in range(8):
                s = q % 2
                mmg(pbc[s][:], [(sel[:], modrow[:, 2048 + q * 512:2048 + (q + 1) * 512])], ["sel", "modrow"], [("pbc", s)])
                cp("act", bcs[s][:], pbc[s][:], [("pbc", s)], [("bcs", s)])
                dma("sp", bc_d[:, q * 512:(q + 1) * 512], bcs[s][:], [("bcs", s)], [], f"bcst{s}")
            ts("pool", HBA[:], rgb[:, 0], 0.5, 0.0, ALU.mult, ALU.add, ["rgb"], ["HBA"])
            ts("pool", HBX[:], rgb[:, 1], 0.5, 0.0, ALU.mult, ALU.add, ["rgb"], ["HBX"])
            act(spl[:], rgb[:, 2], AF.Exp, ["rgb"], ["spl"], scale=-1.0)
            act(spl[:], spl[:], AF.Ln, ["spl"], ["spl"], bias=1.0)
            ts("pool", CN[:], spl[:], -8.0, 0.0, ALU.mult, ALU.add, ["spl"], ["CN"])
            ts("pool", HC[:], spl[:], -4.0, 0.0, ALU.mult, ALU.add, ["spl"], ["HC"])
            ts("pool", CWH[:], cw[:], 0.5, 0.0, ALU.mult, ALU.add, ["cw"], ["CWH"])
            ts("pool", CBH[:], cb[:], 0.5, 0.0, ALU.mult, ALU.add, ["cb"], ["CBH"])
            if debug:
                dma("sp", modT_dbg, modT[:], ["modT"], [], "dbg")
            S.flush()
        if limit <= 0:
            return nc

        with ExitStack() as midstack:
            hT = sb(midstack, "hT", [128, 8, T], BF16)

            with ExitStack() as ph:
                xt = [sb(ph, f"xt{i}", [128, 4, D], F32) for i in range(2)]
                junk = sb(ph, "junk", [128, D], BF16)
                ssA = [sb(ph, f"ssA{i}", [128, 4], F32) for i in range(2)]
                rsA = [sb(ph, f"rsA{i}", [128, 4], F32) for i in range(2)]
                pTa = [ps(ph, f"pTa{i}", [128, 512], F32) for i in range(4)]
                xv = x_d.rearrange("(g c p) d -> g p c d", c=4, p=128)
                cv = ctx_d.rearrange("(c p) d -> p c d", p=128)
                for g in range(9):
                    nC = 2 if g == 0 else 4
                    s = g % 2
                    r = 1 if g == 0 else 0
                    t0 = blk(g)[0]
                    src = cv if g == 0 else xv[g - 1]
                    dma("sp", xt[s][:, 0:nC, :], src, [], [("xt", s)] + [("xt", s, c) for c in range(4)], f"xt{s}")
                    mset("pool", ssA[s][:], 1.0, [("ssA", s)])
                    for c in range(nC):
                        act(junk[:], xt[s][:, c, :], AF.Square, [("xt", s)], ["junk", ("ssA", s)], accum_out=ssA[s][:, c:c + 1])
                    ts("dve", rsA[s][:], ssA[s][:], 1.0 / D, EPS, ALU.mult, ALU.add, [("ssA", s)], [("rsA", s)])
                    act(rsA[s][:], rsA[s][:], AF.Sqrt, [("rsA", s)], [("rsA", s)])
                    recip(rsA[s][:], rsA[s][:], [("rsA", s)], [("rsA", s)])
                    for c in range(nC):
                        eng = "dve" if c % 2 == 0 else "pool"
                        ts(eng, xt[s][:, c, :], xt[s][:, c, :], rsA[s][:, c:c + 1], 0.0, ALU.mult, ALU.add,
                           [("xt", s), ("rsA", s)], [("xt", s, c)])
                    for k in range(8):
                        bank = pTa[k % 4]
                        trs([(bank[:, c * 128:(c + 1) * 128], xt[s][:, c, k * 128:(k + 1) * 128], ident_f[:]) for c in range(nC)],
                            [("xt", s, c) for c in range(nC)] + ["ident_f"], [("pTa", k % 4)])
                        o = hT[:, k, t0:t0 + nC * 128]
                        i_ = bank[:, 0:nC * 128]
                        if k % 2 == 0:
                            ts("dve", o, i_, A1T[:, k, r:r + 1], modT[:, k, r:r + 1], ALU.mult, ALU.add,
                               [("pTa", k % 4), "A1T", "modT"], [("hT", g, k)])
                        else:
                            act(o, i_, AF.Identity, [("pTa", k % 4), "A1T", "modT"], [("hT", g, k)],
                                scale=A1T[:, k, r:r + 1], bias=modT[:, k, r:r + 1])
                if debug:
                    dma("sp", hT_dbg, hT[:], [("hT", g, k) for g in range(9) for k in range(8)], [], "dbg")
                S.flush()
            if limit <= 1:
                return nc
            HT_ALL = []

            with ExitStack() as ph:
                Z0S = [sb(ph, f"Z0S{i}", [128, 4360], F32) for i in range(2)]
                XRS = [sb(ph, f"XRS{i}", [128, T], F32) for i in range(2)]
                X1 = sb(ph, "X1", [128, L], F32)
                X2 = sb(ph, "X2", [128, L], F32)
                wz = [sb(ph, f"wz{i}", [128, 8, 256], BF16) for i in range(2)]
                pz = [ps(ph, f"pz{i}", [128, 512], F32) for i in range(4)]
                NB = 9

                def load_wz(n):
                    s = n % 2
                    dma("pool", wz[s][:, :, 0:128], win_v[:, :, n * 128:(n + 1) * 128], [], [("wz", s, 0)], f"wz{s}")
                    dma("pool", wz[s][:, :, 128:256], win_v[:, :, 1024 + n * 128:1024 + (n + 1) * 128], [], [("wz", s, 1)], f"wz{s}")

                for i in range(2):
                    mset("pool", Z0S[i][:], 0.0, [("Z0S", i, b) for b in range(NB)])
                load_wz(0)
                pc = 0
                for n in range(8):
                    s = n % 2
                    if n + 1 < 8:
                        load_wz(n + 1)
                    for b in range(NB):
                        lo, hi = blk(b)
                        w = hi - lo
                        zc = lo + 2 if b == 0 else lo + 6
                        q = pc % 4
                        pc += 1
                        mmg(pz[q][:, 0:w], [(wz[s][:, k, 0:128], hT[:, k, lo:hi]) for k in range(8)], [("wz", s, 0)], [("pz", q)])
                        cp("act", Z0S[s][:, zc:zc + w], pz[q][:, 0:w], [("pz", q)], [("Z0S", s, b)])
                    zk = [("Z0S", s, b) for b in range(NB)]
                    conv_late = []
                    for (o0, o1, zb) in ((0, LC, 0), (LC, T, 260)):
                        w = o1 - o0
                        ts("pool", XRS[s][:, o0:o1], Z0S[s][:, zb:zb + w], CWH[:, n, 0:1], CBH[:, n:n + 1], ALU.mult, ALU.add,
                           zk, [("XRS", s, o0)])
                        for k in range(1, 4):
                            args = (XRS[s][:, o0:o1], Z0S[s][:, zb + k:zb + k + w], CWH[:, n, k:k + 1], XRS[s][:, o0:o1], ALU.mult, ALU.add,
                                    zk + [("XRS", s, o0)], [("XRS", s, o0)])
                            if o0 == 0:
                                stt(*args)
                            else:
                                conv_late.append(args)
                    def tanh_stage(n_, sg):
                        sl = slice(sg * 1024, (sg + 1) * 1024)
                        act(X2[:, sl], X2[:, sl], AF.Tanh, [("X2", sg)], [("X2", sg)], scale=0.7978845608028654)
                        stt(X2[:, sl], X2[:, sl], 1.0, X1[:, sl], ALU.add, ALU.mult, [("X2", sg), ("X1", sg)], [("X2", sg)])
                        dma("sp", w2_d[n_, :, sg * 1024:(sg + 1) * 1024], X2[:, sl], [("X2", sg)], [], f"w2st{sg % 2}")

                    if n >= 1:
                        tanh_stage(n - 1, 3)
                    for sg in range(4):
                        bs = (1 + 2 * sg, 2 + 2 * sg)
                        l0 = sg * 1024
                        for b in bs:
                            blo, bhi = blk(b)
                            q = pc % 4
                            pc += 1
                            mmg(pz[q][:], [(wz[s][:, k, 128:256], hT[:, k, blo:bhi]) for k in range(8)], [("wz", s, 1)], [("pz", q)])
                            cp("act", X1[:, blo - LC:bhi - LC], pz[q][:], [("pz", q)], [("X1", sg)])
                            act(X2[:, blo - LC:bhi - LC], pz[q][:], AF.Square, [("pz", q)], [("X2", sg)], scale=0.044715 ** 0.5)
                        sl = slice(l0, l0 + 1024)
                        stt(X2[:, sl], X2[:, sl], 1.0, X1[:, sl], ALU.add, ALU.mult, [("X2", sg), ("X1", sg)], [("X2", sg)])
                        if sg >= 1:
                            tanh_stage(n, sg - 1)
                        if sg < 3:
                            stt(*conv_late[sg])
                    dma("sp", z0_d[n], XRS[s][:], [("XRS", s, 0), ("XRS", s, LC)], [], f"z0st{s}")
                tanh_stage(7, 3)
                S.flush()
            if limit <= 2:
                return nc

            with ExitStack() as ph:
                wg = [sb(ph, f"wg{i}", [128, 8, 128], BF16) for i in range(2)]
                GB = [sb(ph, f"GB{i}", [128, L], BF16) for i in range(2)]
                pg = [ps(ph, f"pg{i}", [128, 512], F32) for i in range(4)]
                for j in range(16):
                    s = j % 2
                    dma("pool", wg[s][:], win_v[:, :, 2464 + j * 128:2464 + (j + 1) * 128], [], [("wg", s)], f"wg{s}")
                    for b in range(1, 9):
                        lo, hi = blk(b)
                        mmg(pg[b % 4][:], [(wg[s][:, k, :], hT[:, k, lo:hi]) for k in range(8)], [("wg", s)], [("pg", b % 4)])
                        act(GB[s][:, lo - LC:hi - LC], pg[b % 4][:], AF.Sigmoid, [("pg", b % 4)], [("GB", s)])
                    dma("sp", G_d[j], GB[s][:], [("GB", s)], [], f"gst{s}")
                S.flush()
            if limit <= 3:
                return nc

            with ExitStack() as ph:
                wqkv = sb(ph, "wqkv", [128, 8, 416], BF16)
                wuq = sb(ph, "wuq", [128, 2, 768], BF16)
                wukv = sb(ph, "wukv", [128, 1024], BF16)
                GQL = sb(ph, "GQL", [128, 256], F32)
                GKV = sb(ph, "GKV", [128, 128], F32)
                GQ = sb(ph, "GQ", [128, 96], F32)
                GK = sb(ph, "GK", [128, 96], F32)
                COS = sb(ph, "COS", [128, 32, 16], F32)
                SIN = sb(ph, "SIN", [128, 32, 16], F32)
                INV3 = sb(ph, "INV3", [128, 4, 3], F32)
                INV24 = sb(ph, "INV24", [128, 4, 24], F32)
                Z = sb(ph, "Z", [128, 4, 416], F32)
                SQ = sb(ph, "SQ", [128, 4, 768], F32)
                ss3 = sb(ph, "ss3", [128, 4, 3], F32)
                r3 = sb(ph, "r3", [128, 4, 3], F32)
                ss24 = sb(ph, "ss24", [128, 4, 24], F32)
                r24 = sb(ph, "r24", [128, 4, 24], F32)
                ZQS = sb(ph, "ZQS", [128, 4, 256], F32)
                CKS = sb(ph, "CKS", [128, 4, 128], F32)
                KR = sb(ph, "KR", [128, 4, 32], F32)
                KR2 = sb(ph, "KR2", [128, 4, 32], F32)
                ZT = sb(ph, "ZT", [128, 4, 3, 128], BF16)
                QF = sb(ph, "QF", [128, 4, 768], F32)
                KF = sb(ph, "KF", [128, 4, 512], F32)
                T1 = sb(ph, "T1", [128, 4, 8, 16], F32)
                T2 = sb(ph, "T2", [128, 4, 8, 16], F32)
                T3 = sb(ph, "T3", [128, 4, 16], F32)
                T4 = sb(ph, "T4", [128, 4, 16], F32)
                QTM = sb(ph, "QTM", [128, 4, 8, 96], BF16)
                KTM = sb(ph, "KTM", [128, 4, 8, 96], BF16)
                VTM = sb(ph, "VTM", [128, 4, 8, 65], BF16)
                QST = sb(ph, "QST", [96, 8, 512], BF16)
                KST = sb(ph, "KST", [96, 8, 512], BF16)
                PQ = ps(ph, "PQ", [128, 1024], F32)
                PZ = [ps(ph, "PZ0", [128, 512], F32)] * 2
                PTZ = ps(ph, "PTZ", [128, 3, 128], F32)
                PK = ps(ph, "PK", [128, 512], F32)
                PV = ps(ph, "PV", [128, 512], F32)
                PTQ = ps(ph, "PTQ", [96, 8, 128], BF16)
                PTK = ps(ph, "PTK", [96, 8, 128], BF16)

                dma("pool", wqkv[:], win_v[:, :, 2048:2464], [], ["wqkv"], "ldc")
                dma("pool", wuq[:], wuq_d.rearrange("(k p) n -> p k n", p=128), [], ["wuq"], "ldc")
                dma("pool", wukv[:, 0:512], wuk_d, [], ["wukv"], "ldc")
                dma("pool", wukv[:, 512:1024], wuv_d, [], ["wukv2"], "ldc")
                dma("sp", GQL[:], gql256_d, [], ["GQL"], "ldc2")
                dma("sp", GKV[:], gkv128_d, [], ["GKV"], "ldc2")
                dma("sp", GQ[:], gq96_d, [], ["GQ"], "ldc2")
                dma("sp", GK[:], gk96_d, [], ["GK"], "ldc2")
                dma("sp", COS[:], cos_d, [], ["COS"], "ldc2")
                dma("sp", SIN[:], sin_d, [], ["SIN"], "ldc2")
                ts("dve", GQ[:], GQ[:], 96.0 ** -0.5, 0.0, ALU.mult, ALU.add, ["GQ"], ["GQ"])
                for j, v in enumerate((1.0 / 256, 1.0 / 128, 1.0 / 32)):
                    mset("pool", INV3[:, :, j:j + 1], v, ["INV3"])
                mset("pool", INV24[:, :, 0:8], 1.0 / 64, ["INV24"])
                mset("pool", INV24[:, :, 8:16], 1.0 / 32, ["INV24"])
                mset("pool", INV24[:, :, 16:24], 1.0 / 64, ["INV24"])
                mset("pool", ss3[:], 1.0, ["ss3"])
                mset("pool", ss24[:], 1.0, ["ss24"])
                mset("pool", VTM[:], 1.0, ["VTM"])
                QT_v = QT_d.rearrange("h d t -> d h t")
                KT_v = KT_d.rearrange("h d t -> d h t")
                V_v = V_d.rearrange("(c p) f -> p c f", p=128)

                for g in range(9):
                    nC = 2 if g == 0 else 4
                    lat = g > 0
                    t0 = blk(g)[0]
                    c0 = 0 if lat else 256
                    cg = (g - 1) * 4
                    for c in range(nC):
                        p = PZ[c % 2]
                        mmg(p[:, c0:416], [(hT[:, k, t0 + c * 128:t0 + (c + 1) * 128], wqkv[:, k, c0:416]) for k in range(8)],
                            ["wqkv"], [("PZ", 0)])
                        cp("act", Z[:, c, c0:416], p[:, c0:416], [("PZ", 0)], ["Z"])
                    act(SQ[:, 0:nC, c0:416], Z[:, 0:nC, c0:416], AF.Square, ["Z"], ["SQ"])
                    if lat:
                        red(ss3[:, 0:nC, 0], SQ[:, 0:nC, 0:256], ["SQ"], ["ss3"])
                    red(ss3[:, 0:nC, 1], SQ[:, 0:nC, 256:384], ["SQ"], ["ss3"])
                    red(ss3[:, 0:nC, 2], SQ[:, 0:nC, 384:416], ["SQ"], ["ss3"])
                    tt("dve", r3[:], ss3[:], INV3[:], ALU.mult, ["ss3", "INV3"], ["r3"])
                    act(r3[:], r3[:], AF.Sqrt, ["r3"], ["r3"], bias=EPS)
                    recip(r3[:], r3[:], ["r3"], ["r3"])
                    for c in range(nC):
                        if lat:
                            stt(ZQS[:, c, :], Z[:, c, 0:256], r3[:, c, 0:1], GQL[:], ALU.mult, ALU.mult, ["Z", "r3", "GQL"], ["ZQS"])
                        stt(CKS[:, c, :], Z[:, c, 256:384], r3[:, c, 1:2], GKV[:], ALU.mult, ALU.mult, ["Z", "r3", "GKV"], ["CKS"])
                        stt(KR[:, c, :], Z[:, c, 384:416], r3[:, c, 2:3], GK[:, 64:96], ALU.mult, ALU.mult, ["Z", "r3", "GK"], ["KR"])
                    for c in range(nC):
                        items = [(PTZ[:, 2, :], CKS[:, c, :], ident_f[:])]
                        if lat:
                            items += [(PTZ[:, 0, :], ZQS[:, c, 0:128], ident_f[:]), (PTZ[:, 1, :], ZQS[:, c, 128:256], ident_f[:])]
                        trs(items, ["CKS", "ZQS", "ident_f"], ["PTZ"])
                        if lat:
                            cp("act", ZT[:, c, 0:2, :], PTZ[:, 0:2, :], ["PTZ"], [("ZT", c)])
                        cp("dve", ZT[:, c, 2, :], PTZ[:, 2, :], ["PTZ"], [("ZT", c)])
                        if lat:
                            mmg(PQ[:, 0:512], [(ZT[:, c, k, :], wuq[:, k, 0:512]) for k in range(2)], [("ZT", c), "wuq"], ["PQ"])
                            mmg(PQ[:, 512:768], [(ZT[:, c, k, :], wuq[:, k, 512:768]) for k in range(2)], [("ZT", c), "wuq"], ["PQ2"])
                            cp("act", QF[:, c, :], PQ[:, 0:768], ["PQ", "PQ2"], ["QF"])
                        mmg(PK[:], [(ZT[:, c, 2, :], wukv[:, 0:512])], [("ZT", c), "wukv"], ["PK"])
                        mmg(PV[:], [(ZT[:, c, 2, :], wukv[:, 512:1024])], [("ZT", c), "wukv2"], ["PV"])
                        cp("dve", KF[:, c, :], PK[:], ["PK"], ["KF"])
                        cp("act", VTM[:, c, :, 0:64], PV[:].rearrange("p (h d) -> p h d", d=64), ["PV"], ["VTM"])
                    QF4 = QF[:, 0:nC, :].rearrange("p c (h d) -> p c h d", d=96)
                    SQ4 = SQ[:, 0:nC, :].rearrange("p c (h d) -> p c h d", d=96)
                    KF4 = KF[:, 0:nC, :].rearrange("p c (h d) -> p c h d", d=64)
                    SQK4 = SQ[:, 0:nC, 0:512].rearrange("p c (h d) -> p c h d", d=64)
                    if lat:
                        act(SQ[:, 0:nC, :], QF[:, 0:nC, :], AF.Square, ["QF"], ["SQ"])
                        red(ss24[:, 0:nC, 0:8], SQ4[:, :, :, 0:64], ["SQ"], ["ss24"])
                        red(ss24[:, 0:nC, 8:16], SQ4[:, :, :, 64:96], ["SQ"], ["ss24"])
                    act(SQ[:, 0:nC, 0:512], KF[:, 0:nC, :], AF.Square, ["KF"], ["SQ"])
                    red(ss24[:, 0:nC, 16:24], SQK4, ["SQ"], ["ss24"])
                    tt("dve", r24[:], ss24[:], INV24[:], ALU.mult, ["ss24", "INV24"], ["r24"])
                    act(r24[:], r24[:], AF.Sqrt, ["r24"], ["r24"], bias=EPS)
                    recip(r24[:], r24[:], ["r24"], ["r24"])
                    QTM4 = QTM[:, 0:nC]
                    KTM4 = KTM[:, 0:nC]
                    if lat:
                        tt("dve", QF4[:, :, :, 0:64], QF4[:, :, :, 0:64], r24[:, 0:nC, 0:8].unsqueeze(3).to_broadcast([128, nC, 8, 64]), ALU.mult, ["QF", "r24"], ["QF"])
                        tt("dve", QF4[:, :, :, 64:96], QF4[:, :, :, 64:96], r24[:, 0:nC, 8:16].unsqueeze(3).to_broadcast([128, nC, 8, 32]), ALU.mult, ["QF", "r24"], ["QF"])
                        tt("dve", QTM4[:, :, :, 0:64], QF4[:, :, :, 0:64], GQ[:, 0:64].unsqueeze(1).unsqueeze(1).to_broadcast([128, nC, 8, 64]), ALU.mult, ["QF", "GQ"], ["QTM"])
                        tt("dve", QF4[:, :, :, 64:96], QF4[:, :, :, 64:96], GQ[:, 64:96].unsqueeze(1).unsqueeze(1).to_broadcast([128, nC, 8, 32]), ALU.mult, ["QF", "GQ"], ["QF"])
                    tt("dve", KF4, KF4, r24[:, 0:nC, 16:24].unsqueeze(3).to_broadcast([128, nC, 8, 64]), ALU.mult, ["KF", "r24"], ["KF"])
                    tt("dve", KTM4[:, :, :, 0:64], KF4, GK[:, 0:64].unsqueeze(1).unsqueeze(1).to_broadcast([128, nC, 8, 64]), ALU.mult, ["KF", "GK"], ["KTM"])
                    if lat:
                        Cq = COS[:, cg:cg + nC, :].unsqueeze(2).to_broadcast([128, nC, 8, 16])
                        Sq = SIN[:, cg:cg + nC, :].unsqueeze(2).to_broadcast([128, nC, 8, 16])
                        x1, x2 = QF4[:, :, :, 64:80], QF4[:, :, :, 80:96]
                        tt("dve", T1[:, 0:nC], x1, Cq, ALU.mult, ["QF", "COS"], ["T1"])
                        tt("dve", T2[:, 0:nC], x2, Sq, ALU.mult, ["QF", "SIN"], ["T2"])
                        tt("dve", QTM4[:, :, :, 64:80], T1[:, 0:nC], T2[:, 0:nC], ALU.subtract, ["T1", "T2"], ["QTM"])
                        tt("dve", T1[:, 0:nC], x1, Sq, ALU.mult, ["QF", "SIN"], ["T1"])
                        tt("dve", T2[:, 0:nC], x2, Cq, ALU.mult, ["QF", "COS"], ["T2"])
                        tt("dve", QTM4[:, :, :, 80:96], T1[:, 0:nC], T2[:, 0:nC], ALU.add, ["T1", "T2"], ["QTM"])
                        Ck = COS[:, cg:cg + nC, :]
                        Sk = SIN[:, cg:cg + nC, :]
                        k1, k2 = KR[:, 0:nC, 0:16], KR[:, 0:nC, 16:32]
                        tt("dve", T3[:, 0:nC], k1, Ck, ALU.mult, ["KR", "COS"], ["T3"])
                        tt("dve", T4[:, 0:nC], k2, Sk, ALU.mult, ["KR", "SIN"], ["T4"])
                        tt("dve", KR2[:, 0:nC, 0:16], T3[:, 0:nC], T4[:, 0:nC], ALU.subtract, ["T3", "T4"], ["KR2"])
                        tt("dve", T3[:, 0:nC], k1, Sk, ALU.mult, ["KR", "SIN"], ["T3"])
                        tt("dve", T4[:, 0:nC], k2, Ck, ALU.mult, ["KR", "COS"], ["T4"])
                        tt("dve", KR2[:, 0:nC, 16:32], T3[:, 0:nC], T4[:, 0:nC], ALU.add, ["T3", "T4"], ["KR2"])
                        krs = KR2
                    else:
                        krs = KR
                    cp("dve", KTM4[:, :, :, 64:96], krs[:, 0:nC, :].unsqueeze(2).to_broadcast([128, nC, 8, 32]), ["KR", "KR2"], ["KTM"])
                    for c in range(nC):
                        if lat:
                            trs([(PTQ[:, h, :], QTM[:, c, h, :], ident_b[:]) for h in range(8)], ["QTM", "ident_b"], ["PTQ"])
                            cp("act", QST[:, :, c * 128:(c + 1) * 128], PTQ[:], ["PTQ"], ["QST"])
                        trs([(PTK[:, h, :], KTM[:, c, h, :], ident_b[:]) for h in range(8)], ["KTM", "ident_b"], ["PTK"])
                        cp("dve", KST[:, :, c * 128:(c + 1) * 128], PTK[:], ["PTK"], ["KST"])
                    if lat:
                        dma("sp", QT_v[:, :, t0 - LC:t0 - LC + 512], QST[:], ["QST"], [], "qst")
                    dma("sp", KT_v[:, :, t0:t0 + nC * 128], KST[:, :, 0:nC * 128], ["KST"], [], "kst")
                    dma("sp", V_v[:, t0 // 128:t0 // 128 + nC, :], VTM[:, 0:nC].rearrange("p c h d -> p c (h d)"), ["VTM"], [], "vst")
                S.flush()
        if limit <= 3:
            return nc

        with ExitStack() as ph:
            GZs = [sb(ph, f"GZs{i}", [128, 1024], F32) for i in range(2)]
            XRH = [sb(ph, f"XRH{i}", [128, T], F32) for i in range(2)]
            XRB = [sb(ph, f"XRB{i}", [128, T], BF16) for i in range(2)]
            BB = [[sb(ph, f"B{j}_{d}", [128, T], F32) for j in range(3)] for d in range(2)]
            HF = sb(ph, "HF", [128, L], F32)
            HBC = [sb(ph, f"HBC{d}", [128, LC], F32) for d in range(2)]
            RNNB = [sb(ph, f"RNNB{i}", [128, 1024], BF16) for i in range(2)]
            rgw = [sb(ph, f"rgw{i}", [128, 2, 2, 128], BF16) for i in range(2)]
            pa = [ps(ph, f"pa{i}", [128, 512], F32) for i in range(2)]
            px = [ps(ph, f"px{i}", [128, 512], F32) for i in range(2)]
            NB = 9

            def load_n(n):
                s = n % 2
                for d in range(2):
                    dma("pool", rgw[s][:, d, 0, :], rgwa_d[d, n], [], [("rgw", s, d, 0)], f"rgw{s}")
                    dma("pool", rgw[s][:, d, 1, :], rgwx_d[d, n], [], [("rgw", s, d, 1)], f"rgw{s}")
                dma("sp", XRH[s][:], z0_d[n], [], [("XRH", s)], f"ldx{s}")
                ts("pool", XRB[s][:], XRH[s][:], 2.0, 0.0, ALU.mult, ALU.add, [("XRH", s)], [("XRB", s)])

            load_n(0)
            gzc = 0
            for n in range(8):
                s = n % 2
                if n + 1 < 8:
                    load_n(n + 1)
                for d in range(2):
                    B1, B2, B3 = BB[d]
                    K = lambda nm: [((nm, d), b) for b in range(NB)]
                    for b in range(NB):
                        lo, hi = blk(b)
                        w = hi - lo
                        mmg(pa[b % 2][:, 0:w], [(rgw[s][:, d, 0, :], XRB[s][:, lo:hi])], [("rgw", s, d, 0), ("XRB", s)], [("pa", b % 2)])
                        mmg(px[b % 2][:, 0:w], [(rgw[s][:, d, 1, :], XRB[s][:, lo:hi])], [("rgw", s, d, 1), ("XRB", s)], [("px", b % 2)])
                        act(B1[:, lo:hi], pa[b % 2][:, 0:w], AF.Tanh, [("pa", b % 2)], [(("B1", d), b)], scale=0.5, bias=HBA[:, d, n:n + 1])
                        act(B2[:, lo:hi], B1[:, lo:hi], AF.Exp, [(("B1", d), b)], [(("B2", d), b)], scale=HC[:, d, n:n + 1], bias=HC[:, d, n:n + 1])
                        act(B3[:, lo:hi], B1[:, lo:hi], AF.Exp, [(("B1", d), b)], [(("B3", d), b)], scale=CN[:, d, n:n + 1], bias=CN[:, d, n:n + 1])
                        act(B1[:, lo:hi], px[b % 2][:, 0:w], AF.Tanh, [("px", b % 2)], [(("B1", d), b)], scale=0.5, bias=HBX[:, d, n:n + 1])
                    act(B3[:], B3[:], AF.Sqrt, K("B3"), K("B3"), scale=-1.0, bias=1.0)
                for d in range(2):
                    B1, B2, B3 = BB[d]
                    K = lambda nm: [((nm, d), b) for b in range(NB)]
                    stt(B1[:], B1[:], 1.0, B3[:], ALU.add, ALU.mult, K("B1") + K("B3"), K("B1"))
                    tt("dve", B1[:], B1[:], XRH[s][:], ALU.mult, K("B1") + [("XRH", s)], K("B1"))
                    if d == 0:
                        S.op("dve", lambda e, B1=B1, B2=B2: e.tensor_tensor_scan(HBC[0][:], B2[:, 0:LC], B1[:, 0:LC], 0.0, ALU.mult, ALU.add),
                             K("B1") + K("B2"), ["HBC0"])
                        S.op("dve", lambda e, B1=B1, B2=B2: e.tensor_tensor_scan(HF[:], B2[:, LC:T], B1[:, LC:T], HBC[0][:, LC - 1:LC], ALU.mult, ALU.add),
                             K("B1") + K("B2") + ["HBC0"], ["HF"])
                    else:
                        S.op("dve", lambda e, B1=B1, B2=B2: e.tensor_tensor_scan(HBC[1][:, ::-1], B2[:, 0:LC][:, ::-1], B1[:, 0:LC][:, ::-1], 0.0, ALU.mult, ALU.add),
                             K("B1") + K("B2"), ["HBC1"])
                        S.op("dve", lambda e, B1=B1, B2=B2, B3=B3: e.tensor_tensor_scan(B3[:, LC:T][:, ::-1], B2[:, LC:T][:, ::-1], B1[:, LC:T][:, ::-1], HBC[1][:, 0:1], ALU.mult, ALU.add),
                             K("B1") + K("B2") + K("B3") + ["HBC1"], K("B3"))
                        for sg in range(4):
                            lo = LC + sg * 1024
                            l0 = sg * 1024
                            gq = gzc % 2
                            gzc += 1
                            dma("sp", GZs[gq][:], w2_d[n, :, l0:l0 + 1024], [], [("GZs", gq)], f"ldgz{gq}")
                            kb = [(("B3", d), 1 + 2 * sg), (("B3", d), 2 + 2 * sg)]
                            tt("dve", B3[:, lo:lo + 1024], B3[:, lo:lo + 1024], HF[:, l0:l0 + 1024], ALU.add, K("B3") + ["HF"], kb)
                            stt(RNNB[sg % 2][:], B3[:, lo:lo + 1024], 0.5, GZs[gq][:], ALU.mult, ALU.mult, kb + [("GZs", gq)], [("RNNB", sg % 2)])
                            dma("sp", rnnT_d[n, :, l0:l0 + 1024], RNNB[sg % 2][:], [("RNNB", sg % 2)], [], f"rnn{sg % 2}")
            S.flush()
        if limit <= 4:
            return nc

        with ExitStack() as attnstack:
            attnTM = sb(attnstack, "attnTM", [128, 32, 512], BF16)
            with ExitStack() as ph:
                VT = sb(ph, "VT", [128, 34, 520], BF16)
                KTh = [sb(ph, f"KTh{i}", [96, T], BF16) for i in range(2)]
                QTh = [sb(ph, f"QTh{i}", [96, L], BF16) for i in range(2)]
                PT = [sb(ph, f"PT{i}", [128, 512], BF16) for i in range(4)]
                rec = [sb(ph, f"rec{i}", [128, 4], F32) for i in range(2)]
                Sb = [ps(ph, f"Sb{i}", [128, 512], F32) for i in range(4)]
                Ob = [ps(ph, f"Ob{i}", [128, 4, 65], F32) for i in range(2)]
                dma("sp", VT[:], V_d.rearrange("(c p) f -> p c f", p=128), [], ["VT"], "ldv")
                steps = [(h, qb, kc) for h in range(8) for qb in range(8) for kc in range(34)]
                LA = 3

                def score(i):
                    h, qb, kc = steps[i]
                    s = h % 2
                    mmg(Sb[i % 4][:], [(KTh[s][:, kc * 128:(kc + 1) * 128], QTh[s][:, qb * 512:(qb + 1) * 512])],
                        [("KQ", s)], [("Sb", i % 4)])

                for h in range(8):
                    s = h % 2
                    pass
                loaded = set()

                def load_head(h):
                    if h in loaded or h >= 8:
                        return
                    loaded.add(h)
                    s = h % 2
                    dma("sp", KTh[s][:], KT_d[h], [], [("KQ", s)], f"kq{s}")
                    dma("sp", QTh[s][:], QT_d[h], [], [("KQ", s)], f"kq{s}")

                load_head(0)
                for i in range(LA):
                    score(i)
                for i, (h, qb, kc) in enumerate(steps):
                    if qb == 0 and kc == 0:
                        load_head(h + 1)
                    if i + LA < len(steps):
                        if steps[i + LA][0] not in loaded:
                            load_head(steps[i + LA][0])
                        score(i + LA)
                    o = (h * 8 + qb) % 2
                    act(PT[i % 4][:], Sb[i % 4][:], AF.Exp, [("Sb", i % 4)], [("PT", i % 4)])

                    def pv(e, i=i, h=h, kc=kc, o=o):
                        ins = None
                        for qc in range(4):
                            ins = e.matmul(Ob[o][:, qc, :], lhsT=PT[i % 4][:, qc * 128:(qc + 1) * 128],
                                           rhs=VT[:, kc, h * 65:(h + 1) * 65],
                                           start=(kc == 0 and qc == 0), stop=(kc == 33 and qc == 3), skip_group_check=True)
                        return ins
                    S.op("pe", pv, [("PT", i % 4), "VT"], [("Ob", o)])
                    if kc == 33:
                        recip(rec[o][:], Ob[o][:, :, 64], [("Ob", o)], [("rec", o)])
                        tt("dve", attnTM[:, qb * 4:(qb + 1) * 4, h * 64:(h + 1) * 64], Ob[o][:, :, 0:64],
                           rec[o][:].unsqueeze(2).to_broadcast([128, 4, 64]), ALU.mult, [("Ob", o), ("rec", o)], [("attn", qb)])
                if debug:
                    dma("sp", attn_dbg, attnTM[:], [("attn", qb) for qb in range(8)], [], "dbg")
                S.flush()
            if limit <= 5:
                return nc

            S.op("pool", lambda e: e.iota(TOKID[:], pattern=[[128, 32]], base=0, channel_multiplier=1), [], ["TOKID"])
            with ExitStack() as ph:
                WR = sb(ph, "WR", [128, 8, D], BF16)
                WM = sb(ph, "WM", [128, 4, D], BF16)
                WO = sb(ph, "WO", [128, 8, D], BF16)
                bcE = sb(ph, "bcE", [128, 3072], F32)
                wr32 = sb(ph, "wr32", [128, 8, NE], F32)
                RT = [sb(ph, f"RT{i}", [128, 8, 512], BF16) for i in range(2)]
                GT = [sb(ph, f"GT{i}", [128, 2, 512], BF16) for i in range(2)]
                XB = [sb(ph, f"XB{i}", [128, 4, D], F32) for i in range(2)]
                AT = sb(ph, "AT", [128, 4, 512], BF16)
                MT = sb(ph, "MT", [128, 8, 512], BF16)
                tA = [sb(ph, f"tA{i}", [128, 512], F32) for i in range(2)]
                tB = [sb(ph, f"tB{i}", [128, 512], F32) for i in range(2)]
                tC = [sb(ph, f"tC{i}", [128, 512], F32) for i in range(2)]
                H2T = [sb(ph, f"H2T{i}", [128, 8, 128], F32) for i in range(2)]
                ROWS = [sb(ph, "ROWS0", [128, 4, ROWW], BF16)] * 2
                junkE = sb(ph, "junkE", [128, D], BF16)
                ssE = sb(ph, "ssE", [128, 4], F32)
                rsE = sb(ph, "rsE", [128, 4], F32)
                mxE = sb(ph, "mxE", [128, 4], F32)
                smE = sb(ph, "smE", [128, 4], F32)
                EX = sb(ph, "EX", [128, 4, NE], F32)
                P1 = [ps(ph, f"P1{i}", [128, 512], F32) for i in range(2)]
                P2 = [ps(ph, f"P2{i}", [128, 512], F32) for i in range(2)]
                P3 = [ps(ph, f"P3{i}", [128, 512], F32) for i in range(2)]
                PTA = ps(ph, "PTA", [128, 4, 128], BF16)
                PTH = ps(ph, "PTH", [128, 512], F32)
                dma("pool", WR[:], wrnn_d.rearrange("(k p) n -> p k n", p=128), [], ["WR"], "lde")
                dma("pool", WM[:], wmla_d.rearrange("(k p) n -> p k n", p=128), [], ["WM"], "lde")
                dma("pool", WO[:], wo_d.rearrange("(k p) n -> p k n", p=128), [], ["WO"], "lde")
                dma("sp", bcE[:], bc_d[:, 0:3072], [], ["bcE"], "lde2")
                dma("sp", wr32[:], wr_d.rearrange("(k p) n -> p k n", p=128), [], ["wr32"], "lde2")
                m2bc, m3bc, a2bc = bcE[:, 0:1024], bcE[:, 1024:2048], bcE[:, 2048:3072]
                rn_v = rnnT_d.rearrange("n p t -> p n t")
                G_v = G_d.rearrange("j p t -> p j t")
                x_v = x_d.rearrange("(g c p) d -> g p c d", c=4, p=128)
                o_v = out_d.rearrange("(g c p) d -> g p c d", c=4, p=128)
                rows_v = rows_d.rearrange("(g c p) f -> g p c f", c=4, p=128)

                def load_rt(bi):
                    if bi >= 8:
                        return
                    s = bi % 2
                    dma("sp", RT[s][:], rn_v[:, :, bi * 512:(bi + 1) * 512], [], [("RT", s)], f"ldb{s}")

                def load_xb(bi):
                    if bi >= 8:
                        return
                    s = bi % 2
                    dma("pool", XB[s][:], x_v[bi], [], [("XB", s)] + [("XB", s, c, hf) for c in range(4) for hf in range(2)] + [("H2", s, c) for c in range(4)], f"ldxb{s}")

                def front(bi):
                    s = bi % 2
                    load_rt(bi + 1)
                    for c in range(4):
                        trs([(PTA[:, kk, :], attnTM[:, bi * 4 + c, kk * 128:(kk + 1) * 128], ident_b[:]) for kk in range(4)],
                            ["ident_b"], ["PTA"])
                        cp("dve", AT[:, :, c * 128:(c + 1) * 128], PTA[:], ["PTA"], ["AT"])
                    for j in range(8):
                        q = j % 2
                        dma("sp", GT[q][:], G_d.rearrange("(a j) p t -> j p a t", a=2)[j][:, :, bi * 512:(bi + 1) * 512], [], [("GT", q)], f"ldg{q}")
                        mmg(P1[q][:], [(WR[:, k, j * 128:(j + 1) * 128], RT[s][:, k, :]) for k in range(8)], ["WR", ("RT", s)], [("P1", q)])
                        mmg(P2[q][:], [(WM[:, k, j * 128:(j + 1) * 128], AT[:, k, :]) for k in range(4)], ["WM", "AT"], [("P2", q)])
                        tt("dve", tA[q][:], P1[q][:], GT[q][:, 0, :], ALU.mult, [("P1", q), ("GT", q)], [("tA", q)])
                        tt("dve", tB[q][:], P2[q][:], GT[q][:, 1, :], ALU.mult, [("P2", q), ("GT", q)], [("tB", q)])
                        tt("dve", MT[:, j, :], tA[q][:], tB[q][:], ALU.add, [("tA", q), ("tB", q)], [("MT", j)])
                    for c in range(4):
                        for hf in range(2):
                            q = (c * 2 + hf) % 2
                            mmg(P3[q][:], [(MT[:, k, c * 128:(c + 1) * 128], WO[:, k, hf * 512:(hf + 1) * 512]) for k in range(8)],
                                ["WO"] + [("MT", k) for k in range(8)], [("P3", q)])
                            tt("dve", tC[q][:], P3[q][:], m2bc[:, hf * 512:(hf + 1) * 512], ALU.mult, [("P3", q), "bcE"], [("tC", q)])
                            tt("dve", XB[s][:, c, hf * 512:(hf + 1) * 512], XB[s][:, c, hf * 512:(hf + 1) * 512], tC[q][:], ALU.add,
                               [("XB", s), ("tC", q)], [("XB", s, c, hf)])
                    xk = [("XB", s, c, hf) for c in range(4) for hf in range(2)]
                    dma("sp", o_v[bi], XB[s][:], xk, [], f"ost{s}")

                def tail(bi):
                    s = bi % 2
                    xk = [("XB", s, c, hf) for c in range(4) for hf in range(2)]
                    mset("pool", ssE[:], 1.0, ["ssE"])
                    for c in range(4):
                        act(junkE[:], XB[s][:, c, :], AF.Square, xk, ["junkE", "ssE"], accum_out=ssE[:, c:c + 1])
                    ts("dve", rsE[:], ssE[:], 1.0 / D, EPS, ALU.mult, ALU.add, ["ssE"], ["rsE"])
                    act(rsE[:], rsE[:], AF.Sqrt, ["rsE"], ["rsE"])
                    recip(rsE[:], rsE[:], ["rsE"], ["rsE"])
                    H2F = XB[s]
                    PLv = P1[0][:, 0:64].rearrange("p (c e) -> p c e", e=NE)
                    for c in range(4):
                        stt(H2F[:, c, :], XB[s][:, c, :], rsE[:, c:c + 1], a2bc, ALU.mult, ALU.mult, xk + ["rsE", "bcE"], [("H2", s, c)])
                        tt("dve", H2F[:, c, :], H2F[:, c, :], m3bc, ALU.add, [("H2", s, c), "bcE"], [("H2", s, c)])
                        cp("act", ROWS[s][:, c, 0:1024], H2F[:, c, :], [("H2", s, c)], [("ROWS", s)])
                        hq = c % 2
                        for kh in range(2):
                            trs([(P3[kh][:, kk * 128:(kk + 1) * 128], H2F[:, c, (kh * 4 + kk) * 128:(kh * 4 + kk + 1) * 128], ident_f[:]) for kk in range(4)],
                                [("H2", s, c), "ident_f"], [("P3", kh)])
                            cp("act" if kh else "dve", H2T[hq][:, kh * 4:(kh + 1) * 4, :], P3[kh][:].rearrange("p (k t) -> p k t", t=128), [("P3", kh)], [("H2T", hq, kh)])
                        mmg(PLv[:, c, :], [(H2T[hq][:, k, :], wr32[:, k, :]) for k in range(8)],
                            [("H2T", hq, 0), ("H2T", hq, 1), "wr32"], [("P1", 0)])
                    red(mxE[:], PLv, [("P1", 0)], ["mxE"], op=ALU.max)
                    ts("dve", mxE[:], mxE[:], -1.0, 0.0, ALU.mult, ALU.add, ["mxE"], ["mxE"])
                    for c in range(4):
                        act(EX[:, c, :], PLv[:, c, :], AF.Exp, [("P1", 0), "mxE"], ["EX", "smE"], bias=mxE[:, c:c + 1], accum_out=smE[:, c:c + 1])
                    recip(smE[:], smE[:], ["smE"], ["smE"])
                    tt("dve", AFF[:, bi * 4:(bi + 1) * 4, :], EX[:], smE[:].unsqueeze(2).to_broadcast([128, 4, NE]), ALU.mult, ["EX", "smE"], ["AFF"])
                    cp("dve", ROWS[s][:, :, 1024:1026].bitcast(I32), TOKID[:, bi * 4:(bi + 1) * 4].unsqueeze(2), ["TOKID"], [("ROWS", s)])
                    cp("dve", ROWS[s][:, :, 1026:1058].bitcast(F32), AFF[:, bi * 4:(bi + 1) * 4, :], ["AFF"], [("ROWS", s)])
                    dma("sp", rows_v[bi], ROWS[s][:], [("ROWS", s)], [], f"rst{s}")

                load_rt(0)
                load_xb(0)
                load_xb(1)
                front(0)
                for bi in range(1, 8):
                    front(bi)
                    tail(bi - 1)
                    load_xb(bi + 1)
                tail(7)
                if debug:
                    dma("sp", aff_dbg, AFF[:], ["AFF"], [], "dbg")
                S.flush()
        if limit <= 6:
            return nc

        gstack = es.enter_context(ExitStack())
        ROWSG = sb(gstack, "ROWSG", [128, 32, ROWW], BF16)
        WG = [sb(gstack, f"WG{i}", [128, 8, D], BF16) for i in range(2)]
        WU = [sb(gstack, f"WU{i}", [128, 8, D], BF16) for i in range(2)]
        WD = sb(gstack, "WD", [128, 8, D], BF16)
        rows_g = rows_d.rearrange("(g c p) f -> g p c f", c=8, p=128)
        for g in range(4):
            dma("sp", ROWSG[:, g * 8:(g + 1) * 8, :], rows_g[g], [], [("RG", g)], "ldrg")

        def L_gu(e_):
            if e_ >= NE:
                return
            s = e_ % 2
            dma("pool", WG[s][:], weg_d[e_].rearrange("(k p) f -> p k f", p=128), [], [("WG", s)], f"wg_{s}")
            dma("pool", WU[s][:], weu_d[e_].rearrange("(k p) f -> p k f", p=128), [], [("WU", s)], f"wu_{s}")

        def L_d(e_):
            if e_ >= NE:
                return
            dma("pool", WD[:], wed_d[e_].rearrange("(k p) f -> p k f", p=128), [], ["WD"], "wd_")

        L_gu(0)
        L_d(0)
        L_gu(1)
        with ExitStack() as ph:
            LO = sb(ph, "LO", [128, NE], F32)
            HI = sb(ph, "HI", [128, NE], F32)
            MID = sb(ph, "MID", [128, NE], F32)
            D1 = sb(ph, "D1", [128, NE], F32)
            D2 = sb(ph, "D2", [128, NE], F32)
            GE = sb(ph, "GE", [128, NE], F32)
            MASK = sb(ph, "MASK", [128, 32, NE], BF16)
            CNTP = sb(ph, "CNTP", [128, NE], F32)
            ones32 = sb(ph, "ones32", [128, 128], F32)
            ones_b = sb(ph, "ones_b", [128, 128], BF16)
            ones_f = sb(ph, "ones_f", [128, 32], F32)
            LTRI = sb(ph, "LTRI", [128, 128], BF16)
            LTF = sb(ph, "LTF", [128, 128], F32)
            MF = sb(ph, "MF", [128, 32, NE], F32)
            TOT = sb(ph, "TOT", [128, 32, NE], F32)
            CUM = sb(ph, "CUM", [128, 32, NE], F32)
            POS = sb(ph, "POS", [128, 32, NE], F32)
            VAL = sb(ph, "VAL", [128, 32, NE], F32)
            OFI = sb(ph, "OFI", [128, 32, NE], I32)
            OFF = sb(ph, "OFF", [128, 32, NE], F32)
            PC = ps(ph, "PC", [128, NE], F32)
            PP = ps(ph, "PP", [128, 512], F32)
            PTOT = ps(ph, "PTOT", [128, 512], F32)
            mset("pool", ones_b[:], 1.0, ["ones_b"])
            mset("pool", ones32[:], 1.0, ["ones32"])
            mset("pool", ones_f[:], 1.0, ["ones_f"])
            mset("pool", LTF[:], 1.0, ["LTF"])
            S.op("pool", lambda e: e.affine_select(out=LTF[:], in_=LTF[:], pattern=[[1, 128]], compare_op=ALU.is_ge, fill=0.0,
                                                    base=-1, channel_multiplier=-1), ["LTF"], ["LTF"])
            cp("pool", LTRI[:], LTF[:], ["LTF"], ["LTRI"])
            S.op("pool", lambda e: e.iota(OFI[:], pattern=[[0, 32], [CAP, NE]], base=-int(BIG), channel_multiplier=0), [], ["OFI"])
            cp("pool", OFF[:], OFI[:], ["OFI"], ["OFF"])
            mset("dve", LO[:], 0.0, ["LO"])
            mset("dve", HI[:], 1.0, ["HI"])
            mset("dve", MID[:], 0.5, ["MID"])
            for it in range(30):
                tt("dve", MASK[:], AFF[:], MID[:].unsqueeze(1).to_broadcast([128, 32, NE]), ALU.is_gt, ["AFF", "MID"], ["MASK"])
                red(CNTP[:], MASK[:].rearrange("p c e -> p e c"), ["MASK"], ["CNTP"])
                mmg(PC[:], [(ones32[:], CNTP[:])], ["ones32", "CNTP"], ["PC"])
                ts("dve", GE[:], PC[:], CAP - 0.5, 2.0 ** -(it + 1), ALU.is_ge, ALU.mult, ["PC"], ["GE"])
                tt("dve", LO[:], LO[:], GE[:], ALU.add, ["LO", "GE"], ["LO"])
                ts("dve", MID[:], LO[:], 2.0 ** -(it + 2), 0.0, ALU.add, ALU.add, ["LO"], ["MID"])
            tt("dve", MASK[:], AFF[:], LO[:].unsqueeze(1).to_broadcast([128, 32, NE]), ALU.is_gt, ["AFF", "LO"], ["MASK"])
            cp("pool", MF[:], MASK[:], ["MASK"], ["MF"])
            mflat = MASK[:].rearrange("p c e -> p (c e)")
            mmg(PP[:], [(LTRI[:], mflat)], ["LTRI", "MASK"], ["PP"])
            mmg(PTOT[:], [(ones_b[:], mflat)], ["ones_b", "MASK"], ["PTOT"])
            cp("act", TOT[:].rearrange("p c e -> p (c e)"), PTOT[:], ["PTOT"], ["TOT"])
            for e_ in range(NE):
                S.op("dve", lambda e, e_=e_: e.tensor_tensor_scan(CUM[:, :, e_], ones_f[:], TOT[:, :, e_], 0.0, ALU.mult, ALU.add),
                     ["ones_f", "TOT"], [("CUM", e_)])
            ck = [("CUM", e_) for e_ in range(NE)]
            tt("dve", CUM[:], CUM[:], TOT[:], ALU.subtract, ck + ["TOT"], ["CUMX"])
            tt("dve", POS[:].rearrange("p c e -> p (c e)"), PP[:], CUM[:].rearrange("p c e -> p (c e)"), ALU.add, ["PP", "CUMX"], ["POS"])
            ts("dve", VAL[:], POS[:], float(CAP) - 0.5, 0.0, ALU.is_lt, ALU.add, ["POS"], ["VAL"])
            tt("dve", VAL[:], VAL[:], MF[:], ALU.mult, ["VAL", "MF"], ["VAL"])
            tt("dve", POS[:], POS[:], OFF[:], ALU.add, ["POS", "OFF"], ["POS"])
            tt("dve", POS[:], POS[:], VAL[:], ALU.mult, ["POS", "VAL"], ["POS"])
            ts("dve", POS[:], POS[:], BIG, 0.0, ALU.add, ALU.add, ["POS"], ["POS"])
            cp("dve", IDX[:], POS[:], ["POS"], ["IDX"])
            if debug:
                dma("sp", idx_dbg, IDX[:], ["IDX"], [], "dbg")
            S.flush()
        if limit <= 7:
            return nc

        with ExitStack() as ph:
            XG = sb(ph, "XG", [128, 4, ROWW], BF16)
            TI = [sb(ph, f"TI{i}", [128, 4], I32) for i in range(2)]
            GA = [sb(ph, f"GA{i}", [128, 4], F32) for i in range(2)]
            XGT = sb(ph, "XGT", [128, 8, 512], BF16)
            HID = sb(ph, "HID", [128, 8, 512], BF16)
            SGT = [sb(ph, f"SGT{i}", [128, 512], F32) for i in range(2)]
            YO = sb(ph, "YO", [128, 4, D], F32)
            m5 = sb(ph, "m5", [128, D], F32)
            PTX = [ps(ph, f"PTX{i}", [128, 4, 128], BF16) for i in range(2)]
            PG = [ps(ph, f"PG{i}", [128, 512], F32) for i in range(2)]
            PU = [ps(ph, f"PU{i}", [128, 512], F32) for i in range(2)]
            PY = [ps(ph, f"PY{i}", [128, 512], F32) for i in range(2)]
            dma("sp", m5[:], bc_d[:, 3072:4096], [], ["m5"], "ldg")
            def scat(e_):
                if e_ >= NE:
                    return
                for ci in range(32):
                    S.op("pool", lambda e, ci=ci, e_=e_: e.indirect_dma_start(
                        out=xg_d, out_offset=bass.IndirectOffsetOnAxis(ap=IDX[:, ci, e_:e_ + 1], axis=0),
                        in_=ROWSG[:, ci, :], in_offset=None, bounds_check=R["bc_xg"], oob_is_err=False),
                        [("RG", ci // 8)], [("xg", e_, ci)], dma=f"sc{e_}")

            scat(0)
            scat(1)
            scat(2)
            xg_v = xg_d.rearrange("(e g p) f -> e p g f", g=4, p=128)
            for e_ in range(NE):
                s = e_ % 2
                dma("sp", XG[:], xg_v[e_], [("xg", e_, ci) for ci in range(32)], ["XG"], "xgl")
                cp("dve", TI[s][:].unsqueeze(2), XG[:, :, 1024:1026].bitcast(I32), ["XG"], [("TI", s)])
                cp("dve", GA[s][:].unsqueeze(2), XG[:, :, 1026 + 2 * e_:1028 + 2 * e_].bitcast(F32), ["XG"], [("GA", s)])
                for k in range(8):
                    trs([(PTX[k % 2][:, g, :], XG[:, g, k * 128:(k + 1) * 128], ident_b[:]) for g in range(4)],
                        ["XG", "ident_b"], [("PTX", k % 2)])
                    cp("act" if k % 2 else "dve", XGT[:, k, :], PTX[k % 2][:].rearrange("p g t -> p (g t)"), [("PTX", k % 2)], [("XGT", k)])
                xk = [("XGT", k) for k in range(8)]
                for f in range(8):
                    q = f % 2
                    mmg(PG[q][:], [(WG[s][:, k, f * 128:(f + 1) * 128], XGT[:, k, :]) for k in range(8)], xk + [("WG", s)], [("PG", q)])
                    mmg(PU[q][:], [(WU[s][:, k, f * 128:(f + 1) * 128], XGT[:, k, :]) for k in range(8)], xk + [("WU", s)], [("PU", q)])
                    act(SGT[q][:], PG[q][:], AF.Silu, [("PG", q)], [("SGT", q)])
                    tt("dve", HID[:, f, :], SGT[q][:], PU[q][:], ALU.mult, [("SGT", q), ("PU", q)], [("HID", f)])
                L_gu(e_ + 2)
                hk = [("HID", f) for f in range(8)]
                for g in range(4):
                    for hf in range(2):
                        q = (g * 2 + hf) % 2
                        mmg(PY[q][:], [(HID[:, f, g * 128:(g + 1) * 128], WD[:, f, hf * 512:(hf + 1) * 512]) for f in range(8)],
                            hk + ["WD"], [("PY", q)])
                        stt(YO[:, g, hf * 512:(hf + 1) * 512], PY[q][:], GA[s][:, g:g + 1], m5[:, hf * 512:(hf + 1) * 512], ALU.mult, ALU.mult,
                            [("PY", q), ("GA", s), "m5"], [("YO", g)])
                    if g == 3:
                        L_d(e_ + 1)
                    S.op("pool", lambda e, s=s, g=g: e.indirect_dma_start(
                        out=out_d, out_offset=bass.IndirectOffsetOnAxis(ap=TI[s][:, g:g + 1], axis=0),
                        in_=YO[:, g, :], in_offset=None, bounds_check=R["bc_out"], oob_is_err=True, compute_op=ALU.add),
                        [("YO", g), ("TI", s)] + [("outd", gg) for gg in range(4) if gg != g], [("outd", g)], dma="sadd")
                scat(e_ + 3)
            S.flush()
    return nc


_CACHE = {}


def _rope_tables():
    rows = L // 64
    row = np.repeat(np.arange(rows, dtype=np.float32), 64)
    col = np.tile(np.arange(64, dtype=np.float32), rows)
    inv = (np.float32(10000.0) ** (-np.arange(8, dtype=np.float32) / np.float32(8))).astype(np.float32)
    ang = np.concatenate([row[:, None] * inv, col[:, None] * inv], axis=-1).astype(np.float32)
    return np.cos(ang).astype(np.float32), np.sin(ang).astype(np.float32)


def _fm(v):
    return np.ascontiguousarray(np.moveaxis(v.reshape(*v.shape[:-1], 8, 128), -1, 0))


def make_in_maps(inp):
    f = lambda a: np.ascontiguousarray(np.asarray(a, dtype=np.float32))
    cos, sin = _rope_tables()
    cosT = np.ascontiguousarray(cos.reshape(32, 128, 16).transpose(1, 0, 2))
    sinT = np.ascontiguousarray(sin.reshape(32, 128, 16).transpose(1, 0, 2))
    c_ctx = f(inp["c_ctx"])
    shared = dict(
        w_ada=f(inp["w_ada"][0]), b_ada2=np.ascontiguousarray(np.repeat(f(inp["b_ada"]), 2, axis=0)),
        g1T=_fm(f(inp["g_norm1"][0])), g2row=np.ascontiguousarray(np.repeat(f(inp["g_norm2"]), 2, axis=0)),
        w_in=f(inp["w_in"][0]),
        cwT=np.ascontiguousarray(_fm(f(inp["conv_w"][0])).transpose(0, 2, 1)),
        cbT=_fm(f(inp["conv_b"][0])),
        rg_wa=f(inp["rg_wa"][0]), rg_wx=f(inp["rg_wx"][0]),
        rgbT=np.ascontiguousarray(np.stack([_fm(f(inp["rg_ba"][0])), _fm(f(inp["rg_bx"][0])), _fm(f(inp["rg_lambda"][0]))], axis=1)),
        w_rnn_out=f(inp["w_rnn_out"][0]), w_mla_out=f(inp["w_mla_out"][0]), w_o=f(inp["w_o"][0]),
        w_uq=f(inp["w_uq"][0]), gql256=np.ascontiguousarray(np.tile(f(inp["g_q_lora"][0])[None, :], (128, 1))),
        w_uk=f(inp["w_uk"][0]), w_uv=f(inp["w_uv"][0]), gkv128=np.ascontiguousarray(np.tile(f(inp["g_kv_lora"][0])[None, :], (128, 1))),
        gq96=np.ascontiguousarray(np.tile(np.concatenate([f(inp["g_q_nope"][0]), f(inp["g_q_rope"][0])])[None, :], (128, 1))),
        gk96=np.ascontiguousarray(np.tile(np.concatenate([f(inp["g_k_nope"][0]), f(inp["g_k_rope"][0])])[None, :], (128, 1))),
        cosT=cosT, sinT=sinT, w_router=f(inp["w_router"][0]),
        w_e_gate=f(inp["w_e_gate"][0]), w_e_up=f(inp["w_e_up"][0]), w_e_down=f(inp["w_e_down"][0]),
    )
    maps = []
    for b in range(8):
        c2 = np.stack([f(inp["c"][b]), c_ctx], axis=0)
        m = dict(shared)
        m["x"] = f(inp["x"][b])
        m["ctx"] = f(inp["ctx"][b])
        m["c2T"] = np.ascontiguousarray(c2.reshape(2, 8, 128).transpose(2, 1, 0))
        maps.append(m)
    return maps


def kernel(**inputs):
    if "nc" not in _CACHE:
        _CACHE["nc"] = build_program()
    nc = _CACHE["nc"]
    in_maps = make_in_maps(inputs)
    res = run_bass_kernel_spmd(nc, in_maps, core_ids=list(range(8)))
    return np.stack([np.asarray(r["out"], dtype=np.float32) for r in res.results], axis=0)
```

```python
import bisect
from contextlib import ExitStack

import numpy as np
import concourse.bass as bass
import concourse.mybir as mybir
from concourse.bass_utils import run_bass_kernel_spmd

F32 = mybir.dt.float32
BF16 = mybir.dt.bfloat16
I32 = mybir.dt.int32
AF = mybir.ActivationFunctionType
ALU = mybir.AluOpType
AX = mybir.AxisListType

ENGS = ("pe", "act", "dve", "pool", "sp")
EPOCH = 30000

L = 4096
LC = 256
T = L + LC
D = 1024
NE = 16
CAP = 512
ROWW = 1058
BIG = float(1 << 20)
EPS = 1e-6


class Sched:
    def __init__(self, nc, es):
        self.nc = nc
        self.es = es
        self.ops = []
        self.lastw = {}
        self.readers = {}
        self.last_eng = {}
        self.dma_ops = {}
        self.setups = {}
        self.emitted = 0
        self.cnt = {e: 0 for e in ENGS}
        self.esems = {e: [] for e in ENGS}
        self.dsems = {}
        self.known = {}
        self.setup_done = {}

    def setup(self, eng, fn):
        self.setups.setdefault(eng, []).append(fn)

    def op(self, eng, fn, reads=(), writes=(), dma=None):
        i = len(self.ops)
        deps = {}
        for r in reads:
            w = self.lastw.get(r)
            if w is not None:
                deps[w] = True
        for r in writes:
            w = self.lastw.get(r)
            if w is not None and not deps.get(w):
                deps[w] = 2
            for j in self.readers.get(r, {}).values():
                deps.setdefault(j, False)
        self.ops.append(dict(eng=eng, fn=fn, deps=deps, dma=dma, signal=False))
        slot = ("d", dma) if dma is not None else ("e", eng)
        for r in reads:
            self.readers.setdefault(r, {})[slot] = i
        for r in writes:
            self.lastw[r] = i
            self.readers[r] = {}
        if dma is not None:
            self.dma_ops.setdefault(dma, []).append(i)
        else:
            self.last_eng[eng] = i
        return i

    def barrier(self):
        deps = {}
        for e, i in self.last_eng.items():
            deps[i] = True
        for k, lst in self.dma_ops.items():
            deps[lst[-1]] = True
        for e in ENGS:
            self.ops.append(dict(eng=e, fn=None, deps=dict(deps), dma=None, signal=False, bar=True))
        self.lastw = {}
        self.readers = {}

    def flush(self):
        self.barrier()
        nc = self.nc
        ops = self.ops
        lo = self.emitted
        for o in ops[lo:]:
            for d in o["deps"]:
                ops[d]["signal"] = True
        for o in ops[lo:]:
            if o["fn"] is not None and o["dma"] is None and o["signal"]:
                self.cnt[o["eng"]] += 1
                o["val"] = self.cnt[o["eng"]]
        for k in self.dma_ops:
            if k not in self.dsems:
                self.dsems[k] = self.es.enter_context(nc.semaphore(f"d_{k}"))
        esems, dsems = self.esems, self.dsems

        def esem(e, v):
            k = (v - 1) // EPOCH
            while len(esems[e]) <= k:
                esems[e].append(self.es.enter_context(nc.semaphore(f"s_{e}{len(esems[e])}")))
            return esems[e][k], (v - 1) % EPOCH + 1

        def events(j, o):
            evs = []
            for d, raw in o["deps"].items():
                p = ops[d]
                if p["fn"] is None:
                    continue
                if p["dma"] is not None:
                    lst = self.dma_ops[p["dma"]]
                    n = bisect.bisect_left(lst, j)
                    evs.append((dsems[p["dma"]], 16 * n))
                else:
                    if p["eng"] == o["eng"]:
                        if o["eng"] == "pe" or (not raw and not o.get("bar")):
                            continue
                    evs.append(esem(p["eng"], p["val"]))
            return evs

        for o in ops[lo:]:
            if "val" in o:
                esem(o["eng"], o["val"])

        def make(ename):
            def body(eng):
                known = self.known.setdefault(ename, {})
                if not self.setup_done.get(ename):
                    self.setup_done[ename] = True
                    for f in self.setups.get(ename, []):
                        f(eng)
                for j in range(lo, len(ops)):
                    o = ops[j]
                    if o["eng"] != ename:
                        continue
                    need = {}
                    for sem, v in events(j, o):
                        key = id(sem)
                        if known.get(key, 0) >= v:
                            continue
                        if key not in need or need[key][1] < v:
                            need[key] = (sem, v)
                    for key, (sem, v) in need.items():
                        eng.wait_ge(sem, v)
                        known[key] = v
                    if o["fn"] is None:
                        continue
                    ins = o["fn"](eng)
                    if o["dma"] is not None:
                        ins.then_inc(dsems[o["dma"]], 16)
                    elif o["signal"]:
                        sem, _ = esem(ename, o["val"])
                        ins.then_inc(sem, 1)
            return body

        with nc.Block() as block:
            block.sync(make("sp"))
            block.scalar(make("act"))
            block.vector(make("dve"))
            block.gpsimd(make("pool"))
            block.tensor(make("pe"))
        self.emitted = len(ops)


def blk(b):
    if b == 0:
        return 0, LC
    return LC + (b - 1) * 512, LC + b * 512


def build_program(limit=99, debug=False):
    nc = bass.Bass("TRN2", target_bir_lowering=False)

    def din(name, shape, dt=F32):
        return nc.dram_tensor(name, list(shape), dt, kind="ExternalInput").ap()

    def dscr(name, shape, dt):
        return nc.dram_tensor(name, list(shape), dt, kind="ExternalOutput" if debug else "Internal").ap()

    x_d = din("x", [L, D])
    ctx_d = din("ctx", [LC, D])
    c2T_d = din("c2T", [128, 8, 2])
    wada_d = din("w_ada", [D, 6 * D])
    bada_d = din("b_ada2", [2, 6 * D])
    g1T_d = din("g1T", [128, 8])
    g2row_d = din("g2row", [2, D])
    win_d = din("w_in", [D, 4512])
    cwT_d = din("cwT", [128, 8, 4])
    cbT_d = din("cbT", [128, 8])
    rgwa_d = din("rg_wa", [2, 8, 128, 128])
    rgwx_d = din("rg_wx", [2, 8, 128, 128])
    rgbT_d = din("rgbT", [128, 3, 2, 8])
    wrnn_d = din("w_rnn_out", [D, D])
    wmla_d = din("w_mla_out", [512, D])
    wo_d = din("w_o", [D, D])
    wuq_d = din("w_uq", [256, 768])
    gql256_d = din("gql256", [128, 256])
    wuk_d = din("w_uk", [128, 512])
    wuv_d = din("w_uv", [128, 512])
    gkv128_d = din("gkv128", [128, 128])
    gq96_d = din("gq96", [128, 96])
    gk96_d = din("gk96", [128, 96])
    cos_d = din("cosT", [128, 32, 16])
    sin_d = din("sinT", [128, 32, 16])
    wr_d = din("w_router", [D, NE])
    weg_d = din("w_e_gate", [NE, D, D])
    weu_d = din("w_e_up", [NE, D, D])
    wed_d = din("w_e_down", [NE, D, D])
    out_d = nc.dram_tensor("out", [L, D], F32, kind="ExternalOutput").ap()

    bc_d = dscr("bc_d", [128, 4096], F32)
    rnnT_d = dscr("rnnT_d", [8, 128, L], BF16)
    z0_d = nc.dram_tensor("z0_d", [8, 128, T], F32, kind="Internal").ap()
    w2_d = nc.dram_tensor("w2_d", [8, 128, L], F32, kind="Internal").ap()
    G_d = dscr("G_d", [16, 128, L], BF16)
    QT_d = dscr("QT_d", [8, 96, L], BF16)
    KT_d = dscr("KT_d", [8, 96, T], BF16)
    V_d = dscr("V_d", [T, 520], BF16)
    rows_d = dscr("rows_d", [L, ROWW], BF16)
    xg_d = dscr("xg_d", [NE * CAP, ROWW], BF16)
    if debug:
        hT_dbg = dscr("hT_dbg", [128, 8, T], BF16)
        modT_dbg = dscr("modT_dbg", [128, 48, 2], F32)
        aff_dbg = dscr("aff_dbg", [128, 32, 16], F32)
        idx_dbg = dscr("idx_dbg", [128, 32, 16], I32)
        attn_dbg = dscr("attn_dbg", [128, 32, 512], BF16)

    win_v = win_d.rearrange("(k p) n -> p k n", p=128)

    with ExitStack() as es:
        S = Sched(nc, es)
        R = {}
        S.setup("pool", lambda e: R.__setitem__("bc_xg", e.to_reg(NE * CAP - 1)))
        S.setup("pool", lambda e: R.__setitem__("bc_out", e.to_reg(L - 1)))

        def sb(stack, name, shape, dt):
            return stack.enter_context(nc.sbuf_tensor(name, list(shape), dt))

        def ps(stack, name, shape, dt):
            return stack.enter_context(nc.psum_tensor(name, list(shape), dt))

        def dma(eng, out, in_, reads, writes, key):
            S.op(eng, lambda e: e.dma_start(out=out, in_=in_), reads, writes, dma=key)

        def act(out, in_, func, reads, writes, **kw):
            S.op("act", lambda e: e.activation(out=out, in_=in_, func=func, **kw), reads, writes)

        def ts(eng, out, in0, s1, s2, op0, op1, reads, writes):
            S.op(eng, lambda e: e.tensor_scalar(out, in0, s1, s2, op0, op1), reads, writes)

        def tt(eng, out, in0, in1, op, reads, writes):
            S.op(eng, lambda e: e.tensor_tensor(out, in0, in1, op), reads, writes)

        def stt(out, in0, scalar, in1, op0, op1, reads, writes):
            S.op("dve", lambda e: e.scalar_tensor_tensor(out, in0, scalar, in1, op0, op1), reads, writes)

        def cp(eng, out, in_, reads, writes):
            if eng == "act":
                S.op("act", lambda e: e.copy(out, in_), reads, writes)
            else:
                S.op(eng, lambda e: e.tensor_copy(out, in_), reads, writes)

        def recip(out, in_, reads, writes):
            S.op("dve", lambda e: e.reciprocal(out, in_), reads, writes)

        def red(out, in_, reads, writes, op=ALU.add):
            S.op("dve", lambda e: e.tensor_reduce(out, in_, AX.X, op), reads, writes)

        def mset(eng, ap, val, writes):
            S.op(eng, lambda e: e.memset(ap, val), (), writes)

        def mmg(out, pairs, reads, writes):
            def fn(e):
                n = len(pairs)
                ins = None
                for i, (l, r) in enumerate(pairs):
                    ins = e.matmul(out, lhsT=l, rhs=r, start=(i == 0), stop=(i == n - 1))
                return ins
            S.op("pe", fn, reads, writes)

        def trs(items, reads, writes):
            def fn(e):
                ins = None
                for o, i, idn in items:
                    ins = e.transpose(o, i, idn)
                return ins
            S.op("pe", fn, reads, writes)

        ident_f = sb(es, "ident_f", [128, 128], F32)
        ident_b = sb(es, "ident_b", [128, 128], BF16)
        modT = sb(es, "modT", [128, 48, 2], F32)
        A1T = sb(es, "A1T", [128, 8, 2], F32)
        HBA = sb(es, "HBA", [128, 2, 8], F32)
        HBX = sb(es, "HBX", [128, 2, 8], F32)
        CN = sb(es, "CN", [128, 2, 8], F32)
        HC = sb(es, "HC", [128, 2, 8], F32)
        CWH = sb(es, "CWH", [128, 8, 4], F32)
        CBH = sb(es, "CBH", [128, 8], F32)
        AFF = sb(es, "AFF", [128, 32, 16], F32)
        TOKID = sb(es, "TOKID", [128, 32], I32)
        IDX = sb(es, "IDX", [128, 32, NE], I32)

        mset("pool", ident_f[:], 0.0, ["ident_f"])
        S.op("pool", lambda e: e.affine_select(out=ident_f[:], in_=ident_f[:], pattern=[[-1, 128]],
                                                compare_op=ALU.not_equal, fill=1.0, base=0, channel_multiplier=1),
             ["ident_f"], ["ident_f"])
        cp("pool", ident_b[:], ident_f[:], ["ident_f"], ["ident_b"])

        with ExitStack() as ph:
            c2 = sb(ph, "c2", [128, 8, 2], F32)
            sc = sb(ph, "sc", [128, 8, 2], F32)
            wa = [sb(ph, f"wa{i}", [128, 8, 512], F32) for i in range(2)]
            modrow = sb(ph, "modrow", [2, 6 * D], F32)
            brow = sb(ph, "brow", [2, 6 * D], F32)
            g2r = sb(ph, "g2r", [2, D], F32)
            sel = sb(ph, "sel", [2, 128], F32)
            g1 = sb(ph, "g1", [128, 8], F32)
            rgb = sb(ph, "rgb", [128, 3, 2, 8], F32)
            spl = sb(ph, "spl", [128, 2, 8], F32)
            cw = sb(ph, "cw", [128, 8, 4], F32)
            cb = sb(ph, "cb", [128, 8], F32)
            bcs = [sb(ph, f"bcs{i}", [128, 512], F32) for i in range(2)]
            pm = [ps(ph, f"pm{i}", [2, 512], F32) for i in range(2)]
            pT0 = ps(ph, "pT0", [128, 48, 2], F32)
            pbc = [ps(ph, f"pbc{i}", [128, 512], F32) for i in range(2)]

            dma("sp", c2[:], c2T_d, [], ["c2"], "ld0")
            dma("sp", brow[:], bada_d, [], ["brow"], "ld0")
            dma("sp", g2r[:], g2row_d, [], ["g2r"], "ld0")
            dma("sp", g1[:], g1T_d, [], ["g1"], "ld0")
            dma("sp", rgb[:], rgbT_d, [], ["rgb"], "ld0")
            dma("sp", cw[:], cwT_d, [], ["cw"], "ld0")
            dma("sp", cb[:], cbT_d, [], ["cb"], "ld0")
            act(sc[:], c2[:], AF.Silu, ["c2"], ["sc"])
            wada_v = wada_d.rearrange("(k p) n -> p k n", p=128)
            for j in range(12):
                s = j % 2
                dma("sp", wa[s][:], wada_v[:, :, j * 512:(j + 1) * 512], [], [("wa", s)], f"wa{s}")
                mmg(pm[s][:], [(sc[:, k, :], wa[s][:, k, :]) for k in range(8)], ["sc", ("wa", s)], [("pm", s)])
                tt("dve", modrow[:, j * 512:(j + 1) * 512], pm[s][:], brow[:, j * 512:(j + 1) * 512], ALU.add,
                   [("pm", s), "brow"], ["modrow"])
            trs([(pT0[:, c, :], modrow[:, c * 128:(c + 1) * 128], ident_f[0:2, 0:2]) for c in range(48)],
                ["modrow", "ident_f"], ["pT0"])
            cp("dve", modT[:], pT0[:], ["pT0"], ["modT"])
            for r in range(2):
                stt(A1T[:, :, r], modT[:, 8:16, r], 1.0, g1[:], ALU.add, ALU.mult, ["modT", "g1"], ["A1T"])
            stt(modrow[:, 4096:5120], modrow[:, 4096:5120], 1.0, g2r[:], ALU.add, ALU.mult, ["modrow", "g2r"], ["modrow"])
            mset("pool", sel[:], 0.0, ["sel"])
            mset("pool", sel[0:1, :], 1.0, ["sel"])
            for q in range(8):
                s = q % 2
                mmg(pbc[s][:], [(sel[:], modrow[:, 2048 + q * 512:2048 + (q + 1) * 512])], ["sel", "modrow"], [("pbc", s)])
                cp("act", bcs[s][:], pbc[s][:], [("pbc", s)], [("bcs", s)])
                dma("sp", bc_d[:, q * 512:(q + 1) * 512], bcs[s][:], [("bcs", s)], [], f"bcst{s}")
            ts("pool", HBA[:], rgb[:, 0], 0.5, 0.0, ALU.mult, ALU.add, ["rgb"], ["HBA"])
            ts("pool", HBX[:], rgb[:, 1], 0.5, 0.0, ALU.mult, ALU.add, ["rgb"], ["HBX"])
            act(spl[:], rgb[:, 2], AF.Exp, ["rgb"], ["spl"], scale=-1.0)
            act(spl[:], spl[:], AF.Ln, ["spl"], ["spl"], bias=1.0)
            ts("pool", CN[:], spl[:], -8.0, 0.0, ALU.mult, ALU.add, ["spl"], ["CN"])
            ts("pool", HC[:], spl[:], -4.0, 0.0, ALU.mult, ALU.add, ["spl"], ["HC"])
            ts("pool", CWH[:], cw[:], 0.5, 0.0, ALU.mult, ALU.add, ["cw"], ["CWH"])
            ts("pool", CBH[:], cb[:], 0.5, 0.0, ALU.mult, ALU.add, ["cb"], ["CBH"])
            if debug:
                dma("sp", modT_dbg, modT[:], ["modT"], [], "dbg")
            S.flush()
        if limit <= 0:
            return nc

        with ExitStack() as midstack:
            hT = sb(midstack, "hT", [128, 8, T], BF16)

            with ExitStack() as ph:
                xt = [sb(ph, f"xt{i}", [128, 4, D], F32) for i in range(2)]
                junk = sb(ph, "junk", [128, D], BF16)
                ssA = [sb(ph, f"ssA{i}", [128, 4], F32) for i in range(2)]
                rsA = [sb(ph, f"rsA{i}", [128, 4], F32) for i in range(2)]
                pTa = [ps(ph, f"pTa{i}", [128, 512], F32) for i in range(4)]
                xv = x_d.rearrange("(g c p) d -> g p c d", c=4, p=128)
                cv = ctx_d.rearrange("(c p) d -> p c d", p=128)
                for g in range(9):
                    nC = 2 if g == 0 else 4
                    s = g % 2
                    r = 1 if g == 0 else 0
                    t0 = blk(g)[0]
                    src = cv if g == 0 else xv[g - 1]
                    dma("sp", xt[s][:, 0:nC, :], src, [], [("xt", s)] + [("xt", s, c) for c in range(4)], f"xt{s}")
                    mset("pool", ssA[s][:], 1.0, [("ssA", s)])
                    for c in range(nC):
                        act(junk[:], xt[s][:, c, :], AF.Square, [("xt", s)], ["junk", ("ssA", s)], accum_out=ssA[s][:, c:c + 1])
                    ts("dve", rsA[s][:], ssA[s][:], 1.0 / D, EPS, ALU.mult, ALU.add, [("ssA", s)], [("rsA", s)])
                    act(rsA[s][:], rsA[s][:], AF.Sqrt, [("rsA", s)], [("rsA", s)])
                    recip(rsA[s][:], rsA[s][:], [("rsA", s)], [("rsA", s)])
                    for c in range(nC):
                        eng = "dve" if c % 2 == 0 else "pool"
                        ts(eng, xt[s][:, c, :], xt[s][:, c, :], rsA[s][:, c:c + 1], 0.0, ALU.mult, ALU.add,
                           [("xt", s), ("rsA", s)], [("xt", s, c)])
                    for k in range(8):
                        bank = pTa[k % 4]
                        trs([(bank[:, c * 128:(c + 1) * 128], xt[s][:, c, k * 128:(k + 1) * 128], ident_f[:]) for c in range(nC)],
                            [("xt", s, c) for c in range(nC)] + ["ident_f"], [("pTa", k % 4)])
                        o = hT[:, k, t0:t0 + nC * 128]
                        i_ = bank[:, 0:nC * 128]
                        if k % 2 == 0:
                            ts("dve", o, i_, A1T[:, k, r:r + 1], modT[:, k, r:r + 1], ALU.mult, ALU.add,
                               [("pTa", k % 4), "A1T", "modT"], [("hT", g, k)])
                        else:
                            act(o, i_, AF.Identity, [("pTa", k % 4), "A1T", "modT"], [("hT", g, k)],
                                scale=A1T[:, k, r:r + 1], bias=modT[:, k, r:r + 1])
                if debug:
                    dma("sp", hT_dbg, hT[:], [("hT", g, k) for g in range(9) for k in range(8)], [], "dbg")
                S.flush()
            if limit <= 1:
                return nc
            HT_ALL = []

            with ExitStack() as ph:
                Z0S = [sb(ph, f"Z0S{i}", [128, 4360], F32) for i in range(2)]
                XRS = [sb(ph, f"XRS{i}", [128, T], F32) for i in range(2)]
                X1 = sb(ph, "X1", [128, L], F32)
                X2 = sb(ph, "X2", [128, L], F32)
                wz = [sb(ph, f"wz{i}", [128, 8, 256], BF16) for i in range(2)]
                pz = [ps(ph, f"pz{i}", [128, 512], F32) for i in range(4)]
                NB = 9

                def load_wz(n):
                    s = n % 2
                    dma("pool", wz[s][:, :, 0:128], win_v[:, :, n * 128:(n + 1) * 128], [], [("wz", s, 0)], f"wz{s}")
                    dma("pool", wz[s][:, :, 128:256], win_v[:, :, 1024 + n * 128:1024 + (n + 1) * 128], [], [("wz", s, 1)], f"wz{s}")

                for i in range(2):
                    mset("pool", Z0S[i][:], 0.0, [("Z0S", i, b) for b in range(NB)])
                load_wz(0)
                pc = 0
                for n in range(8):
                    s = n % 2
                    if n + 1 < 8:
                        load_wz(n + 1)
                    for b in range(NB):
                        lo, hi = blk(b)
                        w = hi - lo
                        zc = lo + 2 if b == 0 else lo + 6
                        q = pc % 4
                        pc += 1
                        mmg(pz[q][:, 0:w], [(wz[s][:, k, 0:128], hT[:, k, lo:hi]) for k in range(8)], [("wz", s, 0)], [("pz", q)])
                        cp("act", Z0S[s][:, zc:zc + w], pz[q][:, 0:w], [("pz", q)], [("Z0S", s, b)])
                    zk = [("Z0S", s, b) for b in range(NB)]
                    conv_late = []
                    for (o0, o1, zb) in ((0, LC, 0), (LC, T, 260)):
                        w = o1 - o0
                        ts("pool", XRS[s][:, o0:o1], Z0S[s][:, zb:zb + w], CWH[:, n, 0:1], CBH[:, n:n + 1], ALU.mult, ALU.add,
                           zk, [("XRS", s, o0)])
                        for k in range(1, 4):
                            args = (XRS[s][:, o0:o1], Z0S[s][:, zb + k:zb + k + w], CWH[:, n, k:k + 1], XRS[s][:, o0:o1], ALU.mult, ALU.add,
                                    zk + [("XRS", s, o0)], [("XRS", s, o0)])
                            if o0 == 0:
                                stt(*args)
                            else:
                                conv_late.append(args)
                    def tanh_stage(n_, sg):
                        sl = slice(sg * 1024, (sg + 1) * 1024)
                        act(X2[:, sl], X2[:, sl], AF.Tanh, [("X2", sg)], [("X2", sg)], scale=0.7978845608028654)
                        stt(X2[:, sl], X2[:, sl], 1.0, X1[:, sl], ALU.add, ALU.mult, [("X2", sg), ("X1", sg)], [("X2", sg)])
                        dma("sp", w2_d[n_, :, sg * 1024:(sg + 1) * 1024], X2[:, sl], [("X2", sg)], [], f"w2st{sg % 2}")

                    if n >= 1:
                        tanh_stage(n - 1, 3)
                    for sg in range(4):
                        bs = (1 + 2 * sg, 2 + 2 * sg)
                        l0 = sg * 1024
                        for b in bs:
                            blo, bhi = blk(b)
                            q = pc % 4
                            pc += 1
                            mmg(pz[q][:], [(wz[s][:, k, 128:256], hT[:, k, blo:bhi]) for k in range(8)], [("wz", s, 1)], [("pz", q)])
                            cp("act", X1[:, blo - LC:bhi - LC], pz[q][:], [("pz", q)], [("X1", sg)])
                            act(X2[:, blo - LC:bhi - LC], pz[q][:], AF.Square, [("pz", q)], [("X2", sg)], scale=0.044715 ** 0.5)
                        sl = slice(l0, l0 + 1024)
                        stt(X2[:, sl], X2[:, sl], 1.0, X1[:, sl], ALU.add, ALU.mult, [("X2", sg), ("X1", sg)], [("X2", sg)])
                        if sg >= 1:
                            tanh_stage(n, sg - 1)
                        if sg < 3:
                            stt(*conv_late[sg])
                    dma("sp", z0_d[n], XRS[s][:], [("XRS", s, 0), ("XRS", s, LC)], [], f"z0st{s}")
                tanh_stage(7, 3)
                S.flush()
            if limit <= 2:
                return nc

            with ExitStack() as ph:
                wg = [sb(ph, f"wg{i}", [128, 8, 128], BF16) for i in range(2)]
                GB = [sb(ph, f"GB{i}", [128, L], BF16) for i in range(2)]
                pg = [ps(ph, f"pg{i}", [128, 512], F32) for i in range(4)]
                for j in range(16):
                    s = j % 2
                    dma("pool", wg[s][:], win_v[:, :, 2464 + j * 128:2464 + (j + 1) * 128], [], [("wg", s)], f"wg{s}")
                    for b in range(1, 9):
                        lo, hi = blk(b)
                        mmg(pg[b % 4][:], [(wg[s][:, k, :], hT[:, k, lo:hi]) for k in range(8)], [("wg", s)], [("pg", b % 4)])
                        act(GB[s][:, lo - LC:hi - LC], pg[b % 4][:], AF.Sigmoid, [("pg", b % 4)], [("GB", s)])
                    dma("sp", G_d[j], GB[s][:], [("GB", s)], [], f"gst{s}")
                S.flush()
            if limit <= 3:
                return nc

            with ExitStack() as ph:
                wqkv = sb(ph, "wqkv", [128, 8, 416], BF16)
                wuq = sb(ph, "wuq", [128, 2, 768], BF16)
                wukv = sb(ph, "wukv", [128, 1024], BF16)
                GQL = sb(ph, "GQL", [128, 256], F32)
                GKV = sb(ph, "GKV", [128, 128], F32)
                GQ = sb(ph, "GQ", [128, 96], F32)
                GK = sb(ph, "GK", [128, 96], F32)
                COS = sb(ph, "COS", [128, 32, 16], F32)
                SIN = sb(ph, "SIN", [128, 32, 16], F32)
                INV3 = sb(ph, "INV3", [128, 4, 3], F32)
                INV24 = sb(ph, "INV24", [128, 4, 24], F32)
                Z = sb(ph, "Z", [128, 4, 416], F32)
                SQ = sb(ph, "SQ", [128, 4, 768], F32)
                ss3 = sb(ph, "ss3", [128, 4, 3], F32)
                r3 = sb(ph, "r3", [128, 4, 3], F32)
                ss24 = sb(ph, "ss24", [128, 4, 24], F32)
                r24 = sb(ph, "r24", [128, 4, 24], F32)
                ZQS = sb(ph, "ZQS", [128, 4, 256], F32)
                CKS = sb(ph, "CKS", [128, 4, 128], F32)
                KR = sb(ph, "KR", [128, 4, 32], F32)
                KR2 = sb(ph, "KR2", [128, 4, 32], F32)
                ZT = sb(ph, "ZT", [128, 4, 3, 128], BF16)
                QF = sb(ph, "QF", [128, 4, 768], F32)
                KF = sb(ph, "KF", [128, 4, 512], F32)
                T1 = sb(ph, "T1", [128, 4, 8, 16], F32)
                T2 = sb(ph, "T2", [128, 4, 8, 16], F32)
                T3 = sb(ph, "T3", [128, 4, 16], F32)
                T4 = sb(ph, "T4", [128, 4, 16], F32)
                QTM = sb(ph, "QTM", [128, 4, 8, 96], BF16)
                KTM = sb(ph, "KTM", [128, 4, 8, 96], BF16)
                VTM = sb(ph, "VTM", [128, 4, 8, 65], BF16)
                QST = sb(ph, "QST", [96, 8, 512], BF16)
                KST = sb(ph, "KST", [96, 8, 512], BF16)
                PQ = ps(ph, "PQ", [128, 1024], F32)
                PZ = [ps(ph, "PZ0", [128, 512], F32)] * 2
                PTZ = ps(ph, "PTZ", [128, 3, 128], F32)
                PK = ps(ph, "PK", [128, 512], F32)
                PV = ps(ph, "PV", [128, 512], F32)
                PTQ = ps(ph, "PTQ", [96, 8, 128], BF16)
                PTK = ps(ph, "PTK", [96, 8, 128], BF16)

                dma("pool", wqkv[:], win_v[:, :, 2048:2464], [], ["wqkv"], "ldc")
                dma("pool", wuq[:], wuq_d.rearrange("(k p) n -> p k n", p=128), [], ["wuq"], "ldc")
                dma("pool", wukv[:, 0:512], wuk_d, [], ["wukv"], "ldc")
                dma("pool", wukv[:, 512:1024], wuv_d, [], ["wukv2"], "ldc")
                dma("sp", GQL[:], gql256_d, [], ["GQL"], "ldc2")
                dma("sp", GKV[:], gkv128_d, [], ["GKV"], "ldc2")
                dma("sp", GQ[:], gq96_d, [], ["GQ"], "ldc2")
                dma("sp", GK[:], gk96_d, [], ["GK"], "ldc2")
                dma("sp", COS[:], cos_d, [], ["COS"], "ldc2")
                dma("sp", SIN[:], sin_d, [], ["SIN"], "ldc2")
                ts("dve", GQ[:], GQ[:], 96.0 ** -0.5, 0.0, ALU.mult, ALU.add, ["GQ"], ["GQ"])
                for j, v in enumerate((1.0 / 256, 1.0 / 128, 1.0 / 32)):
                    mset("pool", INV3[:, :, j:j + 1], v, ["INV3"])
                mset("pool", INV24[:, :, 0:8], 1.0 / 64, ["INV24"])
                mset("pool", INV24[:, :, 8:16], 1.0 / 32, ["INV24"])
                mset("pool", INV24[:, :, 16:24], 1.0 / 64, ["INV24"])
                mset("pool", ss3[:], 1.0, ["ss3"])
                mset("pool", ss24[:], 1.0, ["ss24"])
                mset("pool", VTM[:], 1.0, ["VTM"])
                QT_v = QT_d.rearrange("h d t -> d h t")
                KT_v = KT_d.rearrange("h d t -> d h t")
                V_v = V_d.rearrange("(c p) f -> p c f", p=128)

                for g in range(9):
                    nC = 2 if g == 0 else 4
                    lat = g > 0
                    t0 = blk(g)[0]
                    c0 = 0 if lat else 256
                    cg = (g - 1) * 4
                    for c in range(nC):
                        p = PZ[c % 2]
                        mmg(p[:, c0:416], [(hT[:, k, t0 + c * 128:t0 + (c + 1) * 128], wqkv[:, k, c0:416]) for k in range(8)],
                            ["wqkv"], [("PZ", 0)])
                        cp("act", Z[:, c, c0:416], p[:, c0:416], [("PZ", 0)], ["Z"])
                    act(SQ[:, 0:nC, c0:416], Z[:, 0:nC, c0:416], AF.Square, ["Z"], ["SQ"])
                    if lat:
                        red(ss3[:, 0:nC, 0], SQ[:, 0:nC, 0:256], ["SQ"], ["ss3"])
                    red(ss3[:, 0:nC, 1], SQ[:, 0:nC, 256:384], ["SQ"], ["ss3"])
                    red(ss3[:, 0:nC, 2], SQ[:, 0:nC, 384:416], ["SQ"], ["ss3"])
                    tt("dve", r3[:], ss3[:], INV3[:], ALU.mult, ["ss3", "INV3"], ["r3"])
                    act(r3[:], r3[:], AF.Sqrt, ["r3"], ["r3"], bias=EPS)
                    recip(r3[:], r3[:], ["r3"], ["r3"])
                    for c in range(nC):
                        if lat:
                            stt(ZQS[:, c, :], Z[:, c, 0:256], r3[:, c, 0:1], GQL[:], ALU.mult, ALU.mult, ["Z", "r3", "GQL"], ["ZQS"])
                        stt(CKS[:, c, :], Z[:, c, 256:384], r3[:, c, 1:2], GKV[:], ALU.mult, ALU.mult, ["Z", "r3", "GKV"], ["CKS"])
                        stt(KR[:, c, :], Z[:, c, 384:416], r3[:, c, 2:3], GK[:, 64:96], ALU.mult, ALU.mult, ["Z", "r3", "GK"], ["KR"])
                    for c in range(nC):
                        items = [(PTZ[:, 2, :], CKS[:, c, :], ident_f[:])]
                        if lat:
                            items += [(PTZ[:, 0, :], ZQS[:, c, 0:128], ident_f[:]), (PTZ[:, 1, :], ZQS[:, c, 128:256], ident_f[:])]
                        trs(items, ["CKS", "ZQS", "ident_f"], ["PTZ"])
                        if lat:
                            cp("act", ZT[:, c, 0:2, :], PTZ[:, 0:2, :], ["PTZ"], [("ZT", c)])
                        cp("dve", ZT[:, c, 2, :], PTZ[:, 2, :], ["PTZ"], [("ZT", c)])
                        if lat:
                            mmg(PQ[:, 0:512], [(ZT[:, c, k, :], wuq[:, k, 0:512]) for k in range(2)], [("ZT", c), "wuq"], ["PQ"])
                            mmg(PQ[:, 512:768], [(ZT[:, c, k, :], wuq[:, k, 512:768]) for k in range(2)], [("ZT", c), "wuq"], ["PQ2"])
                            cp("act", QF[:, c, :], PQ[:, 0:768], ["PQ", "PQ2"], ["QF"])
                        mmg(PK[:], [(ZT[:, c, 2, :], wukv[:, 0:512])], [("ZT", c), "wukv"], ["PK"])
                        mmg(PV[:], [(ZT[:, c, 2, :], wukv[:, 512:1024])], [("ZT", c), "wukv2"], ["PV"])
                        cp("dve", KF[:, c, :], PK[:], ["PK"], ["KF"])
                        cp("act", VTM[:, c, :, 0:64], PV[:].rearrange("p (h d) -> p h d", d=64), ["PV"], ["VTM"])
                    QF4 = QF[:, 0:nC, :].rearrange("p c (h d) -> p c h d", d=96)
                    SQ4 = SQ[:, 0:nC, :].rearrange("p c (h d) -> p c h d", d=96)
                    KF4 = KF[:, 0:nC, :].rearrange("p c (h d) -> p c h d", d=64)
                    SQK4 = SQ[:, 0:nC, 0:512].rearrange("p c (h d) -> p c h d", d=64)
                    if lat:
                        act(SQ[:, 0:nC, :], QF[:, 0:nC, :], AF.Square, ["QF"], ["SQ"])
                        red(ss24[:, 0:nC, 0:8], SQ4[:, :, :, 0:64], ["SQ"], ["ss24"])
                        red(ss24[:, 0:nC, 8:16], SQ4[:, :, :, 64:96], ["SQ"], ["ss24"])
                    act(SQ[:, 0:nC, 0:512], KF[:, 0:nC, :], AF.Square, ["KF"], ["SQ"])
                    red(ss24[:, 0:nC, 16:24], SQK4, ["SQ"], ["ss24"])
                    tt("dve", r24[:], ss24[:], INV24[:], ALU.mult, ["ss24", "INV24"], ["r24"])
                    act(r24[:], r24[:], AF.Sqrt, ["r24"], ["r24"], bias=EPS)
                    recip(r24[:], r24[:], ["r24"], ["r24"])
                    QTM4 = QTM[:, 0:nC]
                    KTM4 = KTM[:, 0:nC]
                    if lat:
                        tt("dve", QF4[:, :, :, 0:64], QF4[:, :, :, 0:64], r24[:, 0:nC, 0:8].unsqueeze(3).to_broadcast([128, nC, 8, 64]), ALU.mult, ["QF", "r24"], ["QF"])
                        tt("dve", QF4[:, :, :, 64:96], QF4[:, :, :, 64:96], r24[:, 0:nC, 8:16].unsqueeze(3).to_broadcast([128, nC, 8, 32]), ALU.mult, ["QF", "r24"], ["QF"])
                        tt("dve", QTM4[:, :, :, 0:64], QF4[:, :, :, 0:64], GQ[:, 0:64].unsqueeze(1).unsqueeze(1).to_broadcast([128, nC, 8, 64]), ALU.mult, ["QF", "GQ"], ["QTM"])
                        tt("dve", QF4[:, :, :, 64:96], QF4[:, :, :, 64:96], GQ[:, 64:96].unsqueeze(1).unsqueeze(1).to_broadcast([128, nC, 8, 32]), ALU.mult, ["QF", "GQ"], ["QF"])
                    tt("dve", KF4, KF4, r24[:, 0:nC, 16:24].unsqueeze(3).to_broadcast([128, nC, 8, 64]), ALU.mult, ["KF", "r24"], ["KF"])
                    tt("dve", KTM4[:, :, :, 0:64], KF4, GK[:, 0:64].unsqueeze(1).unsqueeze(1).to_broadcast([128, nC, 8, 64]), ALU.mult, ["KF", "GK"], ["KTM"])
                    if lat:
                        Cq = COS[:, cg:cg + nC, :].unsqueeze(2).to_broadcast([128, nC, 8, 16])
                        Sq = SIN[:, cg:cg + nC, :].unsqueeze(2).to_broadcast([128, nC, 8, 16])
                        x1, x2 = QF4[:, :, :, 64:80], QF4[:, :, :, 80:96]
                        tt("dve", T1[:, 0:nC], x1, Cq, ALU.mult, ["QF", "COS"], ["T1"])
                        tt("dve", T2[:, 0:nC], x2, Sq, ALU.mult, ["QF", "SIN"], ["T2"])
                        tt("dve", QTM4[:, :, :, 64:80], T1[:, 0:nC], T2[:, 0:nC], ALU.subtract, ["T1", "T2"], ["QTM"])
                        tt("dve", T1[:, 0:nC], x1, Sq, ALU.mult, ["QF", "SIN"], ["T1"])
                        tt("dve", T2[:, 0:nC], x2, Cq, ALU.mult, ["QF", "COS"], ["T2"])
                        tt("dve", QTM4[:, :, :, 80:96], T1[:, 0:nC], T2[:, 0:nC], ALU.add, ["T1", "T2"], ["QTM"])
                        Ck = COS[:, cg:cg + nC, :]
                        Sk = SIN[:, cg:cg + nC, :]
                        k1, k2 = KR[:, 0:nC, 0:16], KR[:, 0:nC, 16:32]
                        tt("dve", T3[:, 0:nC], k1, Ck, ALU.mult, ["KR", "COS"], ["T3"])
                        tt("dve", T4[:, 0:nC], k2, Sk, ALU.mult, ["KR", "SIN"], ["T4"])
                        tt("dve", KR2[:, 0:nC, 0:16], T3[:, 0:nC], T4[:, 0:nC], ALU.subtract, ["T3", "T4"], ["KR2"])
                        tt("dve", T3[:, 0:nC], k1, Sk, ALU.mult, ["KR", "SIN"], ["T3"])
                        tt("dve", T4[:, 0:nC], k2, Ck, ALU.mult, ["KR", "COS"], ["T4"])
                        tt("dve", KR2[:, 0:nC, 16:32], T3[:, 0:nC], T4[:, 0:nC], ALU.add, ["T3", "T4"], ["KR2"])
                        krs = KR2
                    else:
                        krs = KR
                    cp("dve", KTM4[:, :, :, 64:96], krs[:, 0:nC, :].unsqueeze(2).to_broadcast([128, nC, 8, 32]), ["KR", "KR2"], ["KTM"])
                    for c in range(nC):
                        if lat:
                            trs([(PTQ[:, h, :], QTM[:, c, h, :], ident_b[:]) for h in range(8)], ["QTM", "ident_b"], ["PTQ"])
                            cp("act", QST[:, :, c * 128:(c + 1) * 128], PTQ[:], ["PTQ"], ["QST"])
                        trs([(PTK[:, h, :], KTM[:, c, h, :], ident_b[:]) for h in range(8)], ["KTM", "ident_b"], ["PTK"])
                        cp("dve", KST[:, :, c * 128:(c + 1) * 128], PTK[:], ["PTK"], ["KST"])
                    if lat:
                        dma("sp", QT_v[:, :, t0 - LC:t0 - LC + 512], QST[:], ["QST"], [], "qst")
                    dma("sp", KT_v[:, :, t0:t0 + nC * 128], KST[:, :, 0:nC * 128], ["KST"], [], "kst")
                    dma("sp", V_v[:, t0 // 128:t0 // 128 + nC, :], VTM[:, 0:nC].rearrange("p c h d -> p c (h d)"), ["VTM"], [], "vst")
                S.flush()
        if limit <= 3:
            return nc

        with ExitStack() as ph:
            GZs = [sb(ph, f"GZs{i}", [128, 1024], F32) for i in range(2)]
            XRH = [sb(ph, f"XRH{i}", [128, T], F32) for i in range(2)]
            XRB = [sb(ph, f"XRB{i}", [128, T], BF16) for i in range(2)]
            BB = [[sb(ph, f"B{j}_{d}", [128, T], F32) for j in range(3)] for d in range(2)]
            HF = sb(ph, "HF", [128, L], F32)
            HBC = [sb(ph, f"HBC{d}", [128, LC], F32) for d in range(2)]
            RNNB = [sb(ph, f"RNNB{i}", [128, 1024], BF16) for i in range(2)]
            rgw = [sb(ph, f"rgw{i}", [128, 2, 2, 128], BF16) for i in range(2)]
            pa = [ps(ph, f"pa{i}", [128, 512], F32) for i in range(2)]
            px = [ps(ph, f"px{i}", [128, 512], F32) for i in range(2)]
            NB = 9

            def load_n(n):
                s = n % 2
                for d in range(2):
                    dma("pool", rgw[s][:, d, 0, :], rgwa_d[d, n], [], [("rgw", s, d, 0)], f"rgw{s}")
                    dma("pool", rgw[s][:, d, 1, :], rgwx_d[d, n], [], [("rgw", s, d, 1)], f"rgw{s}")
                dma("sp", XRH[s][:], z0_d[n], [], [("XRH", s)], f"ldx{s}")
                ts("pool", XRB[s][:], XRH[s][:], 2.0, 0.0, ALU.mult, ALU.add, [("XRH", s)], [("XRB", s)])

            load_n(0)
            gzc = 0
            for n in range(8):
                s = n % 2
                if n + 1 < 8:
                    load_n(n + 1)
                for d in range(2):
                    B1, B2, B3 = BB[d]
                    K = lambda nm: [((nm, d), b) for b in range(NB)]
                    for b in range(NB):
                        lo, hi = blk(b)
                        w = hi - lo
                        mmg(pa[b % 2][:, 0:w], [(rgw[s][:, d, 0, :], XRB[s][:, lo:hi])], [("rgw", s, d, 0), ("XRB", s)], [("pa", b % 2)])
                        mmg(px[b % 2][:, 0:w], [(rgw[s][:, d, 1, :], XRB[s][:, lo:hi])], [("rgw", s, d, 1), ("XRB", s)], [("px", b % 2)])
                        act(B1[:, lo:hi], pa[b % 2][:, 0:w], AF.Tanh, [("pa", b % 2)], [(("B1", d), b)], scale=0.5, bias=HBA[:, d, n:n + 1])
                        act(B2[:, lo:hi], B1[:, lo:hi], AF.Exp, [(("B1", d), b)], [(("B2", d), b)], scale=HC[:, d, n:n + 1], bias=HC[:, d, n:n + 1])
                        act(B3[:, lo:hi], B1[:, lo:hi], AF.Exp, [(("B1", d), b)], [(("B3", d), b)], scale=CN[:, d, n:n + 1], bias=CN[:, d, n:n + 1])
                        act(B1[:, lo:hi], px[b % 2][:, 0:w], AF.Tanh, [("px", b % 2)], [(("B1", d), b)], scale=0.5, bias=HBX[:, d, n:n + 1])
                    act(B3[:], B3[:], AF.Sqrt, K("B3"), K("B3"), scale=-1.0, bias=1.0)
                for d in range(2):
                    B1, B2, B3 = BB[d]
                    K = lambda nm: [((nm, d), b) for b in range(NB)]
                    stt(B1[:], B1[:], 1.0, B3[:], ALU.add, ALU.mult, K("B1") + K("B3"), K("B1"))
                    tt("dve", B1[:], B1[:], XRH[s][:], ALU.mult, K("B1") + [("XRH", s)], K("B1"))
                    if d == 0:
                        S.op("dve", lambda e, B1=B1, B2=B2: e.tensor_tensor_scan(HBC[0][:], B2[:, 0:LC], B1[:, 0:LC], 0.0, ALU.mult, ALU.add),
                             K("B1") + K("B2"), ["HBC0"])
                        S.op("dve", lambda e, B1=B1, B2=B2: e.tensor_tensor_scan(HF[:], B2[:, LC:T], B1[:, LC:T], HBC[0][:, LC - 1:LC], ALU.mult, ALU.add),
                             K("B1") + K("B2") + ["HBC0"], ["HF"])
                    else:
                        S.op("dve", lambda e, B1=B1, B2=B2: e.tensor_tensor_scan(HBC[1][:, ::-1], B2[:, 0:LC][:, ::-1], B1[:, 0:LC][:, ::-1], 0.0, ALU.mult, ALU.add),
                             K("B1") + K("B2"), ["HBC1"])
                        S.op("dve", lambda e, B1=B1, B2=B2, B3=B3: e.tensor_tensor_scan(B3[:, LC:T][:, ::-1], B2[:, LC:T][:, ::-1], B1[:, LC:T][:, ::-1], HBC[1][:, 0:1], ALU.mult, ALU.add),
                             K("B1") + K("B2") + K("B3") + ["HBC1"], K("B3"))
                        for sg in range(4):
                            lo = LC + sg * 1024
                            l0 = sg * 1024
                            gq = gzc % 2
                            gzc += 1
                            dma("sp", GZs[gq][:], w2_d[n, :, l0:l0 + 1024], [], [("GZs", gq)], f"ldgz{gq}")
                            kb = [(("B3", d), 1 + 2 * sg), (("B3", d), 2 + 2 * sg)]
                            tt("dve", B3[:, lo:lo + 1024], B3[:, lo:lo + 1024], HF[:, l0:l0 + 1024], ALU.add, K("B3") + ["HF"], kb)
                            stt(RNNB[sg % 2][:], B3[:, lo:lo + 1024], 0.5, GZs[gq][:], ALU.mult, ALU.mult, kb + [("GZs", gq)], [("RNNB", sg % 2)])
                            dma("sp", rnnT_d[n, :, l0:l0 + 1024], RNNB[sg % 2][:], [("RNNB", sg % 2)], [], f"rnn{sg % 2}")
            S.flush()
        if limit <= 4:
            return nc

        with ExitStack() as attnstack:
            attnTM = sb(attnstack, "attnTM", [128, 32, 512], BF16)
            with ExitStack() as ph:
                VT = sb(ph, "VT", [128, 34, 520], BF16)
                KTh = [sb(ph, f"KTh{i}", [96, T], BF16) for i in range(2)]
                QTh = [sb(ph, f"QTh{i}", [96, L], BF16) for i in range(2)]
                PT = [sb(ph, f"PT{i}", [128, 512], BF16) for i in range(4)]
                rec = [sb(ph, f"rec{i}", [128, 4], F32) for i in range(2)]
                Sb = [ps(ph, f"Sb{i}", [128, 512], F32) for i in range(4)]
                Ob = [ps(ph, f"Ob{i}", [128, 4, 65], F32) for i in range(2)]
                dma("sp", VT[:], V_d.rearrange("(c p) f -> p c f", p=128), [], ["VT"], "ldv")
                steps = [(h, qb, kc) for h in range(8) for qb in range(8) for kc in range(34)]
                LA = 3

                def score(i):
                    h, qb, kc = steps[i]
                    s = h % 2
                    mmg(Sb[i % 4][:], [(KTh[s][:, kc * 128:(kc + 1) * 128], QTh[s][:, qb * 512:(qb + 1) * 512])],
                        [("KQ", s)], [("Sb", i % 4)])

                for h in range(8):
                    s = h % 2
                    pass
                loaded = set()

                def load_head(h):
                    if h in loaded or h >= 8:
                        return
                    loaded.add(h)
                    s = h % 2
                    dma("sp", KTh[s][:], KT_d[h], [], [("KQ", s)], f"kq{s}")
                    dma("sp", QTh[s][:], QT_d[h], [], [("KQ", s)], f"kq{s}")

                load_head(0)
                for i in range(LA):
                    score(i)
                for i, (h, qb, kc) in enumerate(steps):
                    if qb == 0 and kc == 0:
                        load_head(h + 1)
                    if i + LA < len(steps):
                        if steps[i + LA][0] not in loaded:
                            load_head(steps[i + LA][0])
                        score(i + LA)
                    o = (h * 8 + qb) % 2
                    act(PT[i % 4][:], Sb[i % 4][:], AF.Exp, [("Sb", i % 4)], [("PT", i % 4)])

                    def pv(e, i=i, h=h, kc=kc, o=o):
                        ins = None
                        for qc in range(4):
                            ins = e.matmul(Ob[o][:, qc, :], lhsT=PT[i % 4][:, qc * 128:(qc + 1) * 128],
                                           rhs=VT[:, kc, h * 65:(h + 1) * 65],
                                           start=(kc == 0 and qc == 0), stop=(kc == 33 and qc == 3), skip_group_check=True)
                        return ins
                    S.op("pe", pv, [("PT", i % 4), "VT"], [("Ob", o)])
                    if kc == 33:
                        recip(rec[o][:], Ob[o][:, :, 64], [("Ob", o)], [("rec", o)])
                        tt("dve", attnTM[:, qb * 4:(qb + 1) * 4, h * 64:(h + 1) * 64], Ob[o][:, :, 0:64],
                           rec[o][:].unsqueeze(2).to_broadcast([128, 4, 64]), ALU.mult, [("Ob", o), ("rec", o)], [("attn", qb)])
                if debug:
                    dma("sp", attn_dbg, attnTM[:], [("attn", qb) for qb in range(8)], [], "dbg")
                S.flush()
            if limit <= 5:
                return nc

            S.op("pool", lambda e: e.iota(TOKID[:], pattern=[[128, 32]], base=0, channel_multiplier=1), [], ["TOKID"])
            with ExitStack() as ph:
                WR = sb(ph, "WR", [128, 8, D], BF16)
                WM = sb(ph, "WM", [128, 4, D], BF16)
                WO = sb(ph, "WO", [128, 8, D], BF16)
                bcE = sb(ph, "bcE", [128, 3072], F32)
                wr32 = sb(ph, "wr32", [128, 8, NE], F32)
                RT = [sb(ph, f"RT{i}", [128, 8, 512], BF16) for i in range(2)]
                GT = [sb(ph, f"GT{i}", [128, 2, 512], BF16) for i in range(2)]
                XB = [sb(ph, f"XB{i}", [128, 4, D], F32) for i in range(2)]
                AT = sb(ph, "AT", [128, 4, 512], BF16)
                MT = sb(ph, "MT", [128, 8, 512], BF16)
                tA = [sb(ph, f"tA{i}", [128, 512], F32) for i in range(2)]
                tB = [sb(ph, f"tB{i}", [128, 512], F32) for i in range(2)]
                tC = [sb(ph, f"tC{i}", [128, 512], F32) for i in range(2)]
                H2T = [sb(ph, f"H2T{i}", [128, 8, 128], F32) for i in range(2)]
                ROWS = [sb(ph, "ROWS0", [128, 4, ROWW], BF16)] * 2
                junkE = sb(ph, "junkE", [128, D], BF16)
                ssE = sb(ph, "ssE", [128, 4], F32)
                rsE = sb(ph, "rsE", [128, 4], F32)
                mxE = sb(ph, "mxE", [128, 4], F32)
                smE = sb(ph, "smE", [128, 4], F32)
                EX = sb(ph, "EX", [128, 4, NE], F32)
                P1 = [ps(ph, f"P1{i}", [128, 512], F32) for i in range(2)]
                P2 = [ps(ph, f"P2{i}", [128, 512], F32) for i in range(2)]
                P3 = [ps(ph, f"P3{i}", [128, 512], F32) for i in range(2)]
                PTA = ps(ph, "PTA", [128, 4, 128], BF16)
                PTH = ps(ph, "PTH", [128, 512], F32)
                dma("pool", WR[:], wrnn_d.rearrange("(k p) n -> p k n", p=128), [], ["WR"], "lde")
                dma("pool", WM[:], wmla_d.rearrange("(k p) n -> p k n", p=128), [], ["WM"], "lde")
                dma("pool", WO[:], wo_d.rearrange("(k p) n -> p k n", p=128), [], ["WO"], "lde")
                dma("sp", bcE[:], bc_d[:, 0:3072], [], ["bcE"], "lde2")
                dma("sp", wr32[:], wr_d.rearrange("(k p) n -> p k n", p=128), [], ["wr32"], "lde2")
                m2bc, m3bc, a2bc = bcE[:, 0:1024], bcE[:, 1024:2048], bcE[:, 2048:3072]
                rn_v = rnnT_d.rearrange("n p t -> p n t")
                G_v = G_d.rearrange("j p t -> p j t")
                x_v = x_d.rearrange("(g c p) d -> g p c d", c=4, p=128)
                o_v = out_d.rearrange("(g c p) d -> g p c d", c=4, p=128)
                rows_v = rows_d.rearrange("(g c p) f -> g p c f", c=4, p=128)

                def load_rt(bi):
                    if bi >= 8:
                        return
                    s = bi % 2
                    dma("sp", RT[s][:], rn_v[:, :, bi * 512:(bi + 1) * 512], [], [("RT", s)], f"ldb{s}")

                def load_xb(bi):
                    if bi >= 8:
                        return
                    s = bi % 2
                    dma("pool", XB[s][:], x_v[bi], [], [("XB", s)] + [("XB", s, c, hf) for c in range(4) for hf in range(2)] + [("H2", s, c) for c in range(4)], f"ldxb{s}")

                def front(bi):
                    s = bi % 2
                    load_rt(bi + 1)
                    for c in range(4):
                        trs([(PTA[:, kk, :], attnTM[:, bi * 4 + c, kk * 128:(kk + 1) * 128], ident_b[:]) for kk in range(4)],
                            ["ident_b"], ["PTA"])
                        cp("dve", AT[:, :, c * 128:(c + 1) * 128], PTA[:], ["PTA"], ["AT"])
                    for j in range(8):
                        q = j % 2
                        dma("sp", GT[q][:], G_d.rearrange("(a j) p t -> j p a t", a=2)[j][:, :, bi * 512:(bi + 1) * 512], [], [("GT", q)], f"ldg{q}")
                        mmg(P1[q][:], [(WR[:, k, j * 128:(j + 1) * 128], RT[s][:, k, :]) for k in range(8)], ["WR", ("RT", s)], [("P1", q)])
                        mmg(P2[q][:], [(WM[:, k, j * 128:(j + 1) * 128], AT[:, k, :]) for k in range(4)], ["WM", "AT"], [("P2", q)])
                        tt("dve", tA[q][:], P1[q][:], GT[q][:, 0, :], ALU.mult, [("P1", q), ("GT", q)], [("tA", q)])
                        tt("dve", tB[q][:], P2[q][:], GT[q][:, 1, :], ALU.mult, [("P2", q), ("GT", q)], [("tB", q)])
                        tt("dve", MT[:, j, :], tA[q][:], tB[q][:], ALU.add, [("tA", q), ("tB", q)], [("MT", j)])
                    for c in range(4):
                        for hf in range(2):
                            q = (c * 2 + hf) % 2
                            mmg(P3[q][:], [(MT[:, k, c * 128:(c + 1) * 128], WO[:, k, hf * 512:(hf + 1) * 512]) for k in range(8)],
                                ["WO"] + [("MT", k) for k in range(8)], [("P3", q)])
                            tt("dve", tC[q][:], P3[q][:], m2bc[:, hf * 512:(hf + 1) * 512], ALU.mult, [("P3", q), "bcE"], [("tC", q)])
                            tt("dve", XB[s][:, c, hf * 512:(hf + 1) * 512], XB[s][:, c, hf * 512:(hf + 1) * 512], tC[q][:], ALU.add,
                               [("XB", s), ("tC", q)], [("XB", s, c, hf)])
                    xk = [("XB", s, c, hf) for c in range(4) for hf in range(2)]
                    dma("sp", o_v[bi], XB[s][:], xk, [], f"ost{s}")

                def tail(bi):
                    s = bi % 2
                    xk = [("XB", s, c, hf) for c in range(4) for hf in range(2)]
                    mset("pool", ssE[:], 1.0, ["ssE"])
                    for c in range(4):
                        act(junkE[:], XB[s][:, c, :], AF.Square, xk, ["junkE", "ssE"], accum_out=ssE[:, c:c + 1])
                    ts("dve", rsE[:], ssE[:], 1.0 / D, EPS, ALU.mult, ALU.add, ["ssE"], ["rsE"])
                    act(rsE[:], rsE[:], AF.Sqrt, ["rsE"], ["rsE"])
                    recip(rsE[:], rsE[:], ["rsE"], ["rsE"])
                    H2F = XB[s]
                    PLv = P1[0][:, 0:64].rearrange("p (c e) -> p c e", e=NE)
                    for c in range(4):
                        stt(H2F[:, c, :], XB[s][:, c, :], rsE[:, c:c + 1], a2bc, ALU.mult, ALU.mult, xk + ["rsE", "bcE"], [("H2", s, c)])
                        tt("dve", H2F[:, c, :], H2F[:, c, :], m3bc, ALU.add, [("H2", s, c), "bcE"], [("H2", s, c)])
                        cp("act", ROWS[s][:, c, 0:1024], H2F[:, c, :], [("H2", s, c)], [("ROWS", s)])
                        hq = c % 2
                        for kh in range(2):
                            trs([(P3[kh][:, kk * 128:(kk + 1) * 128], H2F[:, c, (kh * 4 + kk) * 128:(kh * 4 + kk + 1) * 128], ident_f[:]) for kk in range(4)],
                                [("H2", s, c), "ident_f"], [("P3", kh)])
                            cp("act" if kh else "dve", H2T[hq][:, kh * 4:(kh + 1) * 4, :], P3[kh][:].rearrange("p (k t) -> p k t", t=128), [("P3", kh)], [("H2T", hq, kh)])
                        mmg(PLv[:, c, :], [(H2T[hq][:, k, :], wr32[:, k, :]) for k in range(8)],
                            [("H2T", hq, 0), ("H2T", hq, 1), "wr32"], [("P1", 0)])
                    red(mxE[:], PLv, [("P1", 0)], ["mxE"], op=ALU.max)
                    ts("dve", mxE[:], mxE[:], -1.0, 0.0, ALU.mult, ALU.add, ["mxE"], ["mxE"])
                    for c in range(4):
                        act(EX[:, c, :], PLv[:, c, :], AF.Exp, [("P1", 0), "mxE"], ["EX", "smE"], bias=mxE[:, c:c + 1], accum_out=smE[:, c:c + 1])
                    recip(smE[:], smE[:], ["smE"], ["smE"])
                    tt("dve", AFF[:, bi * 4:(bi + 1) * 4, :], EX[:], smE[:].unsqueeze(2).to_broadcast([128, 4, NE]), ALU.mult, ["EX", "smE"], ["AFF"])
                    cp("dve", ROWS[s][:, :, 1024:1026].bitcast(I32), TOKID[:, bi * 4:(bi + 1) * 4].unsqueeze(2), ["TOKID"], [("ROWS", s)])
                    cp("dve", ROWS[s][:, :, 1026:1058].bitcast(F32), AFF[:, bi * 4:(bi + 1) * 4, :], ["AFF"], [("ROWS", s)])
                    dma("sp", rows_v[bi], ROWS[s][:], [("ROWS", s)], [], f"rst{s}")

                load_rt(0)
                load_xb(0)
                load_xb(1)
                front(0)
                for bi in range(1, 8):
                    front(bi)
                    tail(bi - 1)
                    load_xb(bi + 1)
                tail(7)
                if debug:
                    dma("sp", aff_dbg, AFF[:], ["AFF"], [], "dbg")
                S.flush()
        if limit <= 6:
            return nc

        gstack = es.enter_context(ExitStack())
        ROWSG = sb(gstack, "ROWSG", [128, 32, ROWW], BF16)
        WG = [sb(gstack, f"WG{i}", [128, 8, D], BF16) for i in range(2)]
        WU = [sb(gstack, f"WU{i}", [128, 8, D], BF16) for i in range(2)]
        WD = sb(gstack, "WD", [128, 8, D], BF16)
        rows_g = rows_d.rearrange("(g c p) f -> g p c f", c=8, p=128)
        for g in range(4):
            dma("sp", ROWSG[:, g * 8:(g + 1) * 8, :], rows_g[g], [], [("RG", g)], "ldrg")

        def L_gu(e_):
            if e_ >= NE:
                return
            s = e_ % 2
            dma("pool", WG[s][:], weg_d[e_].rearrange("(k p) f -> p k f", p=128), [], [("WG", s)], f"wg_{s}")
            dma("pool", WU[s][:], weu_d[e_].rearrange("(k p) f -> p k f", p=128), [], [("WU", s)], f"wu_{s}")

        def L_d(e_):
            if e_ >= NE:
                return
            dma("pool", WD[:], wed_d[e_].rearrange("(k p) f -> p k f", p=128), [], ["WD"], "wd_")

        L_gu(0)
        L_d(0)
        L_gu(1)
        with ExitStack() as ph:
            LO = sb(ph, "LO", [128, NE], F32)
            HI = sb(ph, "HI", [128, NE], F32)
            MID = sb(ph, "MID", [128, NE], F32)
            D1 = sb(ph, "D1", [128, NE], F32)
            D2 = sb(ph, "D2", [128, NE], F32)
            GE = sb(ph, "GE", [128, NE], F32)
            MASK = sb(ph, "MASK", [128, 32, NE], BF16)
            CNTP = sb(ph, "CNTP", [128, NE], F32)
            ones32 = sb(ph, "ones32", [128, 128], F32)
            ones_b = sb(ph, "ones_b", [128, 128], BF16)
            ones_f = sb(ph, "ones_f", [128, 32], F32)
            LTRI = sb(ph, "LTRI", [128, 128], BF16)
            LTF = sb(ph, "LTF", [128, 128], F32)
            MF = sb(ph, "MF", [128, 32, NE], F32)
            TOT = sb(ph, "TOT", [128, 32, NE], F32)
            CUM = sb(ph, "CUM", [128, 32, NE], F32)
            POS = sb(ph, "POS", [128, 32, NE], F32)
            VAL = sb(ph, "VAL", [128, 32, NE], F32)
            OFI = sb(ph, "OFI", [128, 32, NE], I32)
            OFF = sb(ph, "OFF", [128, 32, NE], F32)
            PC = ps(ph, "PC", [128, NE], F32)
            PP = ps(ph, "PP", [128, 512], F32)
            PTOT = ps(ph, "PTOT", [128, 512], F32)
            mset("pool", ones_b[:], 1.0, ["ones_b"])
            mset("pool", ones32[:], 1.0, ["ones32"])
            mset("pool", ones_f[:], 1.0, ["ones_f"])
            mset("pool", LTF[:], 1.0, ["LTF"])
            S.op("pool", lambda e: e.affine_select(out=LTF[:], in_=LTF[:], pattern=[[1, 128]], compare_op=ALU.is_ge, fill=0.0,
                                                    base=-1, channel_multiplier=-1), ["LTF"], ["LTF"])
            cp("pool", LTRI[:], LTF[:], ["LTF"], ["LTRI"])
            S.op("pool", lambda e: e.iota(OFI[:], pattern=[[0, 32], [CAP, NE]], base=-int(BIG), channel_multiplier=0), [], ["OFI"])
            cp("pool", OFF[:], OFI[:], ["OFI"], ["OFF"])
            mset("dve", LO[:], 0.0, ["LO"])
            mset("dve", HI[:], 1.0, ["HI"])
            mset("dve", MID[:], 0.5, ["MID"])
            for it in range(30):
                tt("dve", MASK[:], AFF[:], MID[:].unsqueeze(1).to_broadcast([128, 32, NE]), ALU.is_gt, ["AFF", "MID"], ["MASK"])
                red(CNTP[:], MASK[:].rearrange("p c e -> p e c"), ["MASK"], ["CNTP"])
                mmg(PC[:], [(ones32[:], CNTP[:])], ["ones32", "CNTP"], ["PC"])
                ts("dve", GE[:], PC[:], CAP - 0.5, 2.0 ** -(it + 1), ALU.is_ge, ALU.mult, ["PC"], ["GE"])
                tt("dve", LO[:], LO[:], GE[:], ALU.add, ["LO", "GE"], ["LO"])
                ts("dve", MID[:], LO[:], 2.0 ** -(it + 2), 0.0, ALU.add, ALU.add, ["LO"], ["MID"])
            tt("dve", MASK[:], AFF[:], LO[:].unsqueeze(1).to_broadcast([128, 32, NE]), ALU.is_gt, ["AFF", "LO"], ["MASK"])
            cp("pool", MF[:], MASK[:], ["MASK"], ["MF"])
            mflat = MASK[:].rearrange("p c e -> p (c e)")
            mmg(PP[:], [(LTRI[:], mflat)], ["LTRI", "MASK"], ["PP"])
            mmg(PTOT[:], [(ones_b[:], mflat)], ["ones_b", "MASK"], ["PTOT"])
            cp("act", TOT[:].rearrange("p c e -> p (c e)"), PTOT[:], ["PTOT"], ["TOT"])
            for e_ in range(NE):
                S.op("dve", lambda e, e_=e_: e.tensor_tensor_scan(CUM[:, :, e_], ones_f[:], TOT[:, :, e_], 0.0, ALU.mult, ALU.add),
                     ["ones_f", "TOT"], [("CUM", e_)])
            ck = [("CUM", e_) for e_ in range(NE)]
            tt("dve", CUM[:], CUM[:], TOT[:], ALU.subtract, ck + ["TOT"], ["CUMX"])
            tt("dve", POS[:].rearrange("p c e -> p (c e)"), PP[:], CUM[:].rearrange("p c e -> p (c e)"), ALU.add, ["PP", "CUMX"], ["POS"])
            ts("dve", VAL[:], POS[:], float(CAP) - 0.5, 0.0, ALU.is_lt, ALU.add, ["POS"], ["VAL"])
            tt("dve", VAL[:], VAL[:], MF[:], ALU.mult, ["VAL", "MF"], ["VAL"])
            tt("dve", POS[:], POS[:], OFF[:], ALU.add, ["POS", "OFF"], ["POS"])
            tt("dve", POS[:], POS[:], VAL[:], ALU.mult, ["POS", "VAL"], ["POS"])
            ts("dve", POS[:], POS[:], BIG, 0.0, ALU.add, ALU.add, ["POS"], ["POS"])
            cp("dve", IDX[:], POS[:], ["POS"], ["IDX"])
            if debug:
                dma("sp", idx_dbg, IDX[:], ["IDX"], [], "dbg")
            S.flush()
        if limit <= 7:
            return nc

        with ExitStack() as ph:
            XG = sb(ph, "XG", [128, 4, ROWW], BF16)
            TI = [sb(ph, f"TI{i}", [128, 4], I32) for i in range(2)]
            GA = [sb(ph, f"GA{i}", [128, 4], F32) for i in range(2)]
            XGT = sb(ph, "XGT", [128, 8, 512], BF16)
            HID = sb(ph, "HID", [128, 8, 512], BF16)
            SGT = [sb(ph, f"SGT{i}", [128, 512], F32) for i in range(2)]
            YO = sb(ph, "YO", [128, 4, D], F32)
            m5 = sb(ph, "m5", [128, D], F32)
            PTX = [ps(ph, f"PTX{i}", [128, 4, 128], BF16) for i in range(2)]
            PG = [ps(ph, f"PG{i}", [128, 512], F32) for i in range(2)]
            PU = [ps(ph, f"PU{i}", [128, 512], F32) for i in range(2)]
            PY = [ps(ph, f"PY{i}", [128, 512], F32) for i in range(2)]
            dma("sp", m5[:], bc_d[:, 3072:4096], [], ["m5"], "ldg")
            def scat(e_):
                if e_ >= NE:
                    return
                for ci in range(32):
                    S.op("pool", lambda e, ci=ci, e_=e_: e.indirect_dma_start(
                        out=xg_d, out_offset=bass.IndirectOffsetOnAxis(ap=IDX[:, ci, e_:e_ + 1], axis=0),
                        in_=ROWSG[:, ci, :], in_offset=None, bounds_check=R["bc_xg"], oob_is_err=False),
                        [("RG", ci // 8)], [("xg", e_, ci)], dma=f"sc{e_}")

            scat(0)
            scat(1)
            scat(2)
            xg_v = xg_d.rearrange("(e g p) f -> e p g f", g=4, p=128)
            for e_ in range(NE):
                s = e_ % 2
                dma("sp", XG[:], xg_v[e_], [("xg", e_, ci) for ci in range(32)], ["XG"], "xgl")
                cp("dve", TI[s][:].unsqueeze(2), XG[:, :, 1024:1026].bitcast(I32), ["XG"], [("TI", s)])
                cp("dve", GA[s][:].unsqueeze(2), XG[:, :, 1026 + 2 * e_:1028 + 2 * e_].bitcast(F32), ["XG"], [("GA", s)])
                for k in range(8):
                    trs([(PTX[k % 2][:, g, :], XG[:, g, k * 128:(k + 1) * 128], ident_b[:]) for g in range(4)],
                        ["XG", "ident_b"], [("PTX", k % 2)])
                    cp("act" if k % 2 else "dve", XGT[:, k, :], PTX[k % 2][:].rearrange("p g t -> p (g t)"), [("PTX", k % 2)], [("XGT", k)])
                xk = [("XGT", k) for k in range(8)]
                for f in range(8):
                    q = f % 2
                    mmg(PG[q][:], [(WG[s][:, k, f * 128:(f + 1) * 128], XGT[:, k, :]) for k in range(8)], xk + [("WG", s)], [("PG", q)])
                    mmg(PU[q][:], [(WU[s][:, k, f * 128:(f + 1) * 128], XGT[:, k, :]) for k in range(8)], xk + [("WU", s)], [("PU", q)])
                    act(SGT[q][:], PG[q][:], AF.Silu, [("PG", q)], [("SGT", q)])
                    tt("dve", HID[:, f, :], SGT[q][:], PU[q][:], ALU.mult, [("SGT", q), ("PU", q)], [("HID", f)])
                L_gu(e_ + 2)
                hk = [("HID", f) for f in range(8)]
                for g in range(4):
                    for hf in range(2):
                        q = (g * 2 + hf) % 2
                        mmg(PY[q][:], [(HID[:, f, g * 128:(g + 1) * 128], WD[:, f, hf * 512:(hf + 1) * 512]) for f in range(8)],
                            hk + ["WD"], [("PY", q)])
                        stt(YO[:, g, hf * 512:(hf + 1) * 512], PY[q][:], GA[s][:, g:g + 1], m5[:, hf * 512:(hf + 1) * 512], ALU.mult, ALU.mult,
                            [("PY", q), ("GA", s), "m5"], [("YO", g)])
                    if g == 3:
                        L_d(e_ + 1)
                    S.op("pool", lambda e, s=s, g=g: e.indirect_dma_start(
                        out=out_d, out_offset=bass.IndirectOffsetOnAxis(ap=TI[s][:, g:g + 1], axis=0),
                        in_=YO[:, g, :], in_offset=None, bounds_check=R["bc_out"], oob_is_err=True, compute_op=ALU.add),
                        [("YO", g), ("TI", s)] + [("outd", gg) for gg in range(4) if gg != g], [("outd", g)], dma="sadd")
                scat(e_ + 3)
            S.flush()
    return nc


_CACHE = {}


def _rope_tables():
    rows = L // 64
    row = np.repeat(np.arange(rows, dtype=np.float32), 64)
    col = np.tile(np.arange(64, dtype=np.float32), rows)
    inv = (np.float32(10000.0) ** (-np.arange(8, dtype=np.float32) / np.float32(8))).astype(np.float32)
    ang = np.concatenate([row[:, None] * inv, col[:, None] * inv], axis=-1).astype(np.float32)
    return np.cos(ang).astype(np.float32), np.sin(ang).astype(np.float32)


def _fm(v):
    return np.ascontiguousarray(np.moveaxis(v.reshape(*v.shape[:-1], 8, 128), -1, 0))


def make_in_maps(inp):
    f = lambda a: np.ascontiguousarray(np.asarray(a, dtype=np.float32))
    cos, sin = _rope_tables()
    cosT = np.ascontiguousarray(cos.reshape(32, 128, 16).transpose(1, 0, 2))
    sinT = np.ascontiguousarray(sin.reshape(32, 128, 16).transpose(1, 0, 2))
    c_ctx = f(inp["c_ctx"])
    shared = dict(
        w_ada=f(inp["w_ada"][0]), b_ada2=np.ascontiguousarray(np.repeat(f(inp["b_ada"]), 2, axis=0)),
        g1T=_fm(f(inp["g_norm1"][0])), g2row=np.ascontiguousarray(np.repeat(f(inp["g_norm2"]), 2, axis=0)),
        w_in=f(inp["w_in"][0]),
        cwT=np.ascontiguousarray(_fm(f(inp["conv_w"][0])).transpose(0, 2, 1)),
        cbT=_fm(f(inp["conv_b"][0])),
        rg_wa=f(inp["rg_wa"][0]), rg_wx=f(inp["rg_wx"][0]),
        rgbT=np.ascontiguousarray(np.stack([_fm(f(inp["rg_ba"][0])), _fm(f(inp["rg_bx"][0])), _fm(f(inp["rg_lambda"][0]))], axis=1)),
        w_rnn_out=f(inp["w_rnn_out"][0]), w_mla_out=f(inp["w_mla_out"][0]), w_o=f(inp["w_o"][0]),
        w_uq=f(inp["w_uq"][0]), gql256=np.ascontiguousarray(np.tile(f(inp["g_q_lora"][0])[None, :], (128, 1))),
        w_uk=f(inp["w_uk"][0]), w_uv=f(inp["w_uv"][0]), gkv128=np.ascontiguousarray(np.tile(f(inp["g_kv_lora"][0])[None, :], (128, 1))),
        gq96=np.ascontiguousarray(np.tile(np.concatenate([f(inp["g_q_nope"][0]), f(inp["g_q_rope"][0])])[None, :], (128, 1))),
        gk96=np.ascontiguousarray(np.tile(np.concatenate([f(inp["g_k_nope"][0]), f(inp["g_k_rope"][0])])[None, :], (128, 1))),
        cosT=cosT, sinT=sinT, w_router=f(inp["w_router"][0]),
        w_e_gate=f(inp["w_e_gate"][0]), w_e_up=f(inp["w_e_up"][0]), w_e_down=f(inp["w_e_down"][0]),
    )
    maps = []
    for b in range(8):
        c2 = np.stack([f(inp["c"][b]), c_ctx], axis=0)
        m = dict(shared)
        m["x"] = f(inp["x"][b])
        m["ctx"] = f(inp["ctx"][b])
        m["c2T"] = np.ascontiguousarray(c2.reshape(2, 8, 128).transpose(2, 1, 0))
        maps.append(m)
    return maps


def kernel(**inputs):
    if "nc" not in _CACHE:
        _CACHE["nc"] = build_program()
    nc = _CACHE["nc"]
    in_maps = make_in_maps(inputs)
    res = run_bass_kernel_spmd(nc, in_maps, core_ids=list(range(8)))
    return np.stack([np.asarray(r["out"], dtype=np.float32) for r in res.results], axis=0)
```

```python
import bisect
from contextlib import ExitStack

import numpy as np
import concourse.bass as bass
import concourse.mybir as mybir
from concourse.bass_utils import run_bass_kernel_spmd

F32 = mybir.dt.float32
BF16 = mybir.dt.bfloat16
I32 = mybir.dt.int32
AF = mybir.ActivationFunctionType
ALU = mybir.AluOpType
AX = mybir.AxisListType

ENGS = ("pe", "act", "dve", "pool", "sp")
EPOCH = 30000

L = 4096
LC = 256
T = L + LC
D = 1024
NE = 16
CAP = 512
ROWW = 1058
BIG = float(1 << 20)
EPS = 1e-6


class Sched:
    def __init__(self, nc, es):
        self.nc = nc
        self.es = es
        self.ops = []
        self.lastw = {}
        self.readers = {}
        self.last_eng = {}
        self.dma_ops = {}
        self.setups = {}
        self.emitted = 0
        self.cnt = {e: 0 for e in ENGS}
        self.esems = {e: [] for e in ENGS}
        self.dsems = {}
        self.known = {}
        self.setup_done = {}

    def setup(self, eng, fn):
        self.setups.setdefault(eng, []).append(fn)

    def op(self, eng, fn, reads=(), writes=(), dma=None):
        i = len(self.ops)
        deps = {}
        for r in reads:
            w = self.lastw.get(r)
            if w is not None:
                deps[w] = True
        for r in writes:
            w = self.lastw.get(r)
            if w is not None and not deps.get(w):
                deps[w] = 2
            for j in self.readers.get(r, {}).values():
                deps.setdefault(j, False)
        self.ops.append(dict(eng=eng, fn=fn, deps=deps, dma=dma, signal=False))
        slot = ("d", dma) if dma is not None else ("e", eng)
        for r in reads:
            self.readers.setdefault(r, {})[slot] = i
        for r in writes:
            self.lastw[r] = i
            self.readers[r] = {}
        if dma is not None:
            self.dma_ops.setdefault(dma, []).append(i)
        else:
            self.last_eng[eng] = i
        return i

    def barrier(self):
        deps = {}
        for e, i in self.last_eng.items():
            deps[i] = True
        for k, lst in self.dma_ops.items():
            deps[lst[-1]] = True
        for e in ENGS:
            self.ops.append(dict(eng=e, fn=None, deps=dict(deps), dma=None, signal=False, bar=True))
        self.lastw = {}
        self.readers = {}

    def flush(self):
        self.barrier()
        nc = self.nc
        ops = self.ops
        lo = self.emitted
        for o in ops[lo:]:
            for d in o["deps"]:
                ops[d]["signal"] = True
        for o in ops[lo:]:
            if o["fn"] is not None and o["dma"] is None and o["signal"]:
                self.cnt[o["eng"]] += 1
                o["val"] = self.cnt[o["eng"]]
        for k in self.dma_ops:
            if k not in self.dsems:
                self.dsems[k] = self.es.enter_context(nc.semaphore(f"d_{k}"))
        esems, dsems = self.esems, self.dsems

        def esem(e, v):
            k = (v - 1) // EPOCH
            while len(esems[e]) <= k:
                esems[e].append(self.es.enter_context(nc.semaphore(f"s_{e}{len(esems[e])}")))
            return esems[e][k], (v - 1) % EPOCH + 1

        def events(j, o):
            evs = []
            for d, raw in o["deps"].items():
                p = ops[d]
                if p["fn"] is None:
                    continue
                if p["dma"] is not None:
                    lst = self.dma_ops[p["dma"]]
                    n = bisect.bisect_left(lst, j)
                    evs.append((dsems[p["dma"]], 16 * n))
                else:
                    if p["eng"] == o["eng"]:
                        if o["eng"] == "pe" or (not raw and not o.get("bar")):
                            continue
                    evs.append(esem(p["eng"], p["val"]))
            return evs

        for o in ops[lo:]:
            if "val" in o:
                esem(o["eng"], o["val"])

        def make(ename):
            def body(eng):
                known = self.known.setdefault(ename, {})
                if not self.setup_done.get(ename):
                    self.setup_done[ename] = True
                    for f in self.setups.get(ename, []):
                        f(eng)
                for j in range(lo, len(ops)):
                    o = ops[j]
                    if o["eng"] != ename:
                        continue
                    need = {}
                    for sem, v in events(j, o):
                        key = id(sem)
                        if known.get(key, 0) >= v:
                            continue
                        if key not in need or need[key][1] < v:
                            need[key] = (sem, v)
                    for key, (sem, v) in need.items():
                        eng.wait_ge(sem, v)
                        known[key] = v
                    if o["fn"] is None:
                        continue
                    ins = o["fn"](eng)
                    if o["dma"] is not None:
                        ins.then_inc(dsems[o["dma"]], 16)
                    elif o["signal"]:
                        sem, _ = esem(ename, o["val"])
                        ins.then_inc(sem, 1)
            return body

        with nc.Block() as block:
            block.sync(make("sp"))
            block.scalar(make("act"))
            block.vector(make("dve"))
            block.gpsimd(make("pool"))
            block.tensor(make("pe"))
        self.emitted = len(ops)


def blk(b):
    if b == 0:
        return 0, LC
    return LC + (b - 1) * 512, LC + b * 512


def build_program(limit=99, debug=False):
    nc = bass.Bass("TRN2", target_bir_lowering=False)

    def din(name, shape, dt=F32):
        return nc.dram_tensor(name, list(shape), dt, kind="ExternalInput").ap()

    def dscr(name, shape, dt):
        return nc.dram_tensor(name, list(shape), dt, kind="ExternalOutput" if debug else "Internal").ap()

    x_d = din("x", [L, D])
    ctx_d = din("ctx", [LC, D])
    c2T_d = din("c2T", [128, 8, 2])
    wada_d = din("w_ada", [D, 6 * D])
    bada_d = din("b_ada2", [2, 6 * D])
    g1T_d = din("g1T", [128, 8])
    g2row_d = din("g2row", [2, D])
    win_d = din("w_in", [D, 4512])
    cwT_d = din("cwT", [128, 8, 4])
    cbT_d = din("cbT", [128, 8])
    rgwa_d = din("rg_wa", [2, 8, 128, 128])
    rgwx_d = din("rg_wx", [2, 8, 128, 128])
    rgbT_d = din("rgbT", [128, 3, 2, 8])
    wrnn_d = din("w_rnn_out", [D, D])
    wmla_d = din("w_mla_out", [512, D])
    wo_d = din("w_o", [D, D])
    wuq_d = din("w_uq", [256, 768])
    gql256_d = din("gql256", [128, 256])
    wuk_d = din("w_uk", [128, 512])
    wuv_d = din("w_uv", [128, 512])
    gkv128_d = din("gkv128", [128, 128])
    gq96_d = din("gq96", [128, 96])
    gk96_d = din("gk96", [128, 96])
    cos_d = din("cosT", [128, 32, 16])
    sin_d = din("sinT", [128, 32, 16])
    wr_d = din("w_router", [D, NE])
    weg_d = din("w_e_gate", [NE, D, D])
    weu_d = din("w_e_up", [NE, D, D])
    wed_d = din("w_e_down", [NE, D, D])
    out_d = nc.dram_tensor("out", [L, D], F32, kind="ExternalOutput").ap()

    bc_d = dscr("bc_d", [128, 4096], F32)
    rnnT_d = dscr("rnnT_d", [8, 128, L], BF16)
    z0_d = nc.dram_tensor("z0_d", [8, 128, T], F32, kind="Internal").ap()
    w2_d = nc.dram_tensor("w2_d", [8, 128, L], F32, kind="Internal").ap()
    G_d = dscr("G_d", [16, 128, L], BF16)
    QT_d = dscr("QT_d", [8, 96, L], BF16)
    KT_d = dscr("KT_d", [8, 96, T], BF16)
    V_d = dscr("V_d", [T, 520], BF16)
    rows_d = dscr("rows_d", [L, ROWW], BF16)
    xg_d = dscr("xg_d", [NE * CAP, ROWW], BF16)
    if debug:
        hT_dbg = dscr("hT_dbg", [128, 8, T], BF16)
        modT_dbg = dscr("modT_dbg", [128, 48, 2], F32)
        aff_dbg = dscr("aff_dbg", [128, 32, 16], F32)
        idx_dbg = dscr("idx_dbg", [128, 32, 16], I32)
        attn_dbg = dscr("attn_dbg", [128, 32, 512], BF16)

    win_v = win_d.rearrange("(k p) n -> p k n", p=128)

    with ExitStack() as es:
        S = Sched(nc, es)
        R = {}
        S.setup("pool", lambda e: R.__setitem__("bc_xg", e.to_reg(NE * CAP - 1)))
        S.setup("pool", lambda e: R.__setitem__("bc_out", e.to_reg(L - 1)))

        def sb(stack, name, shape, dt):
            return stack.enter_context(nc.sbuf_tensor(name, list(shape), dt))

        def ps(stack, name, shape, dt):
            return stack.enter_context(nc.psum_tensor(name, list(shape), dt))

        def dma(eng, out, in_, reads, writes, key):
            S.op(eng, lambda e: e.dma_start(out=out, in_=in_), reads, writes, dma=key)

        def act(out, in_, func, reads, writes, **kw):
            S.op("act", lambda e: e.activation(out=out, in_=in_, func=func, **kw), reads, writes)

        def ts(eng, out, in0, s1, s2, op0, op1, reads, writes):
            S.op(eng, lambda e: e.tensor_scalar(out, in0, s1, s2, op0, op1), reads, writes)

        def tt(eng, out, in0, in1, op, reads, writes):
            S.op(eng, lambda e: e.tensor_tensor(out, in0, in1, op), reads, writes)

        def stt(out, in0, scalar, in1, op0, op1, reads, writes):
            S.op("dve", lambda e: e.scalar_tensor_tensor(out, in0, scalar, in1, op0, op1), reads, writes)

        def cp(eng, out, in_, reads, writes):
            if eng == "act":
                S.op("act", lambda e: e.copy(out, in_), reads, writes)
            else:
                S.op(eng, lambda e: e.tensor_copy(out, in_), reads, writes)

        def recip(out, in_, reads, writes):
            S.op("dve", lambda e: e.reciprocal(out, in_), reads, writes)

        def red(out, in_, reads, writes, op=ALU.add):
            S.op("dve", lambda e: e.tensor_reduce(out, in_, AX.X, op), reads, writes)

        def mset(eng, ap, val, writes):
            S.op(eng, lambda e: e.memset(ap, val), (), writes)

        def mmg(out, pairs, reads, writes):
            def fn(e):
                n = len(pairs)
                ins = None
                for i, (l, r) in enumerate(pairs):
                    ins = e.matmul(out, lhsT=l, rhs=r, start=(i == 0), stop=(i == n - 1))
                return ins
            S.op("pe", fn, reads, writes)

        def trs(items, reads, writes):
            def fn(e):
                ins = None
                for o, i, idn in items:
                    ins = e.transpose(o, i, idn)
                return ins
            S.op("pe", fn, reads, writes)

        ident_f = sb(es, "ident_f", [128, 128], F32)
        ident_b = sb(es, "ident_b", [128, 128], BF16)
        modT = sb(es, "modT", [128, 48, 2], F32)
        A1T = sb(es, "A1T", [128, 8, 2], F32)
        HBA = sb(es, "HBA", [128, 2, 8], F32)
        HBX = sb(es, "HBX", [128, 2, 8], F32)
        CN = sb(es, "CN", [128, 2, 8], F32)
        HC = sb(es, "HC", [128, 2, 8], F32)
        CWH = sb(es, "CWH", [128, 8, 4], F32)
        CBH = sb(es, "CBH", [128, 8], F32)
        AFF = sb(es, "AFF", [128, 32, 16], F32)
        TOKID = sb(es, "TOKID", [128, 32], I32)
        IDX = sb(es, "IDX", [128, 32, NE], I32)

        mset("pool", ident_f[:], 0.0, ["ident_f"])
        S.op("pool", lambda e: e.affine_select(out=ident_f[:], in_=ident_f[:], pattern=[[-1, 128]],
                                                compare_op=ALU.not_equal, fill=1.0, base=0, channel_multiplier=1),
             ["ident_f"], ["ident_f"])
        cp("pool", ident_b[:], ident_f[:], ["ident_f"], ["ident_b"])

        with ExitStack() as ph:
            c2 = sb(ph, "c2", [128, 8, 2], F32)
            sc = sb(ph, "sc", [128, 8, 2], F32)
            wa = [sb(ph, f"wa{i}", [128, 8, 512], F32) for i in range(2)]
            modrow = sb(ph, "modrow", [2, 6 * D], F32)
            brow = sb(ph, "brow", [2, 6 * D], F32)
            g2r = sb(ph, "g2r", [2, D], F32)
            sel = sb(ph, "sel", [2, 128], F32)
            g1 = sb(ph, "g1", [128, 8], F32)
            rgb = sb(ph, "rgb", [128, 3, 2, 8], F32)
            spl = sb(ph, "spl", [128, 2, 8], F32)
            cw = sb(ph, "cw", [128, 8, 4], F32)
            cb = sb(ph, "cb", [128, 8], F32)
            bcs = [sb(ph, f"bcs{i}", [128, 512], F32) for i in range(2)]
            pm = [ps(ph, f"pm{i}", [2, 512], F32) for i in range(2)]
            pT0 = ps(ph, "pT0", [128, 48, 2], F32)
            pbc = [ps(ph, f"pbc{i}", [128, 512], F32) for i in range(2)]

            dma("sp", c2[:], c2T_d, [], ["c2"], "ld0")
            dma("sp", brow[:], bada_d, [], ["brow"], "ld0")
            dma("sp", g2r[:], g2row_d, [], ["g2r"], "ld0")
            dma("sp", g1[:], g1T_d, [], ["g1"], "ld0")
            dma("sp", rgb[:], rgbT_d, [], ["rgb"], "ld0")
            dma("sp", cw[:], cwT_d, [], ["cw"], "ld0")
            dma("sp", cb[:], cbT_d, [], ["cb"], "ld0")
            act(sc[:], c2[:], AF.Silu, ["c2"], ["sc"])
            wada_v = wada_d.rearrange("(k p) n -> p k n", p=128)
            for j in range(12):
                s = j % 2
                dma("sp", wa[s][:], wada_v[:, :, j * 512:(j + 1) * 512], [], [("wa", s)], f"wa{s}")
                mmg(pm[s][:], [(sc[:, k, :], wa[s][:, k, :]) for k in range(8)], ["sc", ("wa", s)], [("pm", s)])
                tt("dve", modrow[:, j * 512:(j + 1) * 512], pm[s][:], brow[:, j * 512:(j + 1) * 512], ALU.add,
                   [("pm", s), "brow"], ["modrow"])
            trs([(pT0[:, c, :], modrow[:, c * 128:(c + 1) * 128], ident_f[0:2, 0:2]) for c in range(48)],
                ["modrow", "ident_f"], ["pT0"])
            cp("dve", modT[:], pT0[:], ["pT0"], ["modT"])
            for r in range(2):
                stt(A1T[:, :, r], modT[:, 8:16, r], 1.0, g1[:], ALU.add, ALU.mult, ["modT", "g1"], ["A1T"])
            stt(modrow[:, 4096:5120], modrow[:, 4096:5120], 1.0, g2r[:], ALU.add, ALU.mult, ["modrow", "g2r"], ["modrow"])
            mset("pool", sel[:], 0.0, ["sel"])
            mset("pool", sel[0:1, :], 1.0, ["sel"])
            for q in range(8):
                s = q % 2
                mmg(pbc[s][:], [(sel[:], modrow[:, 2048 + q * 512:2048 + (q + 1) * 512])], ["sel", "modrow"], [("pbc", s)])
                cp("act", bcs[s][:], pbc[s][:], [("pbc", s)], [("bcs", s)])
                dma("sp", bc_d[:, q * 512:(q + 1) * 512], bcs[s][:], [("bcs", s)], [], f"bcst{s}")
            ts("pool", HBA[:], rgb[:, 0], 0.5, 0.0, ALU.mult, ALU.add, ["rgb"], ["HBA"])
            ts("pool", HBX[:], rgb[:, 1], 0.5, 0.0, ALU.mult, ALU.add, ["rgb"], ["HBX"])
            act(spl[:], rgb[:, 2], AF.Exp, ["rgb"], ["spl"], scale=-1.0)
            act(spl[:], spl[:], AF.Ln, ["spl"], ["spl"], bias=1.0)
            ts("pool", CN[:], spl[:], -8.0, 0.0, ALU.mult, ALU.add, ["spl"], ["CN"])
            ts("pool", HC[:], spl[:], -4.0, 0.0, ALU.mult, ALU.add, ["spl"], ["HC"])
            ts("pool", CWH[:], cw[:], 0.5, 0.0, ALU.mult, ALU.add, ["cw"], ["CWH"])
            ts("pool", CBH[:], cb[:], 0.5, 0.0, ALU.mult, ALU.add, ["cb"], ["CBH"])
            if debug:
                dma("sp", modT_dbg, modT[:], ["modT"], [], "dbg")
            S.flush()
        if limit <= 0:
            return nc

        with ExitStack() as midstack:
            hT = sb(midstack, "hT", [128, 8, T], BF16)

            with ExitStack() as ph:
                xt = [sb(ph, f"xt{i}", [128, 4, D], F32) for i in range(2)]
                junk = sb(ph, "junk", [128, D], BF16)
                ssA = [sb(ph, f"ssA{i}", [128, 4], F32) for i in range(2)]
                rsA = [sb(ph, f"rsA{i}", [128, 4], F32) for i in range(2)]
                pTa = [ps(ph, f"pTa{i}", [128, 512], F32) for i in range(4)]
                xv = x_d.rearrange("(g c p) d -> g p c d", c=4, p=128)
                cv = ctx_d.rearrange("(c p) d -> p c d", p=128)
                for g in range(9):
                    nC = 2 if g == 0 else 4
                    s = g % 2
                    r = 1 if g == 0 else 0
                    t0 = blk(g)[0]
                    src = cv if g == 0 else xv[g - 1]
                    dma("sp", xt[s][:, 0:nC, :], src, [], [("xt", s)] + [("xt", s, c) for c in range(4)], f"xt{s}")
                    mset("pool", ssA[s][:], 1.0, [("ssA", s)])
                    for c in range(nC):
                        act(junk[:], xt[s][:, c, :], AF.Square, [("xt", s)], ["junk", ("ssA", s)], accum_out=ssA[s][:, c:c + 1])
                    ts("dve", rsA[s][:], ssA[s][:], 1.0 / D, EPS, ALU.mult, ALU.add, [("ssA", s)], [("rsA", s)])
                    act(rsA[s][:], rsA[s][:], AF.Sqrt, [("rsA", s)], [("rsA", s)])
                    recip(rsA[s][:], rsA[s][:], [("rsA", s)], [("rsA", s)])
                    for c in range(nC):
                        eng = "dve"
                        ts(eng, xt[s][:, c, :], xt[s][:, c, :], rsA[s][:, c:c + 1], 0.0, ALU.mult, ALU.add,
                           [("xt", s), ("rsA", s)], [("xt", s, c)])
                    for k in range(8):
                        bank = pTa[k % 4]
                        trs([(bank[:, c * 128:(c + 1) * 128], xt[s][:, c, k * 128:(k + 1) * 128], ident_f[:]) for c in range(nC)],
                            [("xt", s, c) for c in range(nC)] + ["ident_f"], [("pTa", k % 4)])
                        o = hT[:, k, t0:t0 + nC * 128]
                        i_ = bank[:, 0:nC * 128]
                        if k % 2 == 0:
                            ts("dve", o, i_, A1T[:, k, r:r + 1], modT[:, k, r:r + 1], ALU.mult, ALU.add,
                               [("pTa", k % 4), "A1T", "modT"], [("hT", g, k)])
                        else:
                            act(o, i_, AF.Identity, [("pTa", k % 4), "A1T", "modT"], [("hT", g, k)],
                                scale=A1T[:, k, r:r + 1], bias=modT[:, k, r:r + 1])
                if debug:
                    dma("sp", hT_dbg, hT[:], [("hT", g, k) for g in range(9) for k in range(8)], [], "dbg")
                S.flush()
            if limit <= 1:
                return nc
            HT_ALL = []

            with ExitStack() as ph:
                Z0S = [sb(ph, f"Z0S{i}", [128, 4360], F32) for i in range(2)]
                XRS = [sb(ph, f"XRS{i}", [128, T], F32) for i in range(2)]
                X1 = sb(ph, "X1", [128, L], F32)
                X2 = sb(ph, "X2", [128, L], F32)
                wz = [sb(ph, f"wz{i}", [128, 8, 256], BF16) for i in range(2)]
                pz = [ps(ph, f"pz{i}", [128, 512], F32) for i in range(4)]
                NB = 9

                def load_wz(n):
                    s = n % 2
                    dma("pool", wz[s][:, :, 0:128], win_v[:, :, n * 128:(n + 1) * 128], [], [("wz", s, 0)], f"wz{s}")
                    dma("pool", wz[s][:, :, 128:256], win_v[:, :, 1024 + n * 128:1024 + (n + 1) * 128], [], [("wz", s, 1)], f"wz{s}")

                for i in range(2):
                    mset("pool", Z0S[i][:], 0.0, [("Z0S", i, b) for b in range(NB)])
                load_wz(0)
                pc = 0
                for n in range(8):
                    s = n % 2
                    if n + 1 < 8:
                        load_wz(n + 1)
                    for b in range(NB):
                        lo, hi = blk(b)
                        w = hi - lo
                        zc = lo + 2 if b == 0 else lo + 6
                        q = pc % 4
                        pc += 1
                        mmg(pz[q][:, 0:w], [(wz[s][:, k, 0:128], hT[:, k, lo:hi]) for k in range(8)], [("wz", s, 0)], [("pz", q)])
                        cp("act", Z0S[s][:, zc:zc + w], pz[q][:, 0:w], [("pz", q)], [("Z0S", s, b)])
                    zk = [("Z0S", s, b) for b in range(NB)]
                    conv_late = []
                    for (o0, o1, zb) in ((0, LC, 0), (LC, T, 260)):
                        w = o1 - o0
                        ts("pool", XRS[s][:, o0:o1], Z0S[s][:, zb:zb + w], CWH[:, n, 0:1], CBH[:, n:n + 1], ALU.mult, ALU.add,
                           zk, [("XRS", s, o0)])
                        for k in range(1, 4):
                            args = (XRS[s][:, o0:o1], Z0S[s][:, zb + k:zb + k + w], CWH[:, n, k:k + 1], XRS[s][:, o0:o1], ALU.mult, ALU.add,
                                    zk + [("XRS", s, o0)], [("XRS", s, o0)])
                            if o0 == 0:
                                stt(*args)
                            else:
                                conv_late.append(args)
                    def tanh_stage(n_, sg):
                        sl = slice(sg * 1024, (sg + 1) * 1024)
                        act(X2[:, sl], X2[:, sl], AF.Tanh, [("X2", sg)], [("X2", sg)], scale=0.7978845608028654)
                        stt(X2[:, sl], X2[:, sl], 1.0, X1[:, sl], ALU.add, ALU.mult, [("X2", sg), ("X1", sg)], [("X2", sg)])
                        dma("sp", w2_d[n_, :, sg * 1024:(sg + 1) * 1024], X2[:, sl], [("X2", sg)], [], f"w2st{sg % 2}")

                    if n >= 1:
                        tanh_stage(n - 1, 3)
                    for sg in range(4):
                        bs = (1 + 2 * sg, 2 + 2 * sg)
                        l0 = sg * 1024
                        for b in bs:
                            blo, bhi = blk(b)
                            q = pc % 4
                            pc += 1
                            mmg(pz[q][:], [(wz[s][:, k, 128:256], hT[:, k, blo:bhi]) for k in range(8)], [("wz", s, 1)], [("pz", q)])
                            cp("act", X1[:, blo - LC:bhi - LC], pz[q][:], [("pz", q)], [("X1", sg)])
                            act(X2[:, blo - LC:bhi - LC], pz[q][:], AF.Square, [("pz", q)], [("X2", sg)], scale=0.044715 ** 0.5)
                        sl = slice(l0, l0 + 1024)
                        stt(X2[:, sl], X2[:, sl], 1.0, X1[:, sl], ALU.add, ALU.mult, [("X2", sg), ("X1", sg)], [("X2", sg)])
                        if sg >= 1:
                            tanh_stage(n, sg - 1)
                        if sg < 3:
                            stt(*conv_late[sg])
                    dma("sp", z0_d[n], XRS[s][:], [("XRS", s, 0), ("XRS", s, LC)], [], f"z0st{s}")
                tanh_stage(7, 3)
                S.flush()
            if limit <= 2:
                return nc

            with ExitStack() as ph:
                wg = [sb(ph, f"wg{i}", [128, 8, 128], BF16) for i in range(2)]
                GB = [sb(ph, f"GB{i}", [128, L], BF16) for i in range(2)]
                pg = [ps(ph, f"pg{i}", [128, 512], F32) for i in range(4)]
                for j in range(16):
                    s = j % 2
                    dma("pool", wg[s][:], win_v[:, :, 2464 + j * 128:2464 + (j + 1) * 128], [], [("wg", s)], f"wg{s}")
                    for b in range(1, 9):
                        lo, hi = blk(b)
                        mmg(pg[b % 4][:], [(wg[s][:, k, :], hT[:, k, lo:hi]) for k in range(8)], [("wg", s)], [("pg", b % 4)])
                        act(GB[s][:, lo - LC:hi - LC], pg[b % 4][:], AF.Sigmoid, [("pg", b % 4)], [("GB", s)])
                    dma("sp", G_d[j], GB[s][:], [("GB", s)], [], f"gst{s}")
                S.flush()
            if limit <= 3:
                return nc

            with ExitStack() as ph:
                wqkv = sb(ph, "wqkv", [128, 8, 416], BF16)
                wuq = sb(ph, "wuq", [128, 2, 768], BF16)
                wukv = sb(ph, "wukv", [128, 1024], BF16)
                GQL = sb(ph, "GQL", [128, 256], F32)
                GKV = sb(ph, "GKV", [128, 128], F32)
                GQ = sb(ph, "GQ", [128, 96], F32)
                GK = sb(ph, "GK", [128, 96], F32)
                COS = sb(ph, "COS", [128, 32, 16], F32)
                SIN = sb(ph, "SIN", [128, 32, 16], F32)
                INV3 = sb(ph, "INV3", [128, 4, 3], F32)
                INV24 = sb(ph, "INV24", [128, 4, 24], F32)
                Z = sb(ph, "Z", [128, 4, 416], F32)
                SQ = sb(ph, "SQ", [128, 4, 768], F32)
                ss3 = sb(ph, "ss3", [128, 4, 3], F32)
                r3 = sb(ph, "r3", [128, 4, 3], F32)
                ss24 = sb(ph, "ss24", [128, 4, 24], F32)
                r24 = sb(ph, "r24", [128, 4, 24], F32)
                ZQS = sb(ph, "ZQS", [128, 4, 256], F32)
                CKS = sb(ph, "CKS", [128, 4, 128], F32)
                KR = sb(ph, "KR", [128, 4, 32], F32)
                KR2 = sb(ph, "KR2", [128, 4, 32], F32)
                ZT = sb(ph, "ZT", [128, 4, 3, 128], BF16)
                QF = sb(ph, "QF", [128, 4, 768], F32)
                KF = sb(ph, "KF", [128, 4, 512], F32)
                T1 = sb(ph, "T1", [128, 4, 8, 16], F32)
                T2 = sb(ph, "T2", [128, 4, 8, 16], F32)
                T3 = sb(ph, "T3", [128, 4, 16], F32)
                T4 = sb(ph, "T4", [128, 4, 16], F32)
                QTM = sb(ph, "QTM", [128, 4, 8, 96], BF16)
                KTM = sb(ph, "KTM", [128, 4, 8, 96], BF16)
                VTM = sb(ph, "VTM", [128, 4, 8, 65], BF16)
                QST = sb(ph, "QST", [96, 8, 512], BF16)
                KST = sb(ph, "KST", [96, 8, 512], BF16)
                PQ = ps(ph, "PQ", [128, 1024], F32)
                PZ = [ps(ph, "PZ0", [128, 512], F32)] * 2
                PTZ = ps(ph, "PTZ", [128, 3, 128], F32)
                PK = ps(ph, "PK", [128, 512], F32)
                PV = ps(ph, "PV", [128, 512], F32)
                PTQ = ps(ph, "PTQ", [96, 8, 128], BF16)
                PTK = ps(ph, "PTK", [96, 8, 128], BF16)

                dma("pool", wqkv[:], win_v[:, :, 2048:2464], [], ["wqkv"], "ldc")
                dma("pool", wuq[:], wuq_d.rearrange("(k p) n -> p k n", p=128), [], ["wuq"], "ldc")
                dma("pool", wukv[:, 0:512], wuk_d, [], ["wukv"], "ldc")
                dma("pool", wukv[:, 512:1024], wuv_d, [], ["wukv2"], "ldc")
                dma("sp", GQL[:], gql256_d, [], ["GQL"], "ldc2")
                dma("sp", GKV[:], gkv128_d, [], ["GKV"], "ldc2")
                dma("sp", GQ[:], gq96_d, [], ["GQ"], "ldc2")
                dma("sp", GK[:], gk96_d, [], ["GK"], "ldc2")
                dma("sp", COS[:], cos_d, [], ["COS"], "ldc2")
                dma("sp", SIN[:], sin_d, [], ["SIN"], "ldc2")
                ts("dve", GQ[:], GQ[:], 96.0 ** -0.5, 0.0, ALU.mult, ALU.add, ["GQ"], ["GQ"])
                for j, v in enumerate((1.0 / 256, 1.0 / 128, 1.0 / 32)):
                    mset("pool", INV3[:, :, j:j + 1], v, ["INV3"])
                mset("pool", INV24[:, :, 0:8], 1.0 / 64, ["INV24"])
                mset("pool", INV24[:, :, 8:16], 1.0 / 32, ["INV24"])
                mset("pool", INV24[:, :, 16:24], 1.0 / 64, ["INV24"])
                mset("pool", ss3[:], 1.0, ["ss3"])
                mset("pool", ss24[:], 1.0, ["ss24"])
                mset("pool", VTM[:], 1.0, ["VTM"])
                QT_v = QT_d.rearrange("h d t -> d h t")
                KT_v = KT_d.rearrange("h d t -> d h t")
                V_v = V_d.rearrange("(c p) f -> p c f", p=128)

                for g in range(9):
                    nC = 2 if g == 0 else 4
                    lat = g > 0
                    t0 = blk(g)[0]
                    c0 = 0 if lat else 256
                    cg = (g - 1) * 4
                    for c in range(nC):
                        p = PZ[c % 2]
                        mmg(p[:, c0:416], [(hT[:, k, t0 + c * 128:t0 + (c + 1) * 128], wqkv[:, k, c0:416]) for k in range(8)],
                            ["wqkv"], [("PZ", 0)])
                        cp("act", Z[:, c, c0:416], p[:, c0:416], [("PZ", 0)], ["Z"])
                    act(SQ[:, 0:nC, c0:416], Z[:, 0:nC, c0:416], AF.Square, ["Z"], ["SQ"])
                    if lat:
                        red(ss3[:, 0:nC, 0], SQ[:, 0:nC, 0:256], ["SQ"], ["ss3"])
                    red(ss3[:, 0:nC, 1], SQ[:, 0:nC, 256:384], ["SQ"], ["ss3"])
                    red(ss3[:, 0:nC, 2], SQ[:, 0:nC, 384:416], ["SQ"], ["ss3"])
                    tt("dve", r3[:], ss3[:], INV3[:], ALU.mult, ["ss3", "INV3"], ["r3"])
                    act(r3[:], r3[:], AF.Sqrt, ["r3"], ["r3"], bias=EPS)
                    recip(r3[:], r3[:], ["r3"], ["r3"])
                    for c in range(nC):
                        if lat:
                            stt(ZQS[:, c, :], Z[:, c, 0:256], r3[:, c, 0:1], GQL[:], ALU.mult, ALU.mult, ["Z", "r3", "GQL"], ["ZQS"])
                        stt(CKS[:, c, :], Z[:, c, 256:384], r3[:, c, 1:2], GKV[:], ALU.mult, ALU.mult, ["Z", "r3", "GKV"], ["CKS"])
                        stt(KR[:, c, :], Z[:, c, 384:416], r3[:, c, 2:3], GK[:, 64:96], ALU.mult, ALU.mult, ["Z", "r3", "GK"], ["KR"])
                    for c in range(nC):
                        items = [(PTZ[:, 2, :], CKS[:, c, :], ident_f[:])]
                        if lat:
                            items += [(PTZ[:, 0, :], ZQS[:, c, 0:128], ident_f[:]), (PTZ[:, 1, :], ZQS[:, c, 128:256], ident_f[:])]
                        trs(items, ["CKS", "ZQS", "ident_f"], ["PTZ"])
                        if lat:
                            cp("act", ZT[:, c, 0:2, :], PTZ[:, 0:2, :], ["PTZ"], [("ZT", c)])
                        cp("dve", ZT[:, c, 2, :], PTZ[:, 2, :], ["PTZ"], [("ZT", c)])
                        if lat:
                            mmg(PQ[:, 0:512], [(ZT[:, c, k, :], wuq[:, k, 0:512]) for k in range(2)], [("ZT", c), "wuq"], ["PQ"])
                            mmg(PQ[:, 512:768], [(ZT[:, c, k, :], wuq[:, k, 512:768]) for k in range(2)], [("ZT", c), "wuq"], ["PQ2"])
                            cp("act", QF[:, c, :], PQ[:, 0:768], ["PQ", "PQ2"], ["QF"])
                        mmg(PK[:], [(ZT[:, c, 2, :], wukv[:, 0:512])], [("ZT", c), "wukv"], ["PK"])
                        mmg(PV[:], [(ZT[:, c, 2, :], wukv[:, 512:1024])], [("ZT", c), "wukv2"], ["PV"])
                        cp("dve", KF[:, c, :], PK[:], ["PK"], ["KF"])
                        cp("act", VTM[:, c, :, 0:64], PV[:].rearrange("p (h d) -> p h d", d=64), ["PV"], ["VTM"])
                    QF4 = QF[:, 0:nC, :].rearrange("p c (h d) -> p c h d", d=96)
                    SQ4 = SQ[:, 0:nC, :].rearrange("p c (h d) -> p c h d", d=96)
                    KF4 = KF[:, 0:nC, :].rearrange("p c (h d) -> p c h d", d=64)
                    SQK4 = SQ[:, 0:nC, 0:512].rearrange("p c (h d) -> p c h d", d=64)
                    if lat:
                        act(SQ[:, 0:nC, :], QF[:, 0:nC, :], AF.Square, ["QF"], ["SQ"])
                        red(ss24[:, 0:nC, 0:8], SQ4[:, :, :, 0:64], ["SQ"], ["ss24"])
                        red(ss24[:, 0:nC, 8:16], SQ4[:, :, :, 64:96], ["SQ"], ["ss24"])
                    act(SQ[:, 0:nC, 0:512], KF[:, 0:nC, :], AF.Square, ["KF"], ["SQ"])
                    red(ss24[:, 0:nC, 16:24], SQK4, ["SQ"], ["ss24"])
                    tt("dve", r24[:], ss24[:], INV24[:], ALU.mult, ["ss24", "INV24"], ["r24"])
                    act(r24[:], r24[:], AF.Sqrt, ["r24"], ["r24"], bias=EPS)
                    recip(r24[:], r24[:], ["r24"], ["r24"])
                    QTM4 = QTM[:, 0:nC]
                    KTM4 = KTM[:, 0:nC]
                    if lat:
                        tt("dve", QF4[:, :, :, 0:64], QF4[:, :, :, 0:64], r24[:, 0:nC, 0:8].unsqueeze(3).to_broadcast([128, nC, 8, 64]), ALU.mult, ["QF", "r24"], ["QF"])
                        tt("dve", QF4[:, :, :, 64:96], QF4[:, :, :, 64:96], r24[:, 0:nC, 8:16].unsqueeze(3).to_broadcast([128, nC, 8, 32]), ALU.mult, ["QF", "r24"], ["QF"])
                        tt("dve", QTM4[:, :, :, 0:64], QF4[:, :, :, 0:64], GQ[:, 0:64].unsqueeze(1).unsqueeze(1).to_broadcast([128, nC, 8, 64]), ALU.mult, ["QF", "GQ"], ["QTM"])
                        tt("dve", QF4[:, :, :, 64:96], QF4[:, :, :, 64:96], GQ[:, 64:96].unsqueeze(1).unsqueeze(1).to_broadcast([128, nC, 8, 32]), ALU.mult, ["QF", "GQ"], ["QF"])
                    tt("dve", KF4, KF4, r24[:, 0:nC, 16:24].unsqueeze(3).to_broadcast([128, nC, 8, 64]), ALU.mult, ["KF", "r24"], ["KF"])
                    tt("dve", KTM4[:, :, :, 0:64], KF4, GK[:, 0:64].unsqueeze(1).unsqueeze(1).to_broadcast([128, nC, 8, 64]), ALU.mult, ["KF", "GK"], ["KTM"])
                    if lat:
                        Cq = COS[:, cg:cg + nC, :].unsqueeze(2).to_broadcast([128, nC, 8, 16])
                        Sq = SIN[:, cg:cg + nC, :].unsqueeze(2).to_broadcast([128, nC, 8, 16])
                        x1, x2 = QF4[:, :, :, 64:80], QF4[:, :, :, 80:96]
                        tt("dve", T1[:, 0:nC], x1, Cq, ALU.mult, ["QF", "COS"], ["T1"])
                        tt("dve", T2[:, 0:nC], x2, Sq, ALU.mult, ["QF", "SIN"], ["T2"])
                        tt("dve", QTM4[:, :, :, 64:80], T1[:, 0:nC], T2[:, 0:nC], ALU.subtract, ["T1", "T2"], ["QTM"])
                        tt("dve", T1[:, 0:nC], x1, Sq, ALU.mult, ["QF", "SIN"], ["T1"])
                        tt("dve", T2[:, 0:nC], x2, Cq, ALU.mult, ["QF", "COS"], ["T2"])
                        tt("dve", QTM4[:, :, :, 80:96], T1[:, 0:nC], T2[:, 0:nC], ALU.add, ["T1", "T2"], ["QTM"])
                        Ck = COS[:, cg:cg + nC, :]
                        Sk = SIN[:, cg:cg + nC, :]
                        k1, k2 = KR[:, 0:nC, 0:16], KR[:, 0:nC, 16:32]
                        tt("dve", T3[:, 0:nC], k1, Ck, ALU.mult, ["KR", "COS"], ["T3"])
                        tt("dve", T4[:, 0:nC], k2, Sk, ALU.mult, ["KR", "SIN"], ["T4"])
                        tt("dve", KR2[:, 0:nC, 0:16], T3[:, 0:nC], T4[:, 0:nC], ALU.subtract, ["T3", "T4"], ["KR2"])
                        tt("dve", T3[:, 0:nC], k1, Sk, ALU.mult, ["KR", "SIN"], ["T3"])
                        tt("dve", T4[:, 0:nC], k2, Ck, ALU.mult, ["KR", "COS"], ["T4"])
                        tt("dve", KR2[:, 0:nC, 16:32], T3[:, 0:nC], T4[:, 0:nC], ALU.add, ["T3", "T4"], ["KR2"])
                        krs = KR2
                    else:
                        krs = KR
                    cp("dve", KTM4[:, :, :, 64:96], krs[:, 0:nC, :].unsqueeze(2).to_broadcast([128, nC, 8, 32]), ["KR", "KR2"], ["KTM"])
                    for c in range(nC):
                        if lat:
                            trs([(PTQ[:, h, :], QTM[:, c, h, :], ident_b[:]) for h in range(8)], ["QTM", "ident_b"], ["PTQ"])
                            cp("act", QST[:, :, c * 128:(c + 1) * 128], PTQ[:], ["PTQ"], ["QST"])
                        trs([(PTK[:, h, :], KTM[:, c, h, :], ident_b[:]) for h in range(8)], ["KTM", "ident_b"], ["PTK"])
                        cp("dve", KST[:, :, c * 128:(c + 1) * 128], PTK[:], ["PTK"], ["KST"])
                    if lat:
                        dma("sp", QT_v[:, :, t0 - LC:t0 - LC + 512], QST[:], ["QST"], [], "qst")
                    dma("sp", KT_v[:, :, t0:t0 + nC * 128], KST[:, :, 0:nC * 128], ["KST"], [], "kst")
                    dma("sp", V_v[:, t0 // 128:t0 // 128 + nC, :], VTM[:, 0:nC].rearrange("p c h d -> p c (h d)"), ["VTM"], [], "vst")
                S.flush()
        if limit <= 3:
            return nc

        with ExitStack() as ph:
            GZs = [sb(ph, f"GZs{i}", [128, 1024], F32) for i in range(2)]
            XRH = [sb(ph, f"XRH{i}", [128, T], F32) for i in range(2)]
            XRB = [sb(ph, f"XRB{i}", [128, T], BF16) for i in range(2)]
            BB = [[sb(ph, f"B{j}_{d}", [128, T], F32) for j in range(3)] for d in range(2)]
            HF = sb(ph, "HF", [128, L], F32)
            HBC = [sb(ph, f"HBC{d}", [128, LC], F32) for d in range(2)]
            RNNB = [sb(ph, f"RNNB{i}", [128, 1024], BF16) for i in range(2)]
            rgw = [sb(ph, f"rgw{i}", [128, 2, 2, 128], BF16) for i in range(2)]
            pa = [ps(ph, f"pa{i}", [128, 512], F32) for i in range(2)]
            px = [ps(ph, f"px{i}", [128, 512], F32) for i in range(2)]
            NB = 9

            def load_n(n):
                s = n % 2
                for d in range(2):
                    dma("pool", rgw[s][:, d, 0, :], rgwa_d[d, n], [], [("rgw", s, d, 0)], f"rgw{s}")
                    dma("pool", rgw[s][:, d, 1, :], rgwx_d[d, n], [], [("rgw", s, d, 1)], f"rgw{s}")
                dma("sp", XRH[s][:], z0_d[n], [], [("XRH", s)], f"ldx{s}")
                ts("pool", XRB[s][:], XRH[s][:], 2.0, 0.0, ALU.mult, ALU.add, [("XRH", s)], [("XRB", s)])

            load_n(0)
            gzc = 0
            for n in range(8):
                s = n % 2
                if n + 1 < 8:
                    load_n(n + 1)
                for d in range(2):
                    B1, B2, B3 = BB[d]
                    K = lambda nm: [((nm, d), b) for b in range(NB)]
                    for b in range(NB):
                        lo, hi = blk(b)
                        w = hi - lo
                        mmg(pa[b % 2][:, 0:w], [(rgw[s][:, d, 0, :], XRB[s][:, lo:hi])], [("rgw", s, d, 0), ("XRB", s)], [("pa", b % 2)])
                        mmg(px[b % 2][:, 0:w], [(rgw[s][:, d, 1, :], XRB[s][:, lo:hi])], [("rgw", s, d, 1), ("XRB", s)], [("px", b % 2)])
                        act(B1[:, lo:hi], pa[b % 2][:, 0:w], AF.Tanh, [("pa", b % 2)], [(("B1", d), b)], scale=0.5, bias=HBA[:, d, n:n + 1])
                        act(B2[:, lo:hi], B1[:, lo:hi], AF.Exp, [(("B1", d), b)], [(("B2", d), b)], scale=HC[:, d, n:n + 1], bias=HC[:, d, n:n + 1])
                        act(B3[:, lo:hi], B1[:, lo:hi], AF.Exp, [(("B1", d), b)], [(("B3", d), b)], scale=CN[:, d, n:n + 1], bias=CN[:, d, n:n + 1])
                        act(B1[:, lo:hi], px[b % 2][:, 0:w], AF.Tanh, [("px", b % 2)], [(("B1", d), b)], scale=0.5, bias=HBX[:, d, n:n + 1])
                    act(B3[:], B3[:], AF.Sqrt, K("B3"), K("B3"), scale=-1.0, bias=1.0)
                for d in range(2):
                    B1, B2, B3 = BB[d]
                    K = lambda nm: [((nm, d), b) for b in range(NB)]
                    stt(B1[:], B1[:], 1.0, B3[:], ALU.add, ALU.mult, K("B1") + K("B3"), K("B1"))
                    tt("dve", B1[:], B1[:], XRH[s][:], ALU.mult, K("B1") + [("XRH", s)], K("B1"))
                    if d == 0:
                        S.op("dve", lambda e, B1=B1, B2=B2: e.tensor_tensor_scan(HBC[0][:], B2[:, 0:LC], B1[:, 0:LC], 0.0, ALU.mult, ALU.add),
                             K("B1") + K("B2"), ["HBC0"])
                        S.op("dve", lambda e, B1=B1, B2=B2: e.tensor_tensor_scan(HF[:], B2[:, LC:T], B1[:, LC:T], HBC[0][:, LC - 1:LC], ALU.mult, ALU.add),
                             K("B1") + K("B2") + ["HBC0"], ["HF"])
                    else:
                        S.op("dve", lambda e, B1=B1, B2=B2: e.tensor_tensor_scan(HBC[1][:, ::-1], B2[:, 0:LC][:, ::-1], B1[:, 0:LC][:, ::-1], 0.0, ALU.mult, ALU.add),
                             K("B1") + K("B2"), ["HBC1"])
                        S.op("dve", lambda e, B1=B1, B2=B2, B3=B3: e.tensor_tensor_scan(B3[:, LC:T][:, ::-1], B2[:, LC:T][:, ::-1], B1[:, LC:T][:, ::-1], HBC[1][:, 0:1], ALU.mult, ALU.add),
                             K("B1") + K("B2") + K("B3") + ["HBC1"], K("B3"))
                        for sg in range(4):
                            lo = LC + sg * 1024
                            l0 = sg * 1024
                            gq = gzc % 2
                            gzc += 1
                            dma("sp", GZs[gq][:], w2_d[n, :, l0:l0 + 1024], [], [("GZs", gq)], f"ldgz{gq}")
                            kb = [(("B3", d), 1 + 2 * sg), (("B3", d), 2 + 2 * sg)]
                            tt("dve", B3[:, lo:lo + 1024], B3[:, lo:lo + 1024], HF[:, l0:l0 + 1024], ALU.add, K("B3") + ["HF"], kb)
                            stt(RNNB[sg % 2][:], B3[:, lo:lo + 1024], 0.5, GZs[gq][:], ALU.mult, ALU.mult, kb + [("GZs", gq)], [("RNNB", sg % 2)])
                            dma("sp", rnnT_d[n, :, l0:l0 + 1024], RNNB[sg % 2][:], [("RNNB", sg % 2)], [], f"rnn{sg % 2}")
            S.flush()
        if limit <= 4:
            return nc

        with ExitStack() as attnstack:
            attnTM = sb(attnstack, "attnTM", [128, 32, 512], BF16)
            with ExitStack() as ph:
                VT = sb(ph, "VT", [128, 34, 520], BF16)
                KTh = [sb(ph, f"KTh{i}", [96, T], BF16) for i in range(2)]
                QTh = [sb(ph, f"QTh{i}", [96, L], BF16) for i in range(2)]
                PT = [sb(ph, f"PT{i}", [128, 512], BF16) for i in range(4)]
                rec = [sb(ph, f"rec{i}", [128, 4], F32) for i in range(2)]
                Sb = [ps(ph, f"Sb{i}", [128, 512], F32) for i in range(4)]
                Ob = [ps(ph, f"Ob{i}", [128, 4, 65], F32) for i in range(2)]
                dma("sp", VT[:], V_d.rearrange("(c p) f -> p c f", p=128), [], ["VT"], "ldv")
                steps = [(h, qb, kc) for h in range(8) for qb in range(8) for kc in range(34)]
                LA = 3

                def score(i):
                    h, qb, kc = steps[i]
                    s = h % 2
                    mmg(Sb[i % 4][:], [(KTh[s][:, kc * 128:(kc + 1) * 128], QTh[s][:, qb * 512:(qb + 1) * 512])],
                        [("KQ", s)], [("Sb", i % 4)])

                for h in range(8):
                    s = h % 2
                    pass
                loaded = set()

                def load_head(h):
                    if h in loaded or h >= 8:
                        return
                    loaded.add(h)
                    s = h % 2
                    dma("sp", KTh[s][:], KT_d[h], [], [("KQ", s)], f"kq{s}")
                    dma("sp", QTh[s][:], QT_d[h], [], [("KQ", s)], f"kq{s}")

                load_head(0)
                for i in range(LA):
                    score(i)
                for i, (h, qb, kc) in enumerate(steps):
                    if qb == 0 and kc == 0:
                        load_head(h + 1)
                    if i + LA < len(steps):
                        if steps[i + LA][0] not in loaded:
                            load_head(steps[i + LA][0])
                        score(i + LA)
                    o = (h * 8 + qb) % 2
                    act(PT[i % 4][:], Sb[i % 4][:], AF.Exp, [("Sb", i % 4)], [("PT", i % 4)])

                    def pv(e, i=i, h=h, kc=kc, o=o):
                        ins = None
                        for qc in range(4):
                            ins = e.matmul(Ob[o][:, qc, :], lhsT=PT[i % 4][:, qc * 128:(qc + 1) * 128],
                                           rhs=VT[:, kc, h * 65:(h + 1) * 65],
                                           start=(kc == 0 and qc == 0), stop=(kc == 33 and qc == 3), skip_group_check=True)
                        return ins
                    S.op("pe", pv, [("PT", i % 4), "VT"], [("Ob", o)])
                    if kc == 33:
                        recip(rec[o][:], Ob[o][:, :, 64], [("Ob", o)], [("rec", o)])
                        tt("dve", attnTM[:, qb * 4:(qb + 1) * 4, h * 64:(h + 1) * 64], Ob[o][:, :, 0:64],
                           rec[o][:].unsqueeze(2).to_broadcast([128, 4, 64]), ALU.mult, [("Ob", o), ("rec", o)], [("attn", qb)])
                if debug:
                    dma("sp", attn_dbg, attnTM[:], [("attn", qb) for qb in range(8)], [], "dbg")
                S.flush()
            if limit <= 5:
                return nc

            S.op("pool", lambda e: e.iota(TOKID[:], pattern=[[128, 32]], base=0, channel_multiplier=1), [], ["TOKID"])
            with ExitStack() as ph:
                WR = sb(ph, "WR", [128, 8, D], BF16)
                WM = sb(ph, "WM", [128, 4, D], BF16)
                WO = sb(ph, "WO", [128, 8, D], BF16)
                bcE = sb(ph, "bcE", [128, 3072], F32)
                wr32 = sb(ph, "wr32", [128, 8, NE], F32)
                RT = [sb(ph, f"RT{i}", [128, 8, 512], BF16) for i in range(2)]
                GT = [sb(ph, f"GT{i}", [128, 2, 512], BF16) for i in range(2)]
                XB = [sb(ph, f"XB{i}", [128, 4, D], F32) for i in range(2)]
                AT = sb(ph, "AT", [128, 4, 512], BF16)
                MT = sb(ph, "MT", [128, 8, 512], BF16)
                tA = [sb(ph, f"tA{i}", [128, 512], F32) for i in range(2)]
                tB = [sb(ph, f"tB{i}", [128, 512], F32) for i in range(2)]
                tC = [sb(ph, f"tC{i}", [128, 512], F32) for i in range(2)]
                H2T = [sb(ph, f"H2T{i}", [128, 8, 128], F32) for i in range(2)]
                ROWS = [sb(ph, "ROWS0", [128, 4, ROWW], BF16)] * 2
                junkE = sb(ph, "junkE", [128, D], BF16)
                ssE = sb(ph, "ssE", [128, 4], F32)
                rsE = sb(ph, "rsE", [128, 4], F32)
                mxE = sb(ph, "mxE", [128, 4], F32)
                smE = sb(ph, "smE", [128, 4], F32)
                EX = sb(ph, "EX", [128, 4, NE], F32)
                P1 = [ps(ph, f"P1{i}", [128, 512], F32) for i in range(2)]
                P2 = [ps(ph, f"P2{i}", [128, 512], F32) for i in range(2)]
                P3 = [ps(ph, f"P3{i}", [128, 512], F32) for i in range(2)]
                PTA = ps(ph, "PTA", [128, 4, 128], BF16)
                PTH = ps(ph, "PTH", [128, 512], F32)
                dma("pool", WR[:], wrnn_d.rearrange("(k p) n -> p k n", p=128), [], ["WR"], "lde")
                dma("pool", WM[:], wmla_d.rearrange("(k p) n -> p k n", p=128), [], ["WM"], "lde")
                dma("pool", WO[:], wo_d.rearrange("(k p) n -> p k n", p=128), [], ["WO"], "lde")
                dma("sp", bcE[:], bc_d[:, 0:3072], [], ["bcE"], "lde2")
                dma("sp", wr32[:], wr_d.rearrange("(k p) n -> p k n", p=128), [], ["wr32"], "lde2")
                m2bc, m3bc, a2bc = bcE[:, 0:1024], bcE[:, 1024:2048], bcE[:, 2048:3072]
                rn_v = rnnT_d.rearrange("n p t -> p n t")
                G_v = G_d.rearrange("j p t -> p j t")
                x_v = x_d.rearrange("(g c p) d -> g p c d", c=4, p=128)
                o_v = out_d.rearrange("(g c p) d -> g p c d", c=4, p=128)
                rows_v = rows_d.rearrange("(g c p) f -> g p c f", c=4, p=128)

                def load_rt(bi):
                    if bi >= 8:
                        return
                    s = bi % 2
                    dma("sp", RT[s][:], rn_v[:, :, bi * 512:(bi + 1) * 512], [], [("RT", s)], f"ldb{s}")

                def load_xb(bi):
                    if bi >= 8:
                        return
                    s = bi % 2
                    dma("pool", XB[s][:], x_v[bi], [], [("XB", s)] + [("XB", s, c, hf) for c in range(4) for hf in range(2)] + [("H2", s, c) for c in range(4)], f"ldxb{s}")

                def front(bi):
                    s = bi % 2
                    load_rt(bi + 1)
                    for c in range(4):
                        trs([(PTA[:, kk, :], attnTM[:, bi * 4 + c, kk * 128:(kk + 1) * 128], ident_b[:]) for kk in range(4)],
                            ["ident_b"], ["PTA"])
                        cp("dve", AT[:, :, c * 128:(c + 1) * 128], PTA[:], ["PTA"], ["AT"])
                    for j in range(8):
                        q = j % 2
                        dma("sp", GT[q][:], G_d.rearrange("(a j) p t -> j p a t", a=2)[j][:, :, bi * 512:(bi + 1) * 512], [], [("GT", q)], f"ldg{q}")
                        mmg(P1[q][:], [(WR[:, k, j * 128:(j + 1) * 128], RT[s][:, k, :]) for k in range(8)], ["WR", ("RT", s)], [("P1", q)])
                        mmg(P2[q][:], [(WM[:, k, j * 128:(j + 1) * 128], AT[:, k, :]) for k in range(4)], ["WM", "AT"], [("P2", q)])
                        tt("dve", tA[q][:], P1[q][:], GT[q][:, 0, :], ALU.mult, [("P1", q), ("GT", q)], [("tA", q)])
                        tt("dve", tB[q][:], P2[q][:], GT[q][:, 1, :], ALU.mult, [("P2", q), ("GT", q)], [("tB", q)])
                        tt("dve", MT[:, j, :], tA[q][:], tB[q][:], ALU.add, [("tA", q), ("tB", q)], [("MT", j)])
                    for c in range(4):
                        for hf in range(2):
                            q = (c * 2 + hf) % 2
                            mmg(P3[q][:], [(MT[:, k, c * 128:(c + 1) * 128], WO[:, k, hf * 512:(hf + 1) * 512]) for k in range(8)],
                                ["WO"] + [("MT", k) for k in range(8)], [("P3", q)])
                            tt("dve", tC[q][:], P3[q][:], m2bc[:, hf * 512:(hf + 1) * 512], ALU.mult, [("P3", q), "bcE"], [("tC", q)])
                            tt("dve", XB[s][:, c, hf * 512:(hf + 1) * 512], XB[s][:, c, hf * 512:(hf + 1) * 512], tC[q][:], ALU.add,
                               [("XB", s), ("tC", q)], [("XB", s, c, hf)])
                    xk = [("XB", s, c, hf) for c in range(4) for hf in range(2)]
                    dma("sp", o_v[bi], XB[s][:], xk, [], f"ost{s}")

                def tail(bi):
                    s = bi % 2
                    xk = [("XB", s, c, hf) for c in range(4) for hf in range(2)]
                    mset("pool", ssE[:], 1.0, ["ssE"])
                    for c in range(4):
                        act(junkE[:], XB[s][:, c, :], AF.Square, xk, ["junkE", "ssE"], accum_out=ssE[:, c:c + 1])
                    ts("dve", rsE[:], ssE[:], 1.0 / D, EPS, ALU.mult, ALU.add, ["ssE"], ["rsE"])
                    act(rsE[:], rsE[:], AF.Sqrt, ["rsE"], ["rsE"])
                    recip(rsE[:], rsE[:], ["rsE"], ["rsE"])
                    H2F = XB[s]
                    PLv = P1[0][:, 0:64].rearrange("p (c e) -> p c e", e=NE)
                    for c in range(4):
                        stt(H2F[:, c, :], XB[s][:, c, :], rsE[:, c:c + 1], a2bc, ALU.mult, ALU.mult, xk + ["rsE", "bcE"], [("H2", s, c)])
                        tt("dve", H2F[:, c, :], H2F[:, c, :], m3bc, ALU.add, [("H2", s, c), "bcE"], [("H2", s, c)])
                        cp("act", ROWS[s][:, c, 0:1024], H2F[:, c, :], [("H2", s, c)], [("ROWS", s)])
                        hq = c % 2
                        for kh in range(2):
                            trs([(P3[kh][:, kk * 128:(kk + 1) * 128], H2F[:, c, (kh * 4 + kk) * 128:(kh * 4 + kk + 1) * 128], ident_f[:]) for kk in range(4)],
                                [("H2", s, c), "ident_f"], [("P3", kh)])
                            cp("act" if kh else "dve", H2T[hq][:, kh * 4:(kh + 1) * 4, :], P3[kh][:].rearrange("p (k t) -> p k t", t=128), [("P3", kh)], [("H2T", hq, kh)])
                        mmg(PLv[:, c, :], [(H2T[hq][:, k, :], wr32[:, k, :]) for k in range(8)],
                            [("H2T", hq, 0), ("H2T", hq, 1), "wr32"], [("P1", 0)])
                    red(mxE[:], PLv, [("P1", 0)], ["mxE"], op=ALU.max)
                    ts("dve", mxE[:], mxE[:], -1.0, 0.0, ALU.mult, ALU.add, ["mxE"], ["mxE"])
                    for c in range(4):
                        act(EX[:, c, :], PLv[:, c, :], AF.Exp, [("P1", 0), "mxE"], ["EX", "smE"], bias=mxE[:, c:c + 1], accum_out=smE[:, c:c + 1])
                    recip(smE[:], smE[:], ["smE"], ["smE"])
                    tt("dve", AFF[:, bi * 4:(bi + 1) * 4, :], EX[:], smE[:].unsqueeze(2).to_broadcast([128, 4, NE]), ALU.mult, ["EX", "smE"], ["AFF"])
                    cp("dve", ROWS[s][:, :, 1024:1026].bitcast(I32), TOKID[:, bi * 4:(bi + 1) * 4].unsqueeze(2), ["TOKID"], [("ROWS", s)])
                    cp("dve", ROWS[s][:, :, 1026:1058].bitcast(F32), AFF[:, bi * 4:(bi + 1) * 4, :], ["AFF"], [("ROWS", s)])
                    dma("sp", rows_v[bi], ROWS[s][:], [("ROWS", s)], [], f"rst{s}")

                load_rt(0)
                load_xb(0)
                load_xb(1)
                front(0)
                for bi in range(1, 8):
                    front(bi)
                    tail(bi - 1)
                    load_xb(bi + 1)
                tail(7)
                if debug:
                    dma("sp", aff_dbg, AFF[:], ["AFF"], [], "dbg")
                S.flush()
        if limit <= 6:
            return nc

        gstack = es.enter_context(ExitStack())
        ROWSG = sb(gstack, "ROWSG", [128, 32, ROWW], BF16)
        WG = [sb(gstack, f"WG{i}", [128, 8, D], BF16) for i in range(2)]
        WU = [sb(gstack, f"WU{i}", [128, 8, D], BF16) for i in range(2)]
        WD = sb(gstack, "WD", [128, 8, D], BF16)
        rows_g = rows_d.rearrange("(g c p) f -> g p c f", c=8, p=128)
        for g in range(4):
            dma("sp", ROWSG[:, g * 8:(g + 1) * 8, :], rows_g[g], [], [("RG", g)], "ldrg")

        def L_gu(e_):
            if e_ >= NE:
                return
            s = e_ % 2
            dma("pool", WG[s][:], weg_d[e_].rearrange("(k p) f -> p k f", p=128), [], [("WG", s)], f"wg_{s}")
            dma("pool", WU[s][:], weu_d[e_].rearrange("(k p) f -> p k f", p=128), [], [("WU", s)], f"wu_{s}")

        def L_d(e_):
            if e_ >= NE:
                return
            dma("pool", WD[:], wed_d[e_].rearrange("(k p) f -> p k f", p=128), [], ["WD"], "wd_")

        L_gu(0)
        L_d(0)
        L_gu(1)
        with ExitStack() as ph:
            LO = sb(ph, "LO", [128, NE], F32)
            HI = sb(ph, "HI", [128, NE], F32)
            MID = sb(ph, "MID", [128, NE], F32)
            D1 = sb(ph, "D1", [128, NE], F32)
            D2 = sb(ph, "D2", [128, NE], F32)
            GE = sb(ph, "GE", [128, NE], F32)
            MASK = sb(ph, "MASK", [128, 32, NE], BF16)
            CNTP = sb(ph, "CNTP", [128, NE], F32)
            ones32 = sb(ph, "ones32", [128, 128], F32)
            ones_b = sb(ph, "ones_b", [128, 128], BF16)
            ones_f = sb(ph, "ones_f", [128, 32], F32)
            LTRI = sb(ph, "LTRI", [128, 128], BF16)
            LTF = sb(ph, "LTF", [128, 128], F32)
            MF = sb(ph, "MF", [128, 32, NE], F32)
            TOT = sb(ph, "TOT", [128, 32, NE], F32)
            CUM = sb(ph, "CUM", [128, 32, NE], F32)
            POS = sb(ph, "POS", [128, 32, NE], F32)
            VAL = sb(ph, "VAL", [128, 32, NE], F32)
            OFI = sb(ph, "OFI", [128, 32, NE], I32)
            OFF = sb(ph, "OFF", [128, 32, NE], F32)
            PC = ps(ph, "PC", [128, NE], F32)
            PP = ps(ph, "PP", [128, 512], F32)
            PTOT = ps(ph, "PTOT", [128, 512], F32)
            mset("pool", ones_b[:], 1.0, ["ones_b"])
            mset("pool", ones32[:], 1.0, ["ones32"])
            mset("pool", ones_f[:], 1.0, ["ones_f"])
            mset("pool", LTF[:], 1.0, ["LTF"])
            S.op("pool", lambda e: e.affine_select(out=LTF[:], in_=LTF[:], pattern=[[1, 128]], compare_op=ALU.is_ge, fill=0.0,
                                                    base=-1, channel_multiplier=-1), ["LTF"], ["LTF"])
            cp("pool", LTRI[:], LTF[:], ["LTF"], ["LTRI"])
            S.op("pool", lambda e: e.iota(OFI[:], pattern=[[0, 32], [CAP, NE]], base=-int(BIG), channel_multiplier=0), [], ["OFI"])
            cp("pool", OFF[:], OFI[:], ["OFI"], ["OFF"])
            mset("dve", LO[:], 0.0, ["LO"])
            mset("dve", HI[:], 1.0, ["HI"])
            mset("dve", MID[:], 0.5, ["MID"])
            for it in range(30):
                tt("dve", MASK[:], AFF[:], MID[:].unsqueeze(1).to_broadcast([128, 32, NE]), ALU.is_gt, ["AFF", "MID"], ["MASK"])
                red(CNTP[:], MASK[:].rearrange("p c e -> p e c"), ["MASK"], ["CNTP"])
                mmg(PC[:], [(ones32[:], CNTP[:])], ["ones32", "CNTP"], ["PC"])
                ts("dve", GE[:], PC[:], CAP - 0.5, 2.0 ** -(it + 1), ALU.is_ge, ALU.mult, ["PC"], ["GE"])
                tt("dve", LO[:], LO[:], GE[:], ALU.add, ["LO", "GE"], ["LO"])
                ts("dve", MID[:], LO[:], 2.0 ** -(it + 2), 0.0, ALU.add, ALU.add, ["LO"], ["MID"])
            tt("dve", MASK[:], AFF[:], LO[:].unsqueeze(1).to_broadcast([128, 32, NE]), ALU.is_gt, ["AFF", "LO"], ["MASK"])
            cp("pool", MF[:], MASK[:], ["MASK"], ["MF"])
            mflat = MASK[:].rearrange("p c e -> p (c e)")
            mmg(PP[:], [(LTRI[:], mflat)], ["LTRI", "MASK"], ["PP"])
            mmg(PTOT[:], [(ones_b[:], mflat)], ["ones_b", "MASK"], ["PTOT"])
            cp("act", TOT[:].rearrange("p c e -> p (c e)"), PTOT[:], ["PTOT"], ["TOT"])
            for e_ in range(NE):
                S.op("dve", lambda e, e_=e_: e.tensor_tensor_scan(CUM[:, :, e_], ones_f[:], TOT[:, :, e_], 0.0, ALU.mult, ALU.add),
                     ["ones_f", "TOT"], [("CUM", e_)])
            ck = [("CUM", e_) for e_ in range(NE)]
            tt("dve", CUM[:], CUM[:], TOT[:], ALU.subtract, ck + ["TOT"], ["CUMX"])
            tt("dve", POS[:].rearrange("p c e -> p (c e)"), PP[:], CUM[:].rearrange("p c e -> p (c e)"), ALU.add, ["PP", "CUMX"], ["POS"])
            ts("dve", VAL[:], POS[:], float(CAP) - 0.5, 0.0, ALU.is_lt, ALU.add, ["POS"], ["VAL"])
            tt("dve", VAL[:], VAL[:], MF[:], ALU.mult, ["VAL", "MF"], ["VAL"])
            tt("dve", POS[:], POS[:], OFF[:], ALU.add, ["POS", "OFF"], ["POS"])
            tt("dve", POS[:], POS[:], VAL[:], ALU.mult, ["POS", "VAL"], ["POS"])
            ts("dve", POS[:], POS[:], BIG, 0.0, ALU.add, ALU.add, ["POS"], ["POS"])
            cp("dve", IDX[:], POS[:], ["POS"], ["IDX"])
            if debug:
                dma("sp", idx_dbg, IDX[:], ["IDX"], [], "dbg")
            S.flush()
        if limit <= 7:
            return nc

        with ExitStack() as ph:
            XG = sb(ph, "XG", [128, 4, ROWW], BF16)
            TI = [sb(ph, f"TI{i}", [128, 4], I32) for i in range(2)]
            GA = [sb(ph, f"GA{i}", [128, 4], F32) for i in range(2)]
            XGT = sb(ph, "XGT", [128, 8, 512], BF16)
            HID = sb(ph, "HID", [128, 8, 512], BF16)
            SGT = [sb(ph, f"SGT{i}", [128, 512], F32) for i in range(2)]
            YO = sb(ph, "YO", [128, 4, D], F32)
            m5 = sb(ph, "m5", [128, D], F32)
            PTX = [ps(ph, f"PTX{i}", [128, 4, 128], BF16) for i in range(2)]
            PG = [ps(ph, f"PG{i}", [128, 512], F32) for i in range(2)]
            PU = [ps(ph, f"PU{i}", [128, 512], F32) for i in range(2)]
            PY = [ps(ph, f"PY{i}", [128, 512], F32) for i in range(2)]
            dma("sp", m5[:], bc_d[:, 3072:4096], [], ["m5"], "ldg")
            def scat(e_):
                if e_ >= NE:
                    return
                for ci in range(32):
                    S.op("pool", lambda e, ci=ci, e_=e_: e.indirect_dma_start(
                        out=xg_d, out_offset=bass.IndirectOffsetOnAxis(ap=IDX[:, ci, e_:e_ + 1], axis=0),
                        in_=ROWSG[:, ci, :], in_offset=None, bounds_check=R["bc_xg"], oob_is_err=False),
                        [("RG", ci // 8)], [("xg", e_, ci)], dma=f"sc{e_}")

            scat(0)
            scat(1)
            scat(2)
            xg_v = xg_d.rearrange("(e g p) f -> e p g f", g=4, p=128)
            for e_ in range(NE):
                s = e_ % 2
                dma("sp", XG[:], xg_v[e_], [("xg", e_, ci) for ci in range(32)], ["XG"], "xgl")
                cp("dve", TI[s][:].unsqueeze(2), XG[:, :, 1024:1026].bitcast(I32), ["XG"], [("TI", s)])
                cp("dve", GA[s][:].unsqueeze(2), XG[:, :, 1026 + 2 * e_:1028 + 2 * e_].bitcast(F32), ["XG"], [("GA", s)])
                for k in range(8):
                    trs([(PTX[k % 2][:, g, :], XG[:, g, k * 128:(k + 1) * 128], ident_b[:]) for g in range(4)],
                        ["XG", "ident_b"], [("PTX", k % 2)])
                    cp("act" if k % 2 else "dve", XGT[:, k, :], PTX[k % 2][:].rearrange("p g t -> p (g t)"), [("PTX", k % 2)], [("XGT", k)])
                xk = [("XGT", k) for k in range(8)]
                for f in range(8):
                    q = f % 2
                    mmg(PG[q][:], [(WG[s][:, k, f * 128:(f + 1) * 128], XGT[:, k, :]) for k in range(8)], xk + [("WG", s)], [("PG", q)])
                    mmg(PU[q][:], [(WU[s][:, k, f * 128:(f + 1) * 128], XGT[:, k, :]) for k in range(8)], xk + [("WU", s)], [("PU", q)])
                    act(SGT[q][:], PG[q][:], AF.Silu, [("PG", q)], [("SGT", q)])
                    tt("dve", HID[:, f, :], SGT[q][:], PU[q][:], ALU.mult, [("SGT", q), ("PU", q)], [("HID", f)])
                L_gu(e_ + 2)
                hk = [("HID", f) for f in range(8)]
                for g in range(4):
                    for hf in range(2):
                        q = (g * 2 + hf) % 2
                        mmg(PY[q][:], [(HID[:, f, g * 128:(g + 1) * 128], WD[:, f, hf * 512:(hf + 1) * 512]) for f in range(8)],
                            hk + ["WD"], [("PY", q)])
                        stt(YO[:, g, hf * 512:(hf + 1) * 512], PY[q][:], GA[s][:, g:g + 1], m5[:, hf * 512:(hf + 1) * 512], ALU.mult, ALU.mult,
                            [("PY", q), ("GA", s), "m5"], [("YO", g)])
                    if g == 3:
                        L_d(e_ + 1)
                    S.op("pool", lambda e, s=s, g=g: e.indirect_dma_start(
                        out=out_d, out_offset=bass.IndirectOffsetOnAxis(ap=TI[s][:, g:g + 1], axis=0),
                        in_=YO[:, g, :], in_offset=None, bounds_check=R["bc_out"], oob_is_err=True, compute_op=ALU.add),
                        [("YO", g), ("TI", s)] + [("outd", gg) for gg in range(4) if gg != g], [("outd", g)], dma="sadd")
                scat(e_ + 3)
            S.flush()
    return nc


_CACHE = {}


def _rope_tables():
    rows = L // 64
    row = np.repeat(np.arange(rows, dtype=np.float32), 64)
    col = np.tile(np.arange(64, dtype=np.float32), rows)
    inv = (np.float32(10000.0) ** (-np.arange(8, dtype=np.float32) / np.float32(8))).astype(np.float32)
    ang = np.concatenate([row[:, None] * inv, col[:, None] * inv], axis=-1).astype(np.float32)
    return np.cos(ang).astype(np.float32), np.sin(ang).astype(np.float32)


def _fm(v):
    return np.ascontiguousarray(np.moveaxis(v.reshape(*v.shape[:-1], 8, 128), -1, 0))


def make_in_maps(inp):
    f = lambda a: np.ascontiguousarray(np.asarray(a, dtype=np.float32))
    cos, sin = _rope_tables()
    cosT = np.ascontiguousarray(cos.reshape(32, 128, 16).transpose(1, 0, 2))
    sinT = np.ascontiguousarray(sin.reshape(32, 128, 16).transpose(1, 0, 2))
    c_ctx = f(inp["c_ctx"])
    shared = dict(
        w_ada=f(inp["w_ada"][0]), b_ada2=np.ascontiguousarray(np.repeat(f(inp["b_ada"]), 2, axis=0)),
        g1T=_fm(f(inp["g_norm1"][0])), g2row=np.ascontiguousarray(np.repeat(f(inp["g_norm2"]), 2, axis=0)),
        w_in=f(inp["w_in"][0]),
        cwT=np.ascontiguousarray(_fm(f(inp["conv_w"][0])).transpose(0, 2, 1)),
        cbT=_fm(f(inp["conv_b"][0])),
        rg_wa=f(inp["rg_wa"][0]), rg_wx=f(inp["rg_wx"][0]),
        rgbT=np.ascontiguousarray(np.stack([_fm(f(inp["rg_ba"][0])), _fm(f(inp["rg_bx"][0])), _fm(f(inp["rg_lambda"][0]))], axis=1)),
        w_rnn_out=f(inp["w_rnn_out"][0]), w_mla_out=f(inp["w_mla_out"][0]), w_o=f(inp["w_o"][0]),
        w_uq=f(inp["w_uq"][0]), gql256=np.ascontiguousarray(np.tile(f(inp["g_q_lora"][0])[None, :], (128, 1))),
        w_uk=f(inp["w_uk"][0]), w_uv=f(inp["w_uv"][0]), gkv128=np.ascontiguousarray(np.tile(f(inp["g_kv_lora"][0])[None, :], (128, 1))),
        gq96=np.ascontiguousarray(np.tile(np.concatenate([f(inp["g_q_nope"][0]), f(inp["g_q_rope"][0])])[None, :], (128, 1))),
        gk96=np.ascontiguousarray(np.tile(np.concatenate([f(inp["g_k_nope"][0]), f(inp["g_k_rope"][0])])[None, :], (128, 1))),
        cosT=cosT, sinT=sinT, w_router=f(inp["w_router"][0]),
        w_e_gate=f(inp["w_e_gate"][0]), w_e_up=f(inp["w_e_up"][0]), w_e_down=f(inp["w_e_down"][0]),
    )
    maps = []
    for b in range(8):
        c2 = np.stack([f(inp["c"][b]), c_ctx], axis=0)
        m = dict(shared)
        m["x"] = f(inp["x"][b])
        m["ctx"] = f(inp["ctx"][b])
        m["c2T"] = np.ascontiguousarray(c2.reshape(2, 8, 128).transpose(2, 1, 0))
        maps.append(m)
    return maps


def kernel(**inputs):
    if "nc" not in _CACHE:
        _CACHE["nc"] = build_program()
    nc = _CACHE["nc"]
    in_maps = make_in_maps(inputs)
    res = run_bass_kernel_spmd(nc, in_maps, core_ids=list(range(8)))
    return np.stack([np.asarray(r["out"], dtype=np.float32) for r in res.results], axis=0)
```
